# Optimizing a Trainium2 kernel written in Bass

```python
import jax, jax.numpy as jnp
from jax import lax
import numpy as np

D_MODEL = 1024
BATCH = 8
SEQ = 8192
DEPTH = 2

CTX_LEN = 256
GRID_W = 64

CONV_CH = 256
CONV_WIDTH = 31
MLA_HEADS = 4
MLA_NOPE = 128
MLA_ROPE = 64
MLA_V = 128
MLA_Q_RANK = 384
MLA_KV_RANK = 256
RET_HEADS = 4
RET_DK = 32
RET_DV = 64
RET_CHUNK = 128
ATTN_BLOCK = 128
MIX_WIDTH = CONV_CH + MLA_HEADS * MLA_V + RET_HEADS * RET_DV

N_EXPERTS = 32
TOP_K = 4
D_FF_EXPERT = 1024
SWIGLU_LIMIT = 7.0
SWIGLU_ALPHA = 1.702
MOE_BLOCK = 128

ROPE_BASE = 10000.0
RMS_EPS = 1e-6
LN_EPS = 1e-5

COL_CONV = 2 * CONV_CH
COL_QSIDE = MLA_Q_RANK + RET_HEADS * RET_DK + RET_HEADS * RET_DV
COL_KVSIDE = MLA_KV_RANK + MLA_ROPE + RET_HEADS * RET_DK + RET_HEADS * RET_DV
IN_COLS = COL_CONV + COL_QSIDE + COL_KVSIDE
KV_OFF = COL_CONV + COL_QSIDE

kernel_name = 'hybrid_conv_mla_retention_moe_dit'


def rms_norm(x, g):
    xf = x.astype(jnp.float32)
    y = xf * lax.rsqrt(jnp.mean(xf * xf, axis=-1, keepdims=True) + RMS_EPS)
    return (y * g.astype(jnp.float32)).astype(x.dtype)


def rms_norm_plain(x):
    xf = x.astype(jnp.float32)
    return (xf * lax.rsqrt(jnp.mean(xf * xf, axis=-1, keepdims=True) + RMS_EPS)).astype(x.dtype)


def layer_norm(x, g, b):
    xf = x.astype(jnp.float32)
    mu = jnp.mean(xf, axis=-1, keepdims=True)
    var = jnp.mean(jnp.square(xf - mu), axis=-1, keepdims=True)
    return ((xf - mu) * lax.rsqrt(var + LN_EPS) * g.astype(jnp.float32) + b.astype(jnp.float32)).astype(x.dtype)


def rotate_half(x, cos, sin):
    half = x.shape[-1] // 2
    cos = cos.astype(x.dtype)
    sin = sin.astype(x.dtype)
    x1, x2 = x[..., :half], x[..., half:]
    return jnp.concatenate([x1 * cos - x2 * sin, x2 * cos + x1 * sin], axis=-1)


def axial_rope(rows):
    row = jnp.repeat(jnp.arange(rows), GRID_W).astype(jnp.float32)
    col = jnp.tile(jnp.arange(GRID_W), rows).astype(jnp.float32)
    n_pairs_axis = MLA_ROPE // 4
    freq = ROPE_BASE ** (-jnp.arange(n_pairs_axis, dtype=jnp.float32) / n_pairs_axis)
    ang = jnp.concatenate([row[:, None] * freq, col[:, None] * freq], axis=-1)
    return jnp.cos(ang), jnp.sin(ang)


def retention_rot(pos):
    theta = 1.0 / (ROPE_BASE ** jnp.linspace(0.0, 1.0, RET_DK // 2, dtype=jnp.float32))
    ang = pos.astype(jnp.float32)[:, None] * theta
    return jnp.cos(ang), jnp.sin(ang)


def split_heads(t, n_heads):
    b, n, _ = t.shape
    return t.reshape(b, n, n_heads, -1).transpose(0, 2, 1, 3)


def merge_heads(t):
    b, h, n, d = t.shape
    return t.transpose(0, 2, 1, 3).reshape(b, n, h * d)


def conv_module(t, w_dw, b_dw, ln_g, ln_b):
    a, gate = t[..., :CONV_CH], t[..., CONV_CH:]
    z = a * jax.nn.sigmoid(gate)
    pad = CONV_WIDTH // 2
    z = lax.conv_general_dilated(z, w_dw.astype(z.dtype)[:, None, :], window_strides=(1,),
                                 padding=[(pad, pad)], dimension_numbers=('NWC', 'WIO', 'NWC'),
                                 feature_group_count=CONV_CH) + b_dw
    return jax.nn.silu(layer_norm(z, ln_g, ln_b))


def mla_query(cq, q_norm, w_uq, rope):
    q = split_heads(rms_norm(cq, q_norm) @ w_uq, MLA_HEADS)
    q_nope, q_rope = q[..., :MLA_NOPE], q[..., MLA_NOPE:]
    if rope is not None:
        q_rope = rotate_half(q_rope, *rope)
    return jnp.concatenate([q_nope, q_rope], axis=-1)


def mla_keys_values(ckv, k_rope, kv_norm, w_ukv, rope):
    kv = split_heads(rms_norm(ckv, kv_norm) @ w_ukv, MLA_HEADS)
    k_nope, v = kv[..., :MLA_NOPE], kv[..., MLA_NOPE:]
    k_rope = k_rope[:, None]
    if rope is not None:
        k_rope = rotate_half(k_rope, *rope)
    k = jnp.concatenate([k_nope, jnp.broadcast_to(k_rope, k_nope.shape[:-1] + (MLA_ROPE,))], axis=-1)
    return k, v


def block_attention(q, k, v):
    b, h, n, dq = q.shape
    nb = n // ATTN_BLOCK
    scale = dq ** -0.5
    qb = jnp.moveaxis(q.reshape(b, h, nb, ATTN_BLOCK, dq), 2, 0)

    def one_block(qblk):
        s = jnp.einsum('bhqd,bhkd->bhqk', qblk, k, preferred_element_type=jnp.float32) * scale
        p = jax.nn.softmax(s, axis=-1).astype(v.dtype)
        return jnp.einsum('bhqk,bhkd->bhqd', p, v)

    out = lax.map(one_block, qb)
    return jnp.moveaxis(out, 0, 2).reshape(b, h, n, v.shape[-1])


def retention_chunks(q, k, v, log_g, s0):
    b, h, n, dk = q.shape
    dv = v.shape[-1]
    nc = n // RET_CHUNK
    qc = q.reshape(b, h, nc, RET_CHUNK, dk)
    kc = k.reshape(b, h, nc, RET_CHUNK, dk)
    vc = v.reshape(b, h, nc, RET_CHUNK, dv)
    idx = jnp.arange(RET_CHUNK, dtype=jnp.float32)
    diff = idx[:, None] - idx[None, :]
    dmat = jnp.where(diff >= 0, jnp.exp(jnp.maximum(diff, 0.0) * log_g[:, None, None]), 0.0).astype(q.dtype)
    scores = jnp.einsum('bhnid,bhnjd->bhnij', qc, kc) * dmat[:, None]
    inner = jnp.einsum('bhnij,bhnje->bhnie', scores, vc)
    k_dec = jnp.exp((RET_CHUNK - 1 - idx) * log_g[:, None]).astype(q.dtype)
    q_dec = jnp.exp((idx + 1.0) * log_g[:, None]).astype(q.dtype)
    chunk_dec = jnp.exp(RET_CHUNK * log_g).astype(q.dtype)[:, None, None]
    chunk_kv = jnp.einsum('bhnjd,hj,bhnje->bhnde', kc, k_dec, vc)

    def step(s, kv_n):
        return s * chunk_dec + kv_n, s

    s_final, s_before = lax.scan(step, s0.astype(q.dtype), jnp.moveaxis(chunk_kv, 2, 0))
    s_before = jnp.moveaxis(s_before, 0, 2)
    cross = jnp.einsum('bhnid,hi,bhnde->bhnie', qc, q_dec, s_before)
    return (inner + cross).reshape(b, h, n, dv), s_final


def retention_state(k, v, log_g):
    n = k.shape[2]
    w = jnp.exp((n - 1 - jnp.arange(n, dtype=jnp.float32)) * log_g[:, None]).astype(k.dtype)
    return jnp.einsum('bhnd,hn,bhne->bhde', k, w, v)


def retention_output(o, g):
    return jax.nn.silu(g) * merge_heads(rms_norm_plain(o))


def clamped_swiglu(hid):
    glu, lin = hid[..., :D_FF_EXPERT], hid[..., D_FF_EXPERT:]
    glu = jnp.minimum(glu, SWIGLU_LIMIT)
    lin = jnp.clip(lin, -SWIGLU_LIMIT, SWIGLU_LIMIT)
    return glu * jax.nn.sigmoid(SWIGLU_ALPHA * glu) * (lin + 1.0)


def moe_ffn(h, router_w, router_b, w1, b1, w2, b2):
    n_tok, d = h.shape
    logits = jnp.dot(h, router_w, preferred_element_type=jnp.float32) + router_b.astype(jnp.float32)
    top_val, top_idx = lax.top_k(logits, TOP_K)
    gates = jax.nn.softmax(top_val, axis=-1).astype(h.dtype)
    n_assign = n_tok * TOP_K
    e_flat = top_idx.reshape(n_assign)
    order = jnp.argsort(e_flat)
    e_sorted = e_flat[order]
    tok_sorted = (order // TOP_K).astype(jnp.int32)
    gate_sorted = gates.reshape(n_assign)[order]
    counts = jnp.bincount(e_flat, length=N_EXPERTS)
    padded = (counts + MOE_BLOCK - 1) // MOE_BLOCK * MOE_BLOCK
    padded_end = jnp.cumsum(padded)
    group_start = jnp.cumsum(counts) - counts
    dest = (padded_end - padded)[e_sorted] + jnp.arange(n_assign) - group_start[e_sorted]
    n_blocks = -(-n_assign // MOE_BLOCK) + N_EXPERTS
    n_slots = n_blocks * MOE_BLOCK
    slot_tok = jnp.full((n_slots,), n_tok, jnp.int32).at[dest].set(tok_sorted)
    slot_gate = jnp.zeros((n_slots,), h.dtype).at[dest].set(gate_sorted)
    block_expert = jnp.minimum(
        jnp.searchsorted(padded_end, jnp.arange(n_blocks) * MOE_BLOCK, side='right'), N_EXPERTS - 1)

    def expert_block(args):
        tok, gate, e = args
        xb = jnp.take(h, tok, axis=0, mode='fill', fill_value=0)
        hid = xb @ w1[e] + b1[e]
        return (clamped_swiglu(hid) @ w2[e] + b2[e]) * gate[:, None]

    out = lax.map(expert_block, (slot_tok.reshape(n_blocks, MOE_BLOCK),
                                 slot_gate.reshape(n_blocks, MOE_BLOCK), block_expert))
    return jnp.zeros_like(h).at[slot_tok].add(out.reshape(n_slots, d), mode='drop')


def hybrid_layer(x, xc, mod, mod_c, p, rope_lat, rot_ctx, rot_lat, update_ctx):
    b, L, d = x.shape
    C = xc.shape[1]
    sh1, sc1, gt1, sh2, sc2, gt2 = jnp.split(mod[:, None, :], 6, axis=-1)
    csh1, csc1, cgt1, csh2, csc2, cgt2 = jnp.split(mod_c, 6)

    h = rms_norm(x, p['g_norm1']) * (1 + sc1) + sh1
    hc = rms_norm(xc, p['g_norm1']) * (1 + csc1) + csh1
    u = h @ p['w_in']
    if update_ctx:
        uc = hc @ p['w_in']
        uc_kv = uc[..., KV_OFF:]
    else:
        uc_kv = hc @ p['w_in'][:, KV_OFF:]

    def q_side(t):
        t = t[..., COL_CONV:KV_OFF]
        o1 = MLA_Q_RANK
        o2 = o1 + RET_HEADS * RET_DK
        return t[..., :o1], t[..., o1:o2], t[..., o2:]

    def kv_side(t):
        o1 = MLA_KV_RANK
        o2 = o1 + MLA_ROPE
        o3 = o2 + RET_HEADS * RET_DK
        return t[..., :o1], t[..., o1:o2], t[..., o2:o3], t[..., o3:]

    cq, rq, rg = q_side(u)
    ckv, krope, rk, rv = kv_side(u[..., KV_OFF:])
    ckv_c, krope_c, rk_c, rv_c = kv_side(uc_kv)

    y_conv = conv_module(u[..., :COL_CONV], p['conv_w'], p['conv_b'], p['conv_ln_g'], p['conv_ln_b'])

    q_l = mla_query(cq, p['mla_q_norm'], p['mla_w_uq'], rope_lat)
    k_l, v_l = mla_keys_values(ckv, krope, p['mla_kv_norm'], p['mla_w_ukv'], rope_lat)
    k_c, v_c = mla_keys_values(ckv_c, krope_c, p['mla_kv_norm'], p['mla_w_ukv'], None)
    o_mla = block_attention(q_l, jnp.concatenate([k_l, k_c], axis=2), jnp.concatenate([v_l, v_c], axis=2))

    log_gf = jax.nn.log_sigmoid(p['ret_decay_fwd'].astype(jnp.float32))
    log_gb = jax.nn.log_sigmoid(p['ret_decay_bwd'].astype(jnp.float32))
    k_scale = RET_DK ** -0.5
    rq_l = rotate_half(split_heads(rq, RET_HEADS), *rot_lat)
    rk_l = rotate_half(split_heads(rk, RET_HEADS), *rot_lat) * k_scale
    rv_l = split_heads(rv, RET_HEADS)
    rk_cc = rotate_half(split_heads(rk_c, RET_HEADS), *rot_ctx) * k_scale
    rv_cc = split_heads(rv_c, RET_HEADS)
    flip = lambda t: jnp.flip(t, axis=2)
    if update_ctx:
        cq_c, rq_c, rg_c = q_side(uc)
        rq_cc = rotate_half(split_heads(rq_c, RET_HEADS), *rot_ctx)
        zero_s = jnp.zeros((b, RET_HEADS, RET_DK, RET_DV), x.dtype)
        oc_f, s_f = retention_chunks(rq_cc, rk_cc, rv_cc, log_gf, zero_s)
        oc_b, s_b = retention_chunks(flip(rq_cc), flip(rk_cc), flip(rv_cc), log_gb, zero_s)
    else:
        s_f = retention_state(rk_cc, rv_cc, log_gf)
        s_b = retention_state(flip(rk_cc), flip(rv_cc), log_gb)
    o_f, _ = retention_chunks(rq_l, rk_l, rv_l, log_gf, s_f)
    o_b, _ = retention_chunks(flip(rq_l), flip(rk_l), flip(rv_l), log_gb, s_b)
    y_ret = retention_output(o_f + flip(o_b), rg)

    mix = jnp.concatenate([y_conv, merge_heads(o_mla), y_ret], axis=-1) @ p['w_out']
    x = x + gt1 * mix

    if update_ctx:
        y_conv_c = conv_module(uc[..., :COL_CONV], p['conv_w'], p['conv_b'], p['conv_ln_g'], p['conv_ln_b'])
        q_c = mla_query(cq_c, p['mla_q_norm'], p['mla_w_uq'], None)
        o_mla_c = block_attention(q_c, k_c, v_c)
        y_ret_c = retention_output(oc_f + flip(oc_b), rg_c)
        mix_c = jnp.concatenate([y_conv_c, merge_heads(o_mla_c), y_ret_c], axis=-1) @ p['w_out']
        xc = xc + cgt1 * mix_c

    h2 = rms_norm(x, p['g_norm2']) * (1 + sc2) + sh2
    moe_args = (p['router_w'], p['router_b'], p['moe_w1'], p['moe_b1'], p['moe_w2'], p['moe_b2'])
    if update_ctx:
        h2c = rms_norm(xc, p['g_norm2']) * (1 + csc2) + csh2
        tokens = jnp.concatenate([h2.reshape(b * L, d), h2c.reshape(b * C, d)], axis=0)
        y = moe_ffn(tokens, *moe_args)
        x = x + gt2 * y[:b * L].reshape(b, L, d)
        xc = xc + cgt2 * y[b * L:].reshape(b, C, d)
    else:
        x = x + gt2 * moe_ffn(h2.reshape(b * L, d), *moe_args).reshape(b, L, d)
    return x, xc


def setup_inputs(seed: int = 0) -> dict:
    key = jax.random.key(seed)
    ks = jax.random.split(key, 32)
    f32 = jnp.float32
    nrm = lambda k, shape, s: jax.random.normal(k, shape, f32) * s
    D = D_MODEL
    base_logit = jnp.log(2.0 ** (5.0 + jnp.arange(RET_HEADS, dtype=f32)) - 1.0)
    return {
        'x': nrm(ks[0], (BATCH, SEQ, D), 1.0),
        'c': nrm(ks[1], (BATCH, D), 1.0),
        'ctx': nrm(ks[2], (BATCH, CTX_LEN, D), 1.0),
        'c_ctx': nrm(ks[3], (D,), 1.0),
        'w_ada': nrm(ks[4], (DEPTH, D, 6 * D), 0.5 * D ** -0.5),
        'b_ada': nrm(ks[5], (DEPTH, 6 * D), 0.02),
        'g_norm1': 1.0 + nrm(ks[6], (DEPTH, D), 0.05),
        'g_norm2': 1.0 + nrm(ks[7], (DEPTH, D), 0.05),
        'w_in': nrm(ks[8], (DEPTH, D, IN_COLS), D ** -0.5),
        'w_out': nrm(ks[9], (DEPTH, MIX_WIDTH, D), MIX_WIDTH ** -0.5),
        'conv_w': nrm(ks[10], (DEPTH, CONV_WIDTH, CONV_CH), CONV_WIDTH ** -0.5),
        'conv_b': nrm(ks[11], (DEPTH, CONV_CH), 0.02),
        'conv_ln_g': 1.0 + nrm(ks[12], (DEPTH, CONV_CH), 0.05),
        'conv_ln_b': nrm(ks[13], (DEPTH, CONV_CH), 0.02),
        'mla_q_norm': 1.0 + nrm(ks[14], (DEPTH, MLA_Q_RANK), 0.05),
        'mla_w_uq': nrm(ks[15], (DEPTH, MLA_Q_RANK, MLA_HEADS * (MLA_NOPE + MLA_ROPE)), MLA_Q_RANK ** -0.5),
        'mla_kv_norm': 1.0 + nrm(ks[16], (DEPTH, MLA_KV_RANK), 0.05),
        'mla_w_ukv': nrm(ks[17], (DEPTH, MLA_KV_RANK, MLA_HEADS * (MLA_NOPE + MLA_V)), MLA_KV_RANK ** -0.5),
        'ret_decay_fwd': base_logit + nrm(ks[18], (DEPTH, RET_HEADS), 0.1),
        'ret_decay_bwd': base_logit + nrm(ks[19], (DEPTH, RET_HEADS), 0.1),
        'router_w': nrm(ks[20], (DEPTH, D, N_EXPERTS), D ** -0.5),
        'router_b': nrm(ks[21], (DEPTH, N_EXPERTS), 0.01),
        'moe_w1': nrm(ks[22], (DEPTH, N_EXPERTS, D, 2 * D_FF_EXPERT), D ** -0.5),
        'moe_b1': nrm(ks[23], (DEPTH, N_EXPERTS, 2 * D_FF_EXPERT), 0.02),
        'moe_w2': nrm(ks[24], (DEPTH, N_EXPERTS, D_FF_EXPERT, D), D_FF_EXPERT ** -0.5),
        'moe_b2': nrm(ks[25], (DEPTH, N_EXPERTS, D), 0.02),
        'g_final': 1.0 + nrm(ks[26], (D,), 0.05),
    }


def reference(x, c, ctx, c_ctx, w_ada, b_ada, g_norm1, g_norm2, w_in, w_out, conv_w, conv_b,
              conv_ln_g, conv_ln_b, mla_q_norm, mla_w_uq, mla_kv_norm, mla_w_ukv, ret_decay_fwd,
              ret_decay_bwd, router_w, router_b, moe_w1, moe_b1, moe_w2, moe_b2, g_final):
    L = x.shape[1]
    C = ctx.shape[1]
    ROWS = L // GRID_W
    rope_lat = axial_rope(ROWS)
    rot_ctx = retention_rot(jnp.arange(C))
    rot_lat = retention_rot(C + jnp.arange(L))
    s_c = jax.nn.silu(c)
    s_cc = jax.nn.silu(c_ctx)
    xc = ctx
    for l in range(DEPTH):
        mod = s_c @ w_ada[l] + b_ada[l]
        mod_c = s_cc @ w_ada[l] + b_ada[l]
        p = {
            'g_norm1': g_norm1[l], 'g_norm2': g_norm2[l], 'w_in': w_in[l], 'w_out': w_out[l],
            'conv_w': conv_w[l], 'conv_b': conv_b[l], 'conv_ln_g': conv_ln_g[l], 'conv_ln_b': conv_ln_b[l],
            'mla_q_norm': mla_q_norm[l], 'mla_w_uq': mla_w_uq[l], 'mla_kv_norm': mla_kv_norm[l],
            'mla_w_ukv': mla_w_ukv[l], 'ret_decay_fwd': ret_decay_fwd[l], 'ret_decay_bwd': ret_decay_bwd[l],
            'router_w': router_w[l], 'router_b': router_b[l], 'moe_w1': moe_w1[l], 'moe_b1': moe_b1[l],
            'moe_w2': moe_w2[l], 'moe_b2': moe_b2[l],
        }
        x, xc = hybrid_layer(x, xc, mod, mod_c, p, rope_lat, rot_ctx, rot_lat, l < DEPTH - 1)
    return rms_norm(x, g_final)
```

```python
from contextlib import ExitStack
import numpy as np
import concourse.bass as bass
import concourse.mybir as mybir
from concourse.bass_utils import run_bass_kernel_spmd

ALU = mybir.AluOpType
AF = mybir.ActivationFunctionType
AX = mybir.AxisListType
F32 = mybir.dt.float32
BF16 = mybir.dt.bfloat16

NSLOT = 12


class Buf:
    __slots__ = ("name", "lw", "rd")

    def __init__(self, name=""):
        self.name = name
        self.lw = None
        self.rd = []


class T:
    def __init__(self, t, b):
        self.t = t
        self.b = b

    def __getitem__(self, k):
        return self.t[k]


class Pool:
    def __init__(self, tiles):
        self.tiles = tiles
        self.i = 0

    def next(self):
        t = self.tiles[self.i % len(self.tiles)]
        self.i += 1
        return t


class Sched:
    def __init__(self, nc):
        self.nc = nc
        self.engs = {"pe": nc.tensor, "act": nc.scalar, "dve": nc.vector, "pool": nc.gpsimd, "sp": nc.sync}
        self.ops = {k: [] for k in self.engs}
        self.cnt = {k: 0 for k in self.engs}
        self.sem = {k: nc.alloc_semaphore("s_" + k) for k in self.engs}
        self.dq = ("sp", "act", "pool")
        self.dsem = {q: [nc.alloc_semaphore("d_%s_%d" % (q, i)) for i in range(NSLOT)] for q in self.dq}
        self.dcnt = {q: 0 for q in self.dq}
        self.known = {k: {} for k in self.engs}
        self.stack = None
        self.uid = 0
        self.dbufs = {}

    def _nm(self, name):
        self.uid += 1
        return "%s_%d" % (name, self.uid)

    def sb(self, name, shape, dtype=F32):
        nm = self._nm(name)
        if self.stack is not None:
            t = self.stack.enter_context(self.nc.sbuf_tensor(nm, list(shape), dtype))
        else:
            t = self.nc.alloc_sbuf_tensor(nm, list(shape), dtype)
        return T(t, Buf(nm))

    def ps(self, name, shape, dtype=F32):
        nm = self._nm(name)
        if self.stack is not None:
            t = self.stack.enter_context(self.nc.psum_tensor(nm, list(shape), dtype))
        else:
            t = self.nc.alloc_psum_tensor(nm, list(shape), dtype)
        return T(t, Buf(nm))

    def sbpool(self, name, shape, dtype, n):
        return Pool([self.sb(name, shape, dtype) for _ in range(n)])

    def pspool(self, name, shape, dtype, n):
        return Pool([self.ps(name, shape, dtype) for _ in range(n)])

    def db(self, *key):
        b = self.dbufs.get(key)
        if b is None:
            b = Buf(str(key))
            self.dbufs[key] = b
        return b

    class _Phase:
        def __init__(self, s):
            self.s = s

        def __enter__(self):
            self.s.stack = ExitStack()
            self.s.stack.__enter__()
            return self

        def __exit__(self, *a):
            self.s.barrier()
            st = self.s.stack
            self.s.stack = None
            st.__exit__(None, None, None)
            return False

    def phase(self):
        return Sched._Phase(self)

    def _need(self, eng, ev, waits):
        if ev is None:
            return
        sem, val, src = ev
        if src == eng and eng == "pe":
            return
        if self.known[eng].get(sem, 0) >= val:
            return
        if waits.get(sem, (None, 0))[1] < val:
            waits[sem] = (sem, val)

    def _deps(self, eng, reads, writes):
        waits = {}
        for b in reads:
            b = b.b if isinstance(b, T) else b
            self._need(eng, b.lw, waits)
        for b in writes:
            b = b.b if isinstance(b, T) else b
            self._need(eng, b.lw, waits)
            for r in b.rd:
                self._need(eng, r, waits)
        for sem, (s_, v) in waits.items():
            self.known[eng][sem] = v
        return list(waits.values())

    def _commit(self, ev, reads, writes):
        for b in reads:
            b = b.b if isinstance(b, T) else b
            b.rd.append(ev)
        for b in writes:
            b = b.b if isinstance(b, T) else b
            b.lw = ev
            b.rd = []

    def op(self, eng, fn, reads=(), writes=()):
        waits = self._deps(eng, reads, writes)
        self.cnt[eng] += 1
        ev = (self.sem[eng], self.cnt[eng], eng)
        self.ops[eng].append((waits, fn, (self.sem[eng], 1)))
        self._commit(ev, reads, writes)

    def dma(self, q, out, in_, reads=(), writes=(), **kw):
        waits = self._deps(q, reads, writes)
        i = self.dcnt[q]
        self.dcnt[q] += 1
        slot = i % NSLOT
        sem = self.dsem[q][slot]
        rnd = i // NSLOT
        if rnd > 0:
            w = {}
            self._need(q, (sem, 16 * rnd, "dma"), w)
            for sem_, (s_, v) in w.items():
                self.known[q][sem_] = v
            waits = waits + list(w.values())
        ev = (sem, 16 * (rnd + 1), "dma")
        fn = lambda e, out=out, in_=in_, kw=kw: e.dma_start(out=out, in_=in_, **kw)
        self.ops[q].append((waits, fn, (sem, 16)))
        self._commit(ev, reads, writes)

    def _all_events(self):
        evs = []
        for k in self.engs:
            if self.cnt[k] > 0:
                evs.append((self.sem[k], self.cnt[k], k))
        for qq in self.dq:
            n = self.dcnt[qq]
            for slot in range(min(n, NSLOT)):
                cnt = (n - slot + NSLOT - 1) // NSLOT
                evs.append((self.dsem[qq][slot], 16 * cnt, "dma"))
        return evs

    def barrier(self):
        evs = self._all_events()
        for eng in self.engs:
            w = {}
            for ev in evs:
                if ev[2] == eng:
                    continue
                self._need(eng, ev, w)
            for sem_, (s_, v) in w.items():
                self.known[eng][sem_] = v
            if w:
                self.ops[eng].append((list(w.values()), None, None))

    def finish(self):
        self.barrier()

    def emit(self):
        nc = self.nc
        with nc.Block() as block:
            def run(name):
                def body(e):
                    for waits, fn, inc in self.ops[name]:
                        for sem, val in waits:
                            e.wait_ge(sem, val)
                        if fn is not None:
                            ins = fn(e)
                            ins.then_inc(inc[0], inc[1])
                return body

            block.tensor(run("pe"))
            block.scalar(run("act"))
            block.vector(run("dve"))
            block.gpsimd(run("pool"))
            block.sync(run("sp"))

    def mm(self, out, lhsT, rhs, start, stop, R, W):
        self.op("pe", lambda e: e.matmul(out, lhsT=lhsT, rhs=rhs, start=start, stop=stop), R, W)

    def tr(self, out, in_, ident, R, W):
        self.op("pe", lambda e: e.transpose(out=out, in_=in_, identity=ident), R, W)

    def act(self, out, in_, func, R, W, **kw):
        self.op("act", lambda e: e.activation(out=out, in_=in_, func=func, **kw), R, W)

    def ts(self, eng, out, in0, s1, s2, op0, op1, R, W):
        if op1 is None:
            self.op(eng, lambda e: e.tensor_scalar(out=out, in0=in0, scalar1=s1, scalar2=None, op0=op0), R, W)
        else:
            self.op(eng, lambda e: e.tensor_scalar(out=out, in0=in0, scalar1=s1, scalar2=s2, op0=op0, op1=op1), R, W)

    def tt(self, eng, out, in0, in1, op, R, W):
        self.op(eng, lambda e: e.tensor_tensor(out=out, in0=in0, in1=in1, op=op), R, W)

    def stt(self, eng, out, in0, scalar, in1, op0, op1, R, W):
        self.op(eng, lambda e: e.scalar_tensor_tensor(out=out, in0=in0, scalar=scalar, in1=in1, op0=op0, op1=op1), R, W)

    def cp(self, eng, out, in_, R, W):
        if eng == "act":
            self.op("act", lambda e: e.copy(out=out, in_=in_), R, W)
        else:
            self.op(eng, lambda e: e.tensor_copy(out=out, in_=in_), R, W)

    def recip(self, out, in_, R, W):
        self.op("dve", lambda e: e.reciprocal(out=out, in_=in_), R, W)

    def memset(self, eng, ap, val, W):
        self.op(eng, lambda e: e.memset(ap, val), (), W)


CUT = 99

D = 1024
NH = 4
EPS = 1e-6
K_SCALE = 32 ** -0.5
ATT_SCALE = 192 ** -0.5
NE = 32
SENT = {}


def host_consts(L, C):
    TT = C + L
    f32 = np.float32
    cst = {}
    cst["ident"] = np.eye(128, dtype=f32)
    rows = L // 64
    row = np.repeat(np.arange(rows), 64).astype(f32)
    col = np.tile(np.arange(64), rows).astype(f32)
    freq = (f32(10000.0) ** (-np.arange(16, dtype=f32) / f32(16))).astype(f32)
    ang = np.concatenate([row[:, None] * freq, col[:, None] * freq], axis=-1).astype(f32)
    cos = np.ones((TT, 32), f32)
    sin = np.zeros((TT, 32), f32)
    cos[C:] = np.cos(ang)
    sin[C:] = np.sin(ang)
    cst["mc"] = np.ascontiguousarray(np.concatenate([cos, cos], 1).T)
    cst["ms"] = np.ascontiguousarray(np.concatenate([-sin, sin], 1).T)
    theta = (1.0 / (f32(10000.0) ** np.linspace(0.0, 1.0, 16, dtype=f32))).astype(f32)
    a2 = (np.arange(TT, dtype=f32)[:, None] * theta).astype(f32)
    cst["rcs"] = np.ascontiguousarray(np.concatenate([np.cos(a2), np.sin(a2)], 1).astype(f32))
    j = np.arange(128)[:, None]
    i = np.arange(128)[None, :]
    dm = np.zeros((128, 5, 128), f32)
    dm[:, 0] = np.maximum(i - j, 0)
    dm[:, 1] = np.maximum(j - i, 0)
    dm[:, 2] = (i > j)
    dm[:, 3] = (j > i)
    dm[:, 4] = 2.0 * (i == j)
    cst["dm"] = dm
    jj = np.arange(128, dtype=f32)
    cst["pcol"] = np.stack([127 - jj, jj, jj + 1, 128 - jj], 1).astype(f32)
    ii = np.arange(128, dtype=f32)
    cst["irow"] = np.ascontiguousarray(np.broadcast_to(np.stack([ii + 1, 128 - ii], 0)[None], (64, 2, 128)).astype(f32))
    return cst


def col(v, p=128):
    v = np.asarray(v)
    sh = v.shape
    return np.ascontiguousarray(v.reshape(sh[:-1] + (sh[-1] // p, p)).swapaxes(-1, -2))


def host_layout(inp, b, NL):
    m = {}
    m["x"] = np.ascontiguousarray(inp["x"][b])
    m["ctx"] = np.ascontiguousarray(inp["ctx"][b])
    cv = np.stack([inp["c"][b], inp["c_ctx"]], 0)
    m["ccol"] = np.ascontiguousarray(col(cv).transpose(1, 2, 0).reshape(128, 16))
    m["w_ada"] = inp["w_ada"]
    m["b_ada"] = inp["b_ada"]
    m["b_adac"] = col(inp["b_ada"])
    m["g1c"] = col(inp["g_norm1"])
    m["g2"] = inp["g_norm2"]
    m["w_in"] = inp["w_in"]
    m["w_out"] = inp["w_out"]
    cw = inp["conv_w"]
    m["convw"] = np.ascontiguousarray(cw.reshape(NL, 31, 2, 128).transpose(0, 3, 2, 1))
    m["convp"] = np.ascontiguousarray(np.stack([col(inp["conv_b"]), col(inp["conv_ln_g"]), col(inp["conv_ln_b"])], -1))
    m["qng"] = col(inp["mla_q_norm"])
    m["kvng"] = col(inp["mla_kv_norm"])
    m["w_uq"] = inp["mla_w_uq"]
    m["w_ukv"] = inp["mla_w_ukv"]
    dec = np.concatenate([inp["ret_decay_fwd"], inp["ret_decay_bwd"]], -1)
    m["dec"] = np.ascontiguousarray(dec)
    m["router_w"] = inp["router_w"]
    m["router_b"] = inp["router_b"]
    m["w1"] = inp["moe_w1"]
    m["w2"] = inp["moe_w2"]
    m["b1c"] = np.ascontiguousarray(col(inp["moe_b1"]).transpose(0, 2, 1, 3))
    m["b2"] = inp["moe_b2"]
    m["gf"] = inp["g_final"]
    return m


def build(L, C, NL=2, dbg=(), stop_after=None):
    TT = C + L
    NT = TT // 128
    nc = bass.Bass("TRN2", target_bir_lowering=False)
    s = Sched(nc)

    def din(name, shape):
        return nc.dram_tensor(name, list(shape), F32, kind="ExternalInput").ap()

    x_in = din("x", [L, D]); ctx_in = din("ctx", [C, D]); ccol = din("ccol", [128, 16])
    w_ada = din("w_ada", [NL, D, 6 * D]); b_ada = din("b_ada", [NL, 6 * D]); b_adac = din("b_adac", [NL, 128, 48])
    g1c = din("g1c", [NL, 128, 8]); g2 = din("g2", [NL, D])
    w_in = din("w_in", [NL, D, 1984]); w_out = din("w_out", [NL, D, D])
    convw = din("convw", [NL, 128, 2, 31]); convp = din("convp", [NL, 128, 2, 3])
    qng = din("qng", [NL, 128, 3]); kvng = din("kvng", [NL, 128, 2])
    w_uq = din("w_uq", [NL, 384, 768]); w_ukv = din("w_ukv", [NL, 256, 1024])
    dec = din("dec", [NL, 8])
    router_w = din("router_w", [NL, D, NE]); router_b = din("router_b", [NL, NE])
    w1 = din("w1", [NL, NE, D, 2048]); w2 = din("w2", [NL, NE, D, D])
    b1c = din("b1c", [NL, 128, NE, 16]); b2 = din("b2", [NL, NE, D]); gf = din("gf", [D])
    ident_d = din("ident", [128, 128]); mc_d = din("mc", [64, TT]); ms_d = din("ms", [64, TT])
    rcs_d = din("rcs", [TT, 32]); dm_d = din("dm", [128, 5, 128]); pcol_d = din("pcol", [128, 4]); irow_d = din("irow", [64, 2, 128])

    def scratch(name, shape, dt):
        kind = "ExternalOutput" if name in dbg else "Internal"
        return nc.dram_tensor(name, list(shape), dt, kind=kind).ap()

    y_out = nc.dram_tensor("y", [L, D], F32, kind="ExternalOutput").ap()
    XR = scratch("XR", [TT, D], F32)
    MOD = scratch("MOD", [NL, 2, 6 * D], F32)
    QN = scratch("QN", [NH, 128, TT], BF16); QR = scratch("QR", [NH, 64, TT], BF16)
    KN = scratch("KN", [NH, 128, TT], BF16); KR = scratch("KR", [64, TT], BF16)
    VV = scratch("VV", [TT, 512], BF16)
    RQT = scratch("RQT", [2, 64, TT], BF16); RKT = scratch("RKT", [2, 64, TT], BF16)
    RK = scratch("RK", [TT, 128], BF16); RV = scratch("RV", [TT, 256], BF16); SG = scratch("SG", [TT, 256], F32)
    MIXT = scratch("MIXT", [D, TT], BF16)
    H2T = scratch("H2T", [D, TT], BF16)
    GATE = scratch("GATE", [TT, NE], F32)
    ZTD = scratch("ZT", [128, 2, TT], BF16)
    W1B = scratch("W1B", [NL, NE, 4, 128, 4096], BF16)
    W2B = scratch("W2B", [NL, NE, 2, 128, 4096], BF16)

    groups = []
    t = 0
    while t < C:
        n = min(512, C - t); groups.append((t, n, 1)); t += n
    while t < TT:
        n = min(512, TT - t); groups.append((t, n, 0)); t += n

    ident_f = s.sb("ident_f", [128, 128], F32)
    ident_b = s.sb("ident_b", [128, 128], BF16)
    ones_b = s.sb("ones_b", [128, 128], BF16)
    s.dma("sp", ident_f[:], ident_d, writes=[ident_f])
    s.dma("pool", ident_b[:], ident_d, writes=[ident_b])
    s.memset("dve", ones_b[:], 1.0, [ones_b])
    modc = s.sb("modc", [128, NL, 48, 2], F32)
    a1c = s.sb("a1c", [128, NL, 2, 8], F32)
    b1c_ = s.sb("b1c_", [128, NL, 2, 8], F32)
    dtsum = s.sb("dtsum", [128, NL, NH, 128], BF16)
    kqdec = s.sb("kqdec", [128, NL, 4, NH], F32)
    cdec = s.sb("cdec", [64, NL, 2, 2], F32)

    with s.phase():
        cc = s.sb("cc", [128, 16], F32)
        scol = s.sb("scol", [128, 16], F32)
        s.dma("sp", cc[:], ccol, writes=[cc])
        s.act(scol[:], cc[:], AF.Sigmoid, [cc], [scol])
        s.tt("dve", scol[:], scol[:], cc[:], ALU.mult, [scol, cc], [scol])
        scv = scol[:].rearrange("p (k r) -> p k r", r=2)
        bac = s.sb("bac", [128, NL, 48], F32)
        for l in range(NL):
            s.dma("sp", bac[:, l, :], b_adac[l], writes=[bac])
        wts = s.sbpool("wada", [128, 8, 512], F32, 2)
        brow = s.sbpool("brow", [2, 512], F32, 2)
        mrow = s.sbpool("mrow", [2, 512], F32, 2)
        pr = s.pspool("pr", [2, 512], F32, 2)
        pc = s.pspool("pc", [128, 4, 2], F32, 2)
        for l in range(NL):
            for n in range(12):
                wt = wts.next()
                s.dma("sp" if n % 2 == 0 else "act", wt[:], w_ada[l][:, n * 512:(n + 1) * 512].rearrange("(k p) c -> p k c", p=128), writes=[wt])
                br = brow.next()
                s.dma("sp", br[:], b_ada[l:l + 1, n * 512:(n + 1) * 512].to_broadcast([2, 512]), writes=[br])
                p = pr.next()
                for k in range(8):
                    s.mm(p[:], scv[:, k, :], wt[:, k, :], k == 0, k == 7, [scol, wt], [p])
                mr = mrow.next()
                s.tt("dve", mr[:], p[:], br[:], ALU.add, [p, br], [mr])
                s.dma("sp", MOD[l][:, n * 512:(n + 1) * 512], mr[:], reads=[mr], writes=[s.db("MOD")])
                pcc = pc.next()
                for j in range(4):
                    for k in range(8):
                        s.mm(pcc[:, j, :], wt[:, k, j * 128:(j + 1) * 128], scv[:, k, :], k == 0, k == 7, [scol, wt], [pcc])
                s.tt("dve", modc[:, l, n * 4:(n + 1) * 4, :], pcc[:], bac[:, l, n * 4:(n + 1) * 4].unsqueeze(2).to_broadcast([128, 4, 2]), ALU.add, [pcc, bac], [modc])
            g1t = s.sb("g1t", [128, 8], F32)
            s.dma("sp", g1t[:], g1c[l], writes=[g1t])
            for r in range(2):
                s.ts("dve", a1c[:, l, r, :], modc[:, l, 8:16, r], 1.0, None, ALU.add, None, [modc], [a1c])
                s.tt("dve", a1c[:, l, r, :], a1c[:, l, r, :], g1t[:], ALU.mult, [a1c, g1t], [a1c])
                s.cp("dve", b1c_[:, l, r, :], modc[:, l, 0:8, r], [modc], [b1c_])
        dmt = s.sb("dmt", [128, 5, 128], F32)
        pcl = s.sb("pcl", [128, 4], F32)
        s.dma("sp", dmt[:], dm_d, writes=[dmt])
        s.dma("sp", pcl[:], pcol_d, writes=[pcl])
        for l in range(NL):
            dcb = s.sb("dcb", [128, 8], F32)
            s.dma("sp", dcb[:], dec[l:l + 1, :].to_broadcast([128, 8]), writes=[dcb])
            lg = s.sb("lg", [128, 8], F32)
            s.act(lg[:], dcb[:], AF.Exp, [dcb], [lg], scale=-1.0)
            s.act(lg[:], lg[:], AF.Ln, [lg], [lg], bias=1.0)
            s.ts("dve", lg[:], lg[:], -1.0, None, ALU.mult, None, [lg], [lg])
            for h in range(NH):
                ef = s.sb("ef", [128, 128], F32)
                eb = s.sb("eb", [128, 128], F32)
                s.act(ef[:], dmt[:, 0, :], AF.Exp, [dmt, lg], [ef], scale=lg[:, h:h + 1])
                s.act(eb[:], dmt[:, 1, :], AF.Exp, [dmt, lg], [eb], scale=lg[:, 4 + h:5 + h])
                s.tt("dve", ef[:], ef[:], dmt[:, 2, :], ALU.mult, [ef, dmt], [ef])
                s.tt("dve", eb[:], eb[:], dmt[:, 3, :], ALU.mult, [eb, dmt], [eb])
                s.tt("dve", ef[:], ef[:], eb[:], ALU.add, [ef, eb], [ef])
                s.tt("dve", dtsum[:, l, h, :], ef[:], dmt[:, 4, :], ALU.add, [ef, dmt], [dtsum])
            s.act(kqdec[:, l, 0, :], lg[:, 0:4], AF.Exp, [lg, pcl], [kqdec], scale=pcl[:, 0:1])
            s.act(kqdec[:, l, 1, :], lg[:, 4:8], AF.Exp, [lg, pcl], [kqdec], scale=pcl[:, 1:2])
            s.act(kqdec[:, l, 2, :], lg[:, 0:4], AF.Exp, [lg, pcl], [kqdec], scale=pcl[:, 2:3])
            s.act(kqdec[:, l, 3, :], lg[:, 4:8], AF.Exp, [lg, pcl], [kqdec], scale=pcl[:, 3:4])
            cd = s.sb("cd", [64, 2, 2], F32)
            for dr in range(2):
                for pp in range(2):
                    for hl in range(2):
                        h = 2 * pp + hl
                        s.cp("dve", cd[32 * hl:32 * hl + 32, dr, pp:pp + 1], lg[32 * hl:32 * hl + 32, dr * 4 + h:dr * 4 + h + 1], [lg], [cd])
            s.act(cdec[:, l, :, :], cd[:], AF.Exp, [cd], [cdec], scale=128.0)
    SENT["s"] = s
    if stop_after == 0:
        s.finish(); s.emit(); return nc

    for l in range(NL):
        with s.phase():
            win = s.sb("win", [128, 8, 1984], BF16)
            for k in range(8):
                s.dma("pool", win[:, k, :], w_in[l][k * 128:(k + 1) * 128, :], writes=[win])
            winsw = s.sb("winsw", [128, 8, 64], BF16)
            wv = w_in[l].rearrange("(k p) c -> p k c", p=128)
            s.dma("pool", winsw[:, :, 0:32], wv[:, :, 1568:1600], writes=[winsw])
            s.dma("pool", winsw[:, :, 32:64], wv[:, :, 1536:1568], writes=[winsw])
            wuq = s.sb("wuq", [128, 3, 768], BF16)
            s.dma("pool", wuq[:], w_uq[l].rearrange("(k p) c -> p k c", p=128), writes=[wuq])
            wuqsw = s.sb("wuqsw", [128, 3, NH, 64], BF16)
            wq4 = w_uq[l].rearrange("(k p) (h c) -> p k h c", p=128, h=NH)
            for h_ in range(NH):
                s.dma("pool", wuqsw[:, :, h_, 0:32], wq4[:, :, h_, 160:192], writes=[wuqsw])
                s.dma("pool", wuqsw[:, :, h_, 32:64], wq4[:, :, h_, 128:160], writes=[wuqsw])
            wukv = s.sb("wukv", [128, 2, 1024], BF16)
            s.dma("pool", wukv[:], w_ukv[l].rearrange("(k p) c -> p k c", p=128), writes=[wukv])
            wukv4 = wukv[:].rearrange("p k (h c) -> p k h c", h=NH)
            qg_t = s.sb("qg_t", [128, 3], F32); kvg_t = s.sb("kvg_t", [128, 2], F32)
            s.dma("sp", qg_t[:], qng[l], writes=[qg_t]); s.dma("sp", kvg_t[:], kvng[l], writes=[kvg_t])

            xts = s.sbpool("xt", [128, D], F32, 2)
            junk = s.sb("junk", [128, D], F32)
            sss = s.sbpool("ss", [128, 1], F32, 2)
            xns = s.sbpool("xn", [128, D], BF16, 2)
            pTs = s.pspool("pT", [128, 8, 128], BF16, 2)
            pms = s.pspool("pm", [128, 512], F32, 6)
            hTs = s.sbpool("hT", [128, 8, 512], BF16, 2)
            sgs = s.sbpool("sg", [128, 512], F32, 2)
            cqs = s.sb("cq", [128, 3, 512], F32)
            sqs = s.sb("sq", [128, 3, 512], BF16)
            cqn = s.sb("cqn", [128, 3, 512], BF16)
            rstd = s.sbpool("rstd", [128, 512], F32, 2)
            obs = s.sbpool("ob", [128, 512], BF16, 3)
            mct = s.sb("mct", [64, 512], F32); mst = s.sb("mst", [64, 512], F32)
            r1s = s.sbpool("r1", [64, 512], F32, 2); r2s = s.sbpool("r2", [64, 512], F32, 2)
            sqg = s.sbpool("sqg", [128, 384], F32, 2); skv = s.sbpool("skv", [128, 384], F32, 2)
            rcst = s.sbpool("rcst", [128, 32], F32, 2)
            tmpr = s.sbpool("tmpr", [128, 4, 4, 16], F32, 2)
            rot = s.sbpool("rot", [128, 2, 128], BF16, 2)
            rts = s.sbpool("rts", [64, 4, 128], BF16, 2)
            rvs = s.sbpool("rvs", [128, 256], BF16, 2)
            sgo = s.sbpool("sgo", [128, 256], F32, 2)
            zq = s.sbpool("zq", [128, 2, 512], BF16, 2)

            for (t0, n, r) in groups:
                nt = n // 128
                hT = hTs.next()
                for i in range(nt):
                    tok = t0 + i * 128
                    xt = xts.next()
                    if l == 0:
                        src = ctx_in[tok:tok + 128, :] if r == 1 else x_in[tok - C:tok - C + 128, :]
                        s.dma("sp", xt[:], src, writes=[xt])
                        s.dma("act", XR[tok:tok + 128, :], xt[:], reads=[xt], writes=[s.db("XR", tok // 128)])
                    else:
                        s.dma("sp", xt[:], XR[tok:tok + 128, :], reads=[s.db("XR", tok // 128)], writes=[xt])
                    ss = sss.next()
                    s.act(junk[:], xt[:], AF.Square, [xt], [junk, ss], accum_out=ss[:])
                    s.act(ss[:], ss[:], AF.Sqrt, [ss], [ss], scale=1.0 / D, bias=EPS)
                    s.recip(ss[:], ss[:], [ss], [ss])
                    xn = xns.next()
                    s.act(xn[:], xt[:], AF.Copy, [xt, ss], [xn], scale=ss[:])
                    pT = pTs.next()
                    for k in range(8):
                        s.tr(pT[:, k, :], xn[:, k * 128:(k + 1) * 128], ident_b[:], [xn, ident_b], [pT])
                    for k in range(8):
                        if k % 2 == 0:
                            s.ts("dve", hT[:, k, i * 128:(i + 1) * 128], pT[:, k, :], a1c[:, l, r, k:k + 1], b1c_[:, l, r, k:k + 1], ALU.mult, ALU.add, [pT, a1c, b1c_], [hT])
                        else:
                            s.act(hT[:, k, i * 128:(i + 1) * 128], pT[:, k, :], AF.Identity, [pT, a1c, b1c_], [hT], scale=a1c[:, l, r, k:k + 1], bias=b1c_[:, l, r, k:k + 1])

                def proj(cols, M):
                    p = pms.next()
                    for k in range(8):
                        s.mm(p[0:M, 0:n], cols(k), hT[:, k, 0:n], k == 0, k == 7, [hT, win, winsw], [p])
                    return p

                if CUT <= 1: continue
                z = zq.next()
                for c in range(2):
                    pa = proj(lambda k: win[:, k, c * 128:(c + 1) * 128], 128)
                    pg = proj(lambda k: win[:, k, 256 + c * 128:256 + (c + 1) * 128], 128)
                    sg = sgs.next()
                    s.act(sg[:, 0:n], pg[:, 0:n], AF.Sigmoid, [pg], [sg])
                    s.tt("dve", z[:, c, 0:n], pa[:, 0:n], sg[:, 0:n], ALU.mult, [pa, sg], [z])
                s.dma("sp", ZTD[:, :, t0:t0 + n], z[:, :, 0:n], reads=[z], writes=[s.db("ZT", t0)])

                if CUT <= 2: continue
                def featnorm(c0, nk, gt, outn, raw):
                    for k in range(nk):
                        p = proj(lambda kk: win[:, kk, c0 + k * 128:c0 + (k + 1) * 128], 128)
                        s.cp("act", raw[:, k, 0:n], p[:, 0:n], [p], [raw])
                        s.tt("pool", sqs[:, k, 0:n], raw[:, k, 0:n], raw[:, k, 0:n], ALU.mult, [raw], [sqs])
                    pss = pms.next()
                    for k in range(nk):
                        s.mm(pss[:, 0:n], ones_b[:], sqs[:, k, 0:n], k == 0, k == nk - 1, [ones_b, sqs], [pss])
                    rs = rstd.next()
                    s.act(rs[:, 0:n], pss[:, 0:n], AF.Sqrt, [pss], [rs], scale=1.0 / (nk * 128), bias=EPS)
                    s.recip(rs[:, 0:n], rs[:, 0:n], [rs], [rs])
                    for k in range(nk):
                        s.stt("dve", outn[:, k, 0:n], raw[:, k, 0:n], gt[:, k:k + 1], rs[:, 0:n], ALU.mult, ALU.mult, [raw, gt, rs], [outn])

                featnorm(512, 3, qg_t, cqn, cqs)
                s.dma("sp", mct[:, 0:n], mc_d[:, t0:t0 + n], writes=[mct])
                s.dma("sp", mst[:, 0:n], ms_d[:, t0:t0 + n], writes=[mst])

                def rope_fm(p_main, p_sw, dst, dkey):
                    r1 = r1s.next(); r2 = r2s.next()
                    s.tt("dve", r1[:, 0:n], p_main[0:64, 0:n], mct[:, 0:n], ALU.mult, [p_main, mct], [r1])
                    s.tt("dve", r2[:, 0:n], p_sw[0:64, 0:n], mst[:, 0:n], ALU.mult, [p_sw, mst], [r2])
                    ob = obs.next()
                    s.tt("pool", ob[0:64, 0:n], r1[:, 0:n], r2[:, 0:n], ALU.add, [r1, r2], [ob])
                    s.dma("sp", dst, ob[0:64, 0:n], reads=[ob], writes=[dkey])

                for h in range(NH):
                    p = pms.next()
                    for k in range(3):
                        s.mm(p[:, 0:n], wuq[:, k, h * 192:h * 192 + 128], cqn[:, k, 0:n], k == 0, k == 2, [wuq, cqn], [p])
                    ob = obs.next()
                    s.cp("act", ob[:, 0:n], p[:, 0:n], [p], [ob])
                    s.dma("sp", QN[h][:, t0:t0 + n], ob[:, 0:n], reads=[ob], writes=[s.db("QN", h, t0)])
                    p1 = pms.next(); p2 = pms.next()
                    for k in range(3):
                        s.mm(p1[0:64, 0:n], wuq[:, k, h * 192 + 128:h * 192 + 192], cqn[:, k, 0:n], k == 0, k == 2, [wuq, cqn], [p1])
                    for k in range(3):
                        s.mm(p2[0:64, 0:n], wuqsw[:, k, h, :], cqn[:, k, 0:n], k == 0, k == 2, [wuqsw, cqn], [p2])
                    rope_fm(p1, p2, QR[h][:, t0:t0 + n], s.db("QR", h, t0))

                if CUT <= 3: continue
                featnorm(1280, 2, kvg_t, cqn, cqs)
                for h in range(NH):
                    p = pms.next()
                    for k in range(2):
                        s.mm(p[:, 0:n], wukv4[:, k, h, 0:128], cqn[:, k, 0:n], k == 0, k == 1, [wukv, cqn], [p])
                    ob = obs.next()
                    s.cp("act", ob[:, 0:n], p[:, 0:n], [p], [ob])
                    s.dma("sp", KN[h][:, t0:t0 + n], ob[:, 0:n], reads=[ob], writes=[s.db("KN", h, t0)])
                for i in range(nt):
                    p = pms.next()
                    for h in range(NH):
                        for k in range(2):
                            s.mm(p[:, h * 128:(h + 1) * 128], cqn[:, k, i * 128:(i + 1) * 128], wukv4[:, k, h, 128:256], k == 0, k == 1, [wukv, cqn], [p])
                    ob = obs.next()
                    s.cp("act", ob[:], p[:], [p], [ob])
                    s.dma("sp", VV[t0 + i * 128:t0 + (i + 1) * 128, :], ob[:], reads=[ob], writes=[s.db("VV", (t0 + i * 128) // 128)])
                if CUT <= 4: continue
                p1 = proj(lambda k: win[:, k, 1536:1600], 64)
                p2 = proj(lambda k: winsw[:, k, :], 64)
                rope_fm(p1, p2, KR[:, t0:t0 + n], s.db("KR", t0))
                if CUT <= 5: continue
                for i in range(nt):
                    tok = t0 + i * 128
                    pq = pms.next(); pk = pms.next()
                    for k in range(8):
                        s.mm(pq[:, 0:384], hT[:, k, i * 128:(i + 1) * 128], win[:, k, 896:1280], k == 0, k == 7, [hT, win], [pq])
                    for k in range(8):
                        s.mm(pk[:, 0:384], hT[:, k, i * 128:(i + 1) * 128], win[:, k, 1600:1984], k == 0, k == 7, [hT, win], [pk])
                    a = sqg.next(); b = skv.next()
                    s.cp("act", a[:, 0:128], pq[:, 0:128], [pq], [a])
                    s.act(b[:, 0:128], pk[:, 0:128], AF.Identity, [pk], [b], scale=K_SCALE)
                    so = sgo.next()
                    s.act(so[:], pq[:, 128:384], AF.Sigmoid, [pq], [so])
                    s.tt("dve", so[:], so[:], pq[:, 128:384], ALU.mult, [so, pq], [so])
                    s.dma("act", SG[tok:tok + 128, :], so[:], reads=[so], writes=[s.db("SG", tok // 128)])
                    rv_ = rvs.next()
                    s.cp("dve", rv_[:], pk[:, 128:384], [pk], [rv_])
                    s.dma("act", RV[tok:tok + 128, :], rv_[:], reads=[rv_], writes=[s.db("RV", tok // 128)])
                    if CUT <= 6: continue
                    cs = rcst.next()
                    s.dma("act", cs[:], rcs_d[tok:tok + 128, :], writes=[cs])
                    cosb = cs[:, 0:16].unsqueeze(1).to_broadcast([128, 4, 16])
                    sinb = cs[:, 16:32].unsqueeze(1).to_broadcast([128, 4, 16])
                    ro = rot.next()
                    tm = tmpr.next()
                    for qi, srcT in enumerate((a, b)):
                        v4 = srcT[:, 0:128].rearrange("p (h two d) -> p h two d", h=4, two=2)
                        o4 = ro[:, qi, :].rearrange("p (h two d) -> p h two d", h=4, two=2)
                        x1 = v4[:, :, 0, :]; x2 = v4[:, :, 1, :]
                        s.tt("dve", tm[:, :, 0, :], x1, cosb, ALU.mult, [srcT, cs], [tm])
                        s.tt("dve", tm[:, :, 1, :], x2, sinb, ALU.mult, [srcT, cs], [tm])
                        s.tt("dve", tm[:, :, 2, :], x2, cosb, ALU.mult, [srcT, cs], [tm])
                        s.tt("dve", tm[:, :, 3, :], x1, sinb, ALU.mult, [srcT, cs], [tm])
                        s.tt("dve", o4[:, :, 0, :], tm[:, :, 0, :], tm[:, :, 1, :], ALU.subtract, [tm], [ro])
                        s.tt("dve", o4[:, :, 1, :], tm[:, :, 2, :], tm[:, :, 3, :], ALU.add, [tm], [ro])
                    if CUT <= 7: continue
                    s.dma("sp", RK[tok:tok + 128, :], ro[:, 1, :], reads=[ro], writes=[s.db("RK", tok // 128)])
                    pT = pTs.next()
                    for qi in range(2):
                        for pp in range(2):
                            s.tr(pT[0:64, qi * 2 + pp, :], ro[:, qi, pp * 64:(pp + 1) * 64], ident_b[:], [ro, ident_b], [pT])
                    rt = rts.next()
                    s.cp("dve", rt[:], pT[0:64, 0:4, :], [pT], [rt])
                    s.dma("sp", RQT[:, :, tok:tok + 128].rearrange("a p t -> p a t"), rt[:, 0:2, :], reads=[rt], writes=[s.db("RQT", tok // 128)])
                    s.dma("sp", RKT[:, :, tok:tok + 128].rearrange("a p t -> p a t"), rt[:, 2:4, :], reads=[rt], writes=[s.db("RKT", tok // 128)])
        if stop_after == "A":
            break
        with s.phase():
            cwt = s.sb("cwt", [128, 2, 31], F32); cpt = s.sb("cpt", [128, 2, 3], F32)
            s.dma("sp", cwt[:], convw[l], writes=[cwt]); s.dma("sp", cpt[:], convp[l], writes=[cpt])
            zts = s.sbpool("zt", [128, 2, 512 + 30], BF16, 2)
            accs = s.sbpool("acc", [128, 2, 512], F32, 2)
            ybs = s.sbpool("yb", [128, 2, 512], BF16, 2)
            yqs = s.sbpool("yq", [128, 2, 512], BF16, 2)
            pS = s.pspool("pS", [128, 512], F32, 2); pQ = s.pspool("pQ", [128, 512], F32, 2)
            mean = s.sbpool("mean", [128, 512], F32, 2); var = s.sbpool("var", [128, 512], F32, 2)
            msq = s.sbpool("msq", [128, 512], F32, 2)
            dd = s.sbpool("dd", [128, 512], F32, 2)
            oc = s.sbpool("oc", [128, 512], BF16, 3)
            for (t0, n, r) in groups:
                lo, hi = (0, C) if r == 1 else (C, TT)
                zt = zts.next()
                s.memset("pool", zt[:], 0.0, [zt])
                a0 = max(lo, t0 - 15); a1 = min(hi, t0 + n + 15)
                s.dma("sp", zt[:, :, a0 - (t0 - 15):a1 - (t0 - 15)], ZTD[:, :, a0:a1], reads=[s.db("ZT", g[0]) for g in groups], writes=[zt])
                acc = accs.next(); yb = ybs.next(); yq = yqs.next()
                for c in range(2):
                    eng = "dve"
                    s.ts(eng, acc[:, c, 0:n], zt[:, c, 0:n], cwt[:, c, 0:1], cpt[:, c, 0:1], ALU.mult, ALU.add, [zt, cwt, cpt], [acc])
                    for k in range(1, 31):
                        s.stt(eng, acc[:, c, 0:n], zt[:, c, k:k + n], cwt[:, c, k:k + 1], acc[:, c, 0:n], ALU.mult, ALU.add, [zt, cwt, acc], [acc])
                    s.cp("act", yb[:, c, 0:n], acc[:, c, 0:n], [acc], [yb])
                    s.act(yq[:, c, 0:n], acc[:, c, 0:n], AF.Square, [acc], [yq])
                ps_ = pS.next(); pq_ = pQ.next()
                for c in range(2):
                    s.mm(ps_[:, 0:n], ones_b[:], yb[:, c, 0:n], c == 0, c == 1, [ones_b, yb], [ps_])
                for c in range(2):
                    s.mm(pq_[:, 0:n], ones_b[:], yq[:, c, 0:n], c == 0, c == 1, [ones_b, yq], [pq_])
                mn = mean.next(); vr = var.next(); mq = msq.next()
                s.ts("dve", mn[:, 0:n], ps_[:, 0:n], 1.0 / 256, None, ALU.mult, None, [ps_], [mn])
                s.tt("pool", mq[:, 0:n], mn[:, 0:n], mn[:, 0:n], ALU.mult, [mn], [mq])
                s.stt("dve", vr[:, 0:n], pq_[:, 0:n], 1.0 / 256, mq[:, 0:n], ALU.mult, ALU.subtract, [pq_, mq], [vr])
                s.ts("dve", vr[:, 0:n], vr[:, 0:n], 0.0, None, ALU.max, None, [vr], [vr])
                s.act(vr[:, 0:n], vr[:, 0:n], AF.Sqrt, [vr], [vr], bias=1e-5)
                s.recip(vr[:, 0:n], vr[:, 0:n], [vr], [vr])
                for c in range(2):
                    d_ = dd.next()
                    s.tt("pool", d_[:, 0:n], acc[:, c, 0:n], mn[:, 0:n], ALU.subtract, [acc, mn], [d_])
                    s.tt("pool", d_[:, 0:n], d_[:, 0:n], vr[:, 0:n], ALU.mult, [d_, vr], [d_])
                    o = oc.next()
                    s.act(d_[:, 0:n], d_[:, 0:n], AF.Identity, [d_, cpt], [d_], scale=cpt[:, c, 1:2], bias=cpt[:, c, 2:3])
                    sg2 = msq.next()
                    s.act(sg2[:, 0:n], d_[:, 0:n], AF.Sigmoid, [d_], [sg2])
                    s.tt("dve", o[:, 0:n], d_[:, 0:n], sg2[:, 0:n], ALU.mult, [d_, sg2], [o])
                    s.dma("sp", MIXT[c * 128:(c + 1) * 128, t0:t0 + n], o[:, 0:n], reads=[o], writes=[s.db("MIXT", c, t0)])
        with s.phase():
            NCH = NT
            Sst = s.sb("Sst", [64, 2, 2, NCH, 64], BF16)
            Sf = [[s.sb("Sf", [64, 128], F32) for pp in range(2)] for dr in range(2)]
            for dr in range(2):
                for pp in range(2):
                    s.memset("dve", Sf[dr][pp][:], 0.0, [Sf[dr][pp]])
            qd = s.sb("qd", [64, 2, 2, 128], F32)
            irow = s.sb("irow", [64, 2, 128], F32)
            s.dma("sp", irow[:], irow_d, writes=[irow])
            cdl = s.sb("cdl", [64, 2, 2], F32)
            s.act(cdl[:], cdec[:, l, :, :], AF.Ln, [cdec], [cdl], scale=1.0)
            s.ts("dve", cdl[:], cdl[:], 1.0 / 128, None, ALU.mult, None, [cdl], [cdl])
            for dr in range(2):
                for pp in range(2):
                    s.act(qd[:, dr, pp, :], irow[:, dr, :], AF.Exp, [irow, cdl], [qd], scale=cdl[:, dr, pp:pp + 1])
            rkt = s.sbpool("rkt", [128, 128], BF16, 3); rvt = s.sbpool("rvt", [128, 256], BF16, 3)
            kds = s.sbpool("kd", [128, 128], BF16, 3)
            pkv = s.pspool("pkv", [64, 128], F32, 2)
            nchc = C // 128
            order_f = list(range(NCH))
            order_b = list(range(nchc - 1, -1, -1)) + list(range(NCH - 1, nchc - 1, -1))
            for dr, order in ((0, order_f), (1, order_b)):
                for ci in order:
                    tok = ci * 128
                    rk_ = rkt.next(); rv_ = rvt.next()
                    s.dma("sp", rk_[:], RK[tok:tok + 128, :], reads=[s.db("RK", ci)], writes=[rk_])
                    s.dma("act", rv_[:], RV[tok:tok + 128, :], reads=[s.db("RV", ci)], writes=[rv_])
                    kd = kds.next()
                    s.tt("pool", kd[:].rearrange("p (h d) -> p h d", h=4), rk_[:].rearrange("p (h d) -> p h d", h=4),
                         kqdec[:, l, dr, :].unsqueeze(2).to_broadcast([128, 4, 32]), ALU.mult, [rk_, kqdec], [kd])
                    for pp in range(2):
                        S_ = Sf[dr][pp]
                        s.cp("act", Sst[0:32, dr, pp, ci, :], S_[0:32, 0:64], [S_], [Sst])
                        s.cp("act", Sst[32:64, dr, pp, ci, :], S_[32:64, 64:128], [S_], [Sst])
                        p = pkv.next()
                        s.mm(p[:], kd[:, pp * 64:(pp + 1) * 64], rv_[:, pp * 128:(pp + 1) * 128], True, True, [kd, rv_], [p])
                        s.stt("dve", S_[:], S_[:], cdec[:, l, dr, pp:pp + 1], p[:], ALU.mult, ALU.add, [S_, cdec, p], [S_])
            qts = s.sbpool("qt", [64, 2, 128], BF16, 2); kts = s.sbpool("kt", [64, 2, 128], BF16, 2)
            qfs = s.sbpool("qf", [64, 2, 2, 128], BF16, 2)
            sgt = s.sbpool("sgt", [128, 256], F32, 2)
            pst = s.pspool("pst", [128, 128], F32, 2)
            po = s.pspool("po", [128, 256], F32, 2)
            pps = s.sbpool("pp", [128, 128], BF16, 3)
            osb = s.sbpool("osb", [128, 256], F32, 2); osq = s.sbpool("osq", [128, 256], F32, 2)
            ssm = s.sbpool("ssm", [128, 4], F32, 2)
            yrb = s.sbpool("yrb", [128, 256], BF16, 2)
            pTr = s.pspool("pTr", [128, 2, 128], BF16, 2)
            yrt = s.sbpool("yrt", [128, 2, 128], BF16, 2)
            for ci in range(NCH):
                tok = ci * 128
                qt = qts.next(); kt = kts.next(); rv_ = rvt.next(); sg_ = sgt.next()
                s.dma("sp", qt[:], RQT[:, :, tok:tok + 128].rearrange("a p t -> p a t"), reads=[s.db("RQT", ci)], writes=[qt])
                s.dma("sp", kt[:], RKT[:, :, tok:tok + 128].rearrange("a p t -> p a t"), reads=[s.db("RKT", ci)], writes=[kt])
                s.dma("act", rv_[:], RV[tok:tok + 128, :], reads=[s.db("RV", ci)], writes=[rv_])
                s.dma("act", sg_[:], SG[tok:tok + 128, :], reads=[s.db("SG", ci)], writes=[sg_])
                qf = qfs.next()
                for dr in range(2):
                    s.tt("pool", qf[:, dr, :, :], qt[:], qd[:, dr, :, :], ALU.mult, [qt, qd], [qf])
                o = po.next()
                for h in range(NH):
                    pp = h // 2; b0 = 32 * (h % 2)
                    st = pst.next()
                    s.mm(st[:], kt[b0:b0 + 32, pp, :], qt[b0:b0 + 32, pp, :], True, True, [kt, qt], [st])
                    P = pps.next()
                    s.tt("dve", P[:], st[:], dtsum[:, l, h, :], ALU.mult, [st, dtsum], [P])
                    s.mm(o[:, h * 64:(h + 1) * 64], P[:], rv_[:, h * 64:(h + 1) * 64], True, False, [P, rv_], [o])
                    s.mm(o[:, h * 64:(h + 1) * 64], qf[b0:b0 + 32, 0, pp, :], Sst[b0:b0 + 32, 0, pp, ci, :], False, False, [qf, Sst], [o])
                    s.mm(o[:, h * 64:(h + 1) * 64], qf[b0:b0 + 32, 1, pp, :], Sst[b0:b0 + 32, 1, pp, ci, :], False, True, [qf, Sst], [o])
                ob = osb.next(); oq = osq.next(); sm = ssm.next()
                s.cp("act", ob[:], o[:], [o], [ob])
                s.tt("pool", oq[:], ob[:], ob[:], ALU.mult, [ob], [oq])
                s.op("dve", lambda e, sm=sm, oq=oq: e.reduce_sum(out=sm[:], in_=oq[:].rearrange("p (h e) -> p h e", h=4), axis=AX.X), [oq], [sm])
                s.act(sm[:], sm[:], AF.Sqrt, [sm], [sm], scale=1.0 / 64, bias=EPS)
                s.recip(sm[:], sm[:], [sm], [sm])
                s.tt("pool", ob[:].rearrange("p (h e) -> p h e", h=4), ob[:].rearrange("p (h e) -> p h e", h=4), sm[:].unsqueeze(2).to_broadcast([128, 4, 64]), ALU.mult, [ob, sm], [ob])
                yb = yrb.next()
                s.tt("pool", yb[:], ob[:], sg_[:], ALU.mult, [ob, sg_], [yb])
                pt = pTr.next()
                for c in range(2):
                    s.tr(pt[:, c, :], yb[:, c * 128:(c + 1) * 128], ident_b[:], [yb, ident_b], [pt])
                yt = yrt.next()
                s.cp("dve", yt[:], pt[:], [pt], [yt])
                s.dma("sp", MIXT[768:1024, tok:tok + 128].rearrange("(c p) t -> p c t", p=128), yt[:], reads=[yt], writes=[s.db("MIXT", "r", ci)])
        with s.phase():
            knt = s.sb("knt", [128, TT], BF16); krt = s.sb("krt", [64, TT], BF16); vt = s.sb("vt", [128, NT, 128], BF16)
            s.dma("sp", krt[:], KR, reads=[s.db("KR", g[0]) for g in groups], writes=[krt])
            qnt = s.sbpool("qnt", [128, 512], BF16, 2); qrt = s.sbpool("qrt", [64, 512], BF16, 2)
            pst = s.pspool("pst", [128, 512], F32, 3)
            pacc = s.pspool("pacc", [128, 512], F32, 2); pden = s.pspool("pden", [128, 512], F32, 2)
            Ps = s.sbpool("P", [128, 512], BF16, 3)
            rec = s.sbpool("rec", [128, 512], F32, 2)
            oat = s.sbpool("oat", [128, 512], BF16, 2)
            for h in range(NH):
                s.dma("sp", knt[:], KN[h], reads=[s.db("KN", h, g[0]) for g in groups], writes=[knt])
                s.dma("act", vt[:], VV[:, h * 128:(h + 1) * 128].rearrange("(t p) c -> p t c", p=128), reads=[s.db("VV", i) for i in range(NT)], writes=[vt])
                for (t0, n, r) in groups:
                    qn = qnt.next(); qr = qrt.next()
                    s.dma("sp", qn[:, 0:n], QN[h][:, t0:t0 + n], reads=[s.db("QN", h, t0)], writes=[qn])
                    s.dma("act", qr[:, 0:n], QR[h][:, t0:t0 + n], reads=[s.db("QR", h, t0)], writes=[qr])
                    nk = C // 128 if r == 1 else NT
                    acc = pacc.next(); den = pden.next()
                    for jt in range(nk):
                        st = pst.next()
                        s.mm(st[:, 0:n], knt[:, jt * 128:(jt + 1) * 128], qn[:, 0:n], True, False, [knt, qn], [st])
                        s.mm(st[:, 0:n], krt[:, jt * 128:(jt + 1) * 128], qr[:, 0:n], False, True, [krt, qr], [st])
                        P = Ps.next()
                        s.act(P[:, 0:n], st[:, 0:n], AF.Exp, [st], [P], scale=ATT_SCALE)
                        s.mm(acc[:, 0:n], vt[:, jt, :], P[:, 0:n], jt == 0, jt == nk - 1, [vt, P], [acc])
                        s.mm(den[:, 0:n], ones_b[:], P[:, 0:n], jt == 0, jt == nk - 1, [ones_b, P], [den])
                    rc = rec.next()
                    s.recip(rc[:, 0:n], den[:, 0:n], [den], [rc])
                    oa = oat.next()
                    s.tt("dve", oa[:, 0:n], acc[:, 0:n], rc[:, 0:n], ALU.mult, [acc, rc], [oa])
                    s.dma("sp", MIXT[256 + h * 128:256 + (h + 1) * 128, t0:t0 + n], oa[:, 0:n], reads=[oa], writes=[s.db("MIXT", "a", h, t0)])
        if stop_after == "D":
            break
        with s.phase():
            wo = s.sb("wo", [128, 8, D], BF16)
            for k in range(8):
                s.dma("pool", wo[:, k, :], w_out[l][k * 128:(k + 1) * 128, :], writes=[wo])
            rw = s.sb("rw", [128, 8, NE], F32)
            s.dma("sp", rw[:], router_w[l].rearrange("(k p) e -> p k e", p=128), writes=[rw])
            rb = s.sb("rb", [128, NE], F32)
            s.dma("sp", rb[:], router_b[l:l + 1, :].to_broadcast([128, NE]), writes=[rb])
            g2b = s.sb("g2b", [128, D], F32)
            s.dma("sp", g2b[:], g2[l:l + 1, :].to_broadcast([128, D]), writes=[g2b])
            GT1 = []; A2 = []; B2 = []
            for r in range(2):
                gt = s.sb("gt1", [128, D], F32); a2 = s.sb("a2", [128, D], F32); b2_ = s.sb("b2_", [128, D], F32)
                s.dma("sp", gt[:], MOD[l][r:r + 1, 2 * D:3 * D].to_broadcast([128, D]), reads=[s.db("MOD")], writes=[gt])
                s.dma("sp", b2_[:], MOD[l][r:r + 1, 3 * D:4 * D].to_broadcast([128, D]), reads=[s.db("MOD")], writes=[b2_])
                s.dma("sp", a2[:], MOD[l][r:r + 1, 4 * D:5 * D].to_broadcast([128, D]), reads=[s.db("MOD")], writes=[a2])
                s.stt("dve", a2[:], a2[:], 1.0, g2b[:], ALU.add, ALU.mult, [a2, g2b], [a2])
                GT1.append(gt); A2.append(a2); B2.append(b2_)
            mts = s.sbpool("mt", [128, 8, 128], BF16, 2)
            xts = s.sbpool("xt", [128, D], F32, 2)
            pmx = s.pspool("pmx", [128, D], F32, 1)
            tmps = s.sbpool("tmp", [128, D], F32, 2)
            junk = s.sb("junk", [128, D], F32)
            sss = s.sbpool("ss", [128, 1], F32, 2)
            h2s = s.sbpool("h2", [128, D], F32, 2)
            pT2 = s.pspool("pT2", [128, 8, 128], BF16, 1); pT3 = s.pspool("pT3", [128, 8, 128], BF16, 1)
            h2b = s.sbpool("h2b", [128, 16, 128], BF16, 2)
            hhis = s.sbpool("hhi", [128, D], BF16, 2); hlos = s.sbpool("hlo", [128, D], BF16, 2)
            rwh = s.sb("rwh", [128, 8, NE], BF16); rwl = s.sb("rwl", [128, 8, NE], BF16)
            s.cp("dve", rwh[:], rw[:], [rw], [rwh])
            s.tt("dve", rw[:], rw[:], rwh[:], ALU.subtract, [rw, rwh], [rw])
            s.cp("dve", rwl[:], rw[:], [rw], [rwl])
            plg = s.pspool("plg", [128, NE], F32, 2)
            lgs = s.sbpool("lgs", [128, NE], F32, 2); t8 = s.sbpool("t8", [128, 8], F32, 2)
            msk = s.sbpool("msk", [128, NE], F32, 2); ex = s.sbpool("ex", [128, NE], F32, 2)
            nm = s.sbpool("nm", [128, 1], F32, 2); sm1 = s.sbpool("sm1", [128, 1], F32, 2)
            for ti in range(NT):
                tok = ti * 128
                r = 1 if tok < C else 0
                if r == 1 and l == NL - 1:
                    continue
                mt = mts.next()
                s.dma("sp", mt[:], MIXT[:, tok:tok + 128].rearrange("(k p) t -> p k t", p=128), reads=[s.db(*k_) for k_ in list(s.dbufs) if k_[0] == "MIXT"], writes=[mt])
                xt = xts.next()
                s.dma("act", xt[:], XR[tok:tok + 128, :], reads=[s.db("XR", ti)], writes=[xt])
                pm = pmx.next()
                for hf in range(2):
                    for k in range(8):
                        s.mm(pm[:, hf * 512:(hf + 1) * 512], mt[:, k, :], wo[:, k, hf * 512:(hf + 1) * 512], k == 0, k == 7, [mt, wo], [pm])
                tp = tmps.next()
                for hf in range(2):
                    s.tt("dve", tp[:, hf * 512:(hf + 1) * 512], pm[:, hf * 512:(hf + 1) * 512], GT1[r][:, hf * 512:(hf + 1) * 512], ALU.mult, [pm, GT1[r]], [tp])
                s.tt("pool", xt[:], xt[:], tp[:], ALU.add, [xt, tp], [xt])
                s.dma("sp", XR[tok:tok + 128, :], xt[:], reads=[xt], writes=[s.db("XR", ti)])
                if CUT == 11: continue
                ss = sss.next()
                s.act(junk[:], xt[:], AF.Square, [xt], [junk, ss], accum_out=ss[:])
                s.act(ss[:], ss[:], AF.Sqrt, [ss], [ss], scale=1.0 / D, bias=EPS)
                s.recip(ss[:], ss[:], [ss], [ss])
                h2 = h2s.next()
                s.stt("dve", h2[:], xt[:], ss[:, 0:1], A2[r][:], ALU.mult, ALU.mult, [xt, ss, A2[r]], [h2])
                s.tt("pool", h2[:], h2[:], B2[r][:], ALU.add, [h2, B2[r]], [h2])
                if CUT == 12: continue
                hhi = hhis.next(); hlo = hlos.next()
                s.cp("act", hhi[:], h2[:], [h2], [hhi])
                s.tt("pool", h2[:], h2[:], hhi[:], ALU.subtract, [h2, hhi], [h2])
                s.cp("pool", hlo[:], h2[:], [h2], [hlo])
                if CUT == 13: continue
                pt = pT2.next(); ptl = pT3.next()
                for k in range(8):
                    s.tr(pt[:, k, :], hhi[:, k * 128:(k + 1) * 128], ident_b[:], [hhi, ident_b], [pt])
                for k in range(8):
                    s.tr(ptl[:, k, :], hlo[:, k * 128:(k + 1) * 128], ident_b[:], [hlo, ident_b], [ptl])
                hb_ = h2b.next()
                s.cp("dve", hb_[:, 0:8, :], pt[:], [pt], [hb_])
                s.cp("dve", hb_[:, 8:16, :], ptl[:], [ptl], [hb_])
                for k in range(8):
                    s.dma("sp" if k % 2 == 0 else "act", H2T[k * 128:(k + 1) * 128, tok:tok + 128], hb_[:, k, :], reads=[hb_], writes=[s.db("H2T", ti)])
                if CUT == 8: continue
                pl_ = plg.next()
                for k in range(8):
                    s.mm(pl_[:], hb_[:, k, :], rwh[:, k, :], k == 0, False, [hb_, rwh], [pl_])
                    s.mm(pl_[:], hb_[:, 8 + k, :], rwh[:, k, :], False, False, [hb_, rwh], [pl_])
                    s.mm(pl_[:], hb_[:, k, :], rwl[:, k, :], False, k == 7, [hb_, rwl], [pl_])
                lg_ = lgs.next()
                s.tt("dve", lg_[:], pl_[:], rb[:], ALU.add, [pl_, rb], [lg_])
                if CUT == 9: continue
                t8_ = t8.next()
                s.op("dve", lambda e, t8_=t8_, lg_=lg_: e.max(out=t8_[:], in_=lg_[:]), [lg_], [t8_])
                mk_ = msk.next()
                s.ts("dve", mk_[:], lg_[:], t8_[:, 3:4], None, ALU.is_ge, None, [lg_, t8_], [mk_])
                nm_ = nm.next()
                s.ts("dve", nm_[:], t8_[:, 0:1], -1.0, None, ALU.mult, None, [t8_], [nm_])
                ex_ = ex.next()
                s.act(ex_[:], lg_[:], AF.Exp, [lg_, nm_], [ex_], bias=nm_[:, 0:1])
                s.tt("dve", ex_[:], ex_[:], mk_[:], ALU.mult, [ex_, mk_], [ex_])
                sm_ = sm1.next()
                s.op("dve", lambda e, sm_=sm_, ex_=ex_: e.reduce_sum(out=sm_[:], in_=ex_[:], axis=AX.X), [ex_], [sm_])
                s.recip(sm_[:], sm_[:], [sm_], [sm_])
                s.ts("dve", ex_[:], ex_[:], sm_[:, 0:1], None, ALU.mult, None, [ex_, sm_], [ex_])
                s.dma("sp", GATE[tok:tok + 128, :], ex_[:], reads=[ex_], writes=[s.db("GATE", ti)])
        if stop_after == "E":
            break
        with s.phase():
            b1t = s.sb("b1t", [128, NE, 16], F32)
            s.dma("sp", b1t[:], b1c[l], writes=[b1t])
            GT2 = []
            for r in range(2):
                gt = s.sb("gt2", [128, D], F32)
                s.dma("sp", gt[:], MOD[l][r:r + 1, 5 * D:6 * D].to_broadcast([128, D]), reads=[s.db("MOD")], writes=[gt])
                GT2.append(gt)
            wps = s.sbpool("wp", [128, 8, 512], BF16, 8)
            b2s = s.sbpool("b2s", [128, D], F32, 2)
            hts = s.sb("hts", [128, 8, 1024], BF16)
            yacc = s.sb("yacc", [128, 8, D], F32)
            gts = s.sb("gts", [128, 8, NE], F32)
            actt = s.sb("actt", [128, 8, 1024], BF16)
            gq = s.sbpool("gq", [128, 512], F32, 2); sq_ = s.sbpool("sq_", [128, 512], F32, 2)
            lq = s.sbpool("lq", [128, 512], F32, 2); tq = s.sbpool("tq", [128, 512], F32, 2)
            tmo = s.sbpool("tmo", [128, 512], F32, 2)
            xts = s.sbpool("xt", [128, D], F32, 2)
            pg_ = s.pspool("pg", [128, 512], F32, 2); pl_ = s.pspool("pl", [128, 512], F32, 2); po_ = s.pspool("po", [128, 512], F32, 3)
            sgs_ = []
            ti = 0
            while ti < NT:
                nt_ = (C // 128) if ti < C // 128 else min(8, NT - ti)
                nt_ = min(nt_, 8)
                sgs_.append((ti, nt_)); ti += nt_
            for e_ in range(NE):
                for q in range(4):
                    wp = wps.next()
                    s.dma("pool", wp[:], w1[l][e_][:, q * 512:(q + 1) * 512].rearrange("(k p) c -> p k c", p=128), writes=[wp])
                    s.dma("sp" if q % 2 == 0 else "act", W1B[l][e_][q], wp[:].rearrange("p k c -> p (k c)"), reads=[wp], writes=[s.db("W1B", e_, q)])
                for q in range(2):
                    wp = wps.next()
                    s.dma("pool", wp[:], w2[l][e_][:, q * 512:(q + 1) * 512].rearrange("(k p) c -> p k c", p=128), writes=[wp])
                    s.dma("sp" if q % 2 == 0 else "act", W2B[l][e_][q], wp[:].rearrange("p k c -> p (k c)"), reads=[wp], writes=[s.db("W2B", e_, q)])
            for (ti0, ntl) in sgs_:
                S_ = ntl * 128; tok0 = ti0 * 128
                r = 1 if tok0 < C else 0
                if r == 1 and l == NL - 1:
                    continue
                s.dma("sp", hts[:, :, 0:S_], H2T[:, tok0:tok0 + S_].rearrange("(k p) t -> p k t", p=128), reads=[s.db("H2T", ti0 + i) for i in range(ntl)], writes=[hts])
                s.dma("act", gts[:, 0:ntl, :], GATE[tok0:tok0 + S_, :].rearrange("(i p) e -> p i e", p=128), reads=[s.db("GATE", ti0 + i) for i in range(ntl)], writes=[gts])
                s.memset("pool", yacc[:], 0.0, [yacc])
                ngs = [(a, min(512, S_ - a)) for a in range(0, S_, 512)]
                for e_ in range(NE):
                    pieces = {}
                    for q in (0, 2, 1, 3):
                        wp = wps.next()
                        s.dma("sp", wp[:].rearrange("p k c -> p (k c)"), W1B[l][e_][q], reads=[s.db("W1B", e_, q)], writes=[wp])
                        pieces[q] = wp
                    w2p = []
                    for q in range(2):
                        wp = wps.next()
                        s.dma("sp", wp[:].rearrange("p k c -> p (k c)"), W2B[l][e_][q], reads=[s.db("W2B", e_, q)], writes=[wp])
                        w2p.append(wp)
                    b2t = b2s.next()
                    s.dma("sp", b2t[:], b2[l][e_:e_ + 1, :].to_broadcast([128, D]), writes=[b2t])
                    for c in range(8):
                        wg = pieces[c // 4]; wl = pieces[2 + c // 4]; cc_ = c % 4
                        for (a, nn) in ngs:
                            pg = pg_.next(); pl = pl_.next()
                            for k in range(8):
                                s.mm(pg[:, 0:nn], wg[:, k, cc_ * 128:(cc_ + 1) * 128], hts[:, k, a:a + nn], k == 0, k == 7, [wg, hts], [pg])
                            for k in range(8):
                                s.mm(pl[:, 0:nn], wl[:, k, cc_ * 128:(cc_ + 1) * 128], hts[:, k, a:a + nn], k == 0, k == 7, [wl, hts], [pl])
                            g_ = gq.next(); sg_ = sq_.next(); l_ = lq.next(); t_ = tq.next()
                            s.ts("dve", g_[:, 0:nn], pg[:, 0:nn], b1t[:, e_, c:c + 1], 7.0, ALU.add, ALU.min, [pg, b1t], [g_])
                            s.act(sg_[:, 0:nn], g_[:, 0:nn], AF.Sigmoid, [g_], [sg_], scale=1.702)
                            s.act(l_[:, 0:nn], pl[:, 0:nn], AF.Identity, [pl, b1t], [l_], bias=b1t[:, e_, 8 + c:9 + c])
                            s.ts("pool", l_[:, 0:nn], l_[:, 0:nn], 7.0, -7.0, ALU.min, ALU.max, [l_], [l_])
                            s.tt("pool", t_[:, 0:nn], g_[:, 0:nn], sg_[:, 0:nn], ALU.mult, [g_, sg_], [t_])
                            s.stt("dve", actt[:, c, a:a + nn], l_[:, 0:nn], 1.0, t_[:, 0:nn], ALU.add, ALU.mult, [l_, t_], [actt])
                    for i in range(ntl):
                        for hf in range(2):
                            po = po_.next()
                            for k in range(8):
                                s.mm(po[:], actt[:, k, i * 128:(i + 1) * 128], w2p[hf][:, k, :], k == 0, k == 7, [actt, w2p[hf]], [po])
                            tm = tmo.next()
                            s.tt("dve", tm[:], po[:], b2t[:, hf * 512:(hf + 1) * 512], ALU.add, [po, b2t], [tm])
                            s.stt("dve", yacc[:, i, hf * 512:(hf + 1) * 512], tm[:], gts[:, i, e_:e_ + 1], yacc[:, i, hf * 512:(hf + 1) * 512], ALU.mult, ALU.add, [tm, gts, yacc], [yacc])
                for i in range(ntl):
                    tok = tok0 + i * 128
                    xt = xts.next()
                    s.dma("sp", xt[:], XR[tok:tok + 128, :], reads=[s.db("XR", ti0 + i)], writes=[xt])
                    s.tt("dve", yacc[:, i, :], yacc[:, i, :], GT2[r][:], ALU.mult, [yacc, GT2[r]], [yacc])
                    s.tt("pool", xt[:], xt[:], yacc[:, i, :], ALU.add, [xt, yacc], [xt])
                    s.dma("sp", XR[tok:tok + 128, :], xt[:], reads=[xt], writes=[s.db("XR", ti0 + i)])
    if stop_after is None:
        with s.phase():
            gfb = s.sb("gfb", [128, D], F32)
            s.dma("sp", gfb[:], gf.rearrange("(o d) -> o d", o=1).to_broadcast([128, D]), writes=[gfb])
            xts = s.sbpool("xt", [128, D], F32, 3)
            junk = s.sb("junk", [128, D], F32)
            sss = s.sbpool("ss", [128, 1], F32, 2)
            for ti in range(C // 128, NT):
                tok = ti * 128
                xt = xts.next()
                s.dma("sp", xt[:], XR[tok:tok + 128, :], reads=[s.db("XR", ti)], writes=[xt])
                ss = sss.next()
                s.act(junk[:], xt[:], AF.Square, [xt], [junk, ss], accum_out=ss[:])
                s.act(ss[:], ss[:], AF.Sqrt, [ss], [ss], scale=1.0 / D, bias=EPS)
                s.recip(ss[:], ss[:], [ss], [ss])
                s.stt("dve", xt[:], xt[:], ss[:, 0:1], gfb[:], ALU.mult, ALU.mult, [xt, ss, gfb], [xt])
                s.dma("act", y_out[tok - C:tok - C + 128, :], xt[:], reads=[xt])
    s.finish()
    s.emit()
    return nc


def kernel(**inputs):
    inp = {k: np.asarray(v) for k, v in inputs.items()}
    B, L, _ = inp["x"].shape
    C = inp["ctx"].shape[1]
    NL = inp["w_ada"].shape[0]
    nc = build(L, C, NL)
    cst = host_consts(L, C)
    in_maps = []
    for b in range(B):
        m = host_layout(inp, b, NL)
        m.update(cst)
        in_maps.append({k: np.ascontiguousarray(v, dtype=np.float32) for k, v in m.items()})
    res = run_bass_kernel_spmd(nc, in_maps, core_ids=list(range(B)))
    return np.stack([np.asarray(r["y"], dtype=np.float32) for r in res.results], 0)
```

```python
from contextlib import ExitStack
import numpy as np
import concourse.bass as bass
import concourse.mybir as mybir
from concourse.bass_utils import run_bass_kernel_spmd

ALU = mybir.AluOpType
AF = mybir.ActivationFunctionType
AX = mybir.AxisListType
F32 = mybir.dt.float32
BF16 = mybir.dt.bfloat16

NSLOT = 12


class Buf:
    __slots__ = ("name", "lw", "rd")

    def __init__(self, name=""):
        self.name = name
        self.lw = None
        self.rd = []


class T:
    def __init__(self, t, b):
        self.t = t
        self.b = b

    def __getitem__(self, k):
        return self.t[k]


class Pool:
    def __init__(self, tiles):
        self.tiles = tiles
        self.i = 0

    def next(self):
        t = self.tiles[self.i % len(self.tiles)]
        self.i += 1
        return t


class Sched:
    def __init__(self, nc):
        self.nc = nc
        self.engs = {"pe": nc.tensor, "act": nc.scalar, "dve": nc.vector, "pool": nc.gpsimd, "sp": nc.sync}
        self.ops = {k: [] for k in self.engs}
        self.cnt = {k: 0 for k in self.engs}
        self.sem = {k: nc.alloc_semaphore("s_" + k) for k in self.engs}
        self.dq = ("sp", "act", "pool")
        self.dsem = {q: [nc.alloc_semaphore("d_%s_%d" % (q, i)) for i in range(NSLOT)] for q in self.dq}
        self.dcnt = {q: 0 for q in self.dq}
        self.known = {k: {} for k in self.engs}
        self.stack = None
        self.uid = 0
        self.dbufs = {}

    def _nm(self, name):
        self.uid += 1
        return "%s_%d" % (name, self.uid)

    def sb(self, name, shape, dtype=F32):
        nm = self._nm(name)
        if self.stack is not None:
            t = self.stack.enter_context(self.nc.sbuf_tensor(nm, list(shape), dtype))
        else:
            t = self.nc.alloc_sbuf_tensor(nm, list(shape), dtype)
        return T(t, Buf(nm))

    def ps(self, name, shape, dtype=F32):
        nm = self._nm(name)
        if self.stack is not None:
            t = self.stack.enter_context(self.nc.psum_tensor(nm, list(shape), dtype))
        else:
            t = self.nc.alloc_psum_tensor(nm, list(shape), dtype)
        return T(t, Buf(nm))

    def sbpool(self, name, shape, dtype, n):
        return Pool([self.sb(name, shape, dtype) for _ in range(n)])

    def pspool(self, name, shape, dtype, n):
        return Pool([self.ps(name, shape, dtype) for _ in range(n)])

    def db(self, *key):
        b = self.dbufs.get(key)
        if b is None:
            b = Buf(str(key))
            self.dbufs[key] = b
        return b

    class _Phase:
        def __init__(self, s):
            self.s = s

        def __enter__(self):
            self.s.stack = ExitStack()
            self.s.stack.__enter__()
            return self

        def __exit__(self, *a):
            self.s.barrier()
            st = self.s.stack
            self.s.stack = None
            st.__exit__(None, None, None)
            return False

    def phase(self):
        return Sched._Phase(self)

    def _need(self, eng, ev, waits):
        if ev is None:
            return
        sem, val, src = ev
        if src == eng and eng == "pe":
            return
        if self.known[eng].get(sem, 0) >= val:
            return
        if waits.get(sem, (None, 0))[1] < val:
            waits[sem] = (sem, val)

    def _deps(self, eng, reads, writes):
        waits = {}
        for b in reads:
            b = b.b if isinstance(b, T) else b
            self._need(eng, b.lw, waits)
        for b in writes:
            b = b.b if isinstance(b, T) else b
            self._need(eng, b.lw, waits)
            for r in b.rd:
                self._need(eng, r, waits)
        for sem, (s_, v) in waits.items():
            self.known[eng][sem] = v
        return list(waits.values())

    def _commit(self, ev, reads, writes):
        for b in reads:
            b = b.b if isinstance(b, T) else b
            b.rd.append(ev)
        for b in writes:
            b = b.b if isinstance(b, T) else b
            b.lw = ev
            b.rd = []

    def op(self, eng, fn, reads=(), writes=()):
        waits = self._deps(eng, reads, writes)
        self.cnt[eng] += 1
        ev = (self.sem[eng], self.cnt[eng], eng)
        self.ops[eng].append((waits, fn, (self.sem[eng], 1)))
        self._commit(ev, reads, writes)

    def dma(self, q, out, in_, reads=(), writes=(), **kw):
        waits = self._deps(q, reads, writes)
        i = self.dcnt[q]
        self.dcnt[q] += 1
        slot = i % NSLOT
        sem = self.dsem[q][slot]
        rnd = i // NSLOT
        if rnd > 0:
            w = {}
            self._need(q, (sem, 16 * rnd, "dma"), w)
            for sem_, (s_, v) in w.items():
                self.known[q][sem_] = v
            waits = waits + list(w.values())
        ev = (sem, 16 * (rnd + 1), "dma")
        fn = lambda e, out=out, in_=in_, kw=kw: e.dma_start(out=out, in_=in_, **kw)
        self.ops[q].append((waits, fn, (sem, 16)))
        self._commit(ev, reads, writes)

    def _all_events(self):
        evs = []
        for k in self.engs:
            if self.cnt[k] > 0:
                evs.append((self.sem[k], self.cnt[k], k))
        for qq in self.dq:
            n = self.dcnt[qq]
            for slot in range(min(n, NSLOT)):
                cnt = (n - slot + NSLOT - 1) // NSLOT
                evs.append((self.dsem[qq][slot], 16 * cnt, "dma"))
        return evs

    def barrier(self):
        evs = self._all_events()
        for eng in self.engs:
            w = {}
            for ev in evs:
                if ev[2] == eng:
                    continue
                self._need(eng, ev, w)
            for sem_, (s_, v) in w.items():
                self.known[eng][sem_] = v
            if w:
                self.ops[eng].append((list(w.values()), None, None))

    def finish(self):
        self.barrier()

    def emit(self):
        nc = self.nc
        with nc.Block() as block:
            def run(name):
                def body(e):
                    for waits, fn, inc in self.ops[name]:
                        for sem, val in waits:
                            e.wait_ge(sem, val)
                        if fn is not None:
                            ins = fn(e)
                            ins.then_inc(inc[0], inc[1])
                return body

            block.tensor(run("pe"))
            block.scalar(run("act"))
            block.vector(run("dve"))
            block.gpsimd(run("pool"))
            block.sync(run("sp"))

    def mm(self, out, lhsT, rhs, start, stop, R, W):
        self.op("pe", lambda e: e.matmul(out, lhsT=lhsT, rhs=rhs, start=start, stop=stop), R, W)

    def tr(self, out, in_, ident, R, W):
        self.op("pe", lambda e: e.transpose(out=out, in_=in_, identity=ident), R, W)

    def act(self, out, in_, func, R, W, **kw):
        self.op("act", lambda e: e.activation(out=out, in_=in_, func=func, **kw), R, W)

    def ts(self, eng, out, in0, s1, s2, op0, op1, R, W):
        if op1 is None:
            self.op(eng, lambda e: e.tensor_scalar(out=out, in0=in0, scalar1=s1, scalar2=None, op0=op0), R, W)
        else:
            self.op(eng, lambda e: e.tensor_scalar(out=out, in0=in0, scalar1=s1, scalar2=s2, op0=op0, op1=op1), R, W)

    def tt(self, eng, out, in0, in1, op, R, W):
        self.op(eng, lambda e: e.tensor_tensor(out=out, in0=in0, in1=in1, op=op), R, W)

    def stt(self, eng, out, in0, scalar, in1, op0, op1, R, W):
        self.op(eng, lambda e: e.scalar_tensor_tensor(out=out, in0=in0, scalar=scalar, in1=in1, op0=op0, op1=op1), R, W)

    def cp(self, eng, out, in_, R, W):
        if eng == "act":
            self.op("act", lambda e: e.copy(out=out, in_=in_), R, W)
        else:
            self.op(eng, lambda e: e.tensor_copy(out=out, in_=in_), R, W)

    def recip(self, out, in_, R, W):
        self.op("dve", lambda e: e.reciprocal(out=out, in_=in_), R, W)

    def memset(self, eng, ap, val, W):
        self.op(eng, lambda e: e.memset(ap, val), (), W)


CUT = 99

D = 1024
NH = 4
EPS = 1e-6
K_SCALE = 32 ** -0.5
ATT_SCALE = 192 ** -0.5
NE = 32
SENT = {}


def host_consts(L, C):
    TT = C + L
    f32 = np.float32
    cst = {}
    cst["ident"] = np.eye(128, dtype=f32)
    rows = L // 64
    row = np.repeat(np.arange(rows), 64).astype(f32)
    col = np.tile(np.arange(64), rows).astype(f32)
    freq = (f32(10000.0) ** (-np.arange(16, dtype=f32) / f32(16))).astype(f32)
    ang = np.concatenate([row[:, None] * freq, col[:, None] * freq], axis=-1).astype(f32)
    cos = np.ones((TT, 32), f32)
    sin = np.zeros((TT, 32), f32)
    cos[C:] = np.cos(ang)
    sin[C:] = np.sin(ang)
    cst["mc"] = np.ascontiguousarray(np.concatenate([cos, cos], 1).T)
    cst["ms"] = np.ascontiguousarray(np.concatenate([-sin, sin], 1).T)
    theta = (1.0 / (f32(10000.0) ** np.linspace(0.0, 1.0, 16, dtype=f32))).astype(f32)
    a2 = (np.arange(TT, dtype=f32)[:, None] * theta).astype(f32)
    cst["rcs"] = np.ascontiguousarray(np.concatenate([np.cos(a2), np.sin(a2)], 1).astype(f32))
    j = np.arange(128)[:, None]
    i = np.arange(128)[None, :]
    dm = np.zeros((128, 5, 128), f32)
    dm[:, 0] = np.maximum(i - j, 0)
    dm[:, 1] = np.maximum(j - i, 0)
    dm[:, 2] = (i > j)
    dm[:, 3] = (j > i)
    dm[:, 4] = 2.0 * (i == j)
    cst["dm"] = dm
    jj = np.arange(128, dtype=f32)
    cst["pcol"] = np.stack([127 - jj, jj, jj + 1, 128 - jj], 1).astype(f32)
    ii = np.arange(128, dtype=f32)
    cst["irow"] = np.ascontiguousarray(np.broadcast_to(np.stack([ii + 1, 128 - ii], 0)[None], (64, 2, 128)).astype(f32))
    return cst


def col(v, p=128):
    v = np.asarray(v)
    sh = v.shape
    return np.ascontiguousarray(v.reshape(sh[:-1] + (sh[-1] // p, p)).swapaxes(-1, -2))


def host_layout(inp, b, NL):
    m = {}
    m["x"] = np.ascontiguousarray(inp["x"][b])
    m["ctx"] = np.ascontiguousarray(inp["ctx"][b])
    cv = np.stack([inp["c"][b], inp["c_ctx"]], 0)
    m["ccol"] = np.ascontiguousarray(col(cv).transpose(1, 2, 0).reshape(128, 16))
    m["w_ada"] = inp["w_ada"]
    m["b_ada"] = inp["b_ada"]
    m["b_adac"] = col(inp["b_ada"])
    m["g1c"] = col(inp["g_norm1"])
    m["g2"] = inp["g_norm2"]
    m["w_in"] = inp["w_in"]
    m["w_out"] = inp["w_out"]
    cw = inp["conv_w"]
    m["convw"] = np.ascontiguousarray(cw.reshape(NL, 31, 2, 128).transpose(0, 3, 2, 1))
    m["convp"] = np.ascontiguousarray(np.stack([col(inp["conv_b"]), col(inp["conv_ln_g"]), col(inp["conv_ln_b"])], -1))
    m["qng"] = col(inp["mla_q_norm"])
    m["kvng"] = col(inp["mla_kv_norm"])
    m["w_uq"] = inp["mla_w_uq"]
    m["w_ukv"] = inp["mla_w_ukv"]
    dec = np.concatenate([inp["ret_decay_fwd"], inp["ret_decay_bwd"]], -1)
    m["dec"] = np.ascontiguousarray(dec)
    m["router_w"] = inp["router_w"]
    m["router_b"] = inp["router_b"]
    m["w1"] = inp["moe_w1"]
    m["w2"] = inp["moe_w2"]
    m["b1c"] = np.ascontiguousarray(col(inp["moe_b1"]).transpose(0, 2, 1, 3))
    m["b2"] = inp["moe_b2"]
    m["gf"] = inp["g_final"]
    return m


def build(L, C, NL=2, dbg=(), stop_after=None):
    TT = C + L
    NT = TT // 128
    nc = bass.Bass("TRN2", target_bir_lowering=False)
    s = Sched(nc)

    def din(name, shape):
        return nc.dram_tensor(name, list(shape), F32, kind="ExternalInput").ap()

    x_in = din("x", [L, D]); ctx_in = din("ctx", [C, D]); ccol = din("ccol", [128, 16])
    w_ada = din("w_ada", [NL, D, 6 * D]); b_ada = din("b_ada", [NL, 6 * D]); b_adac = din("b_adac", [NL, 128, 48])
    g1c = din("g1c", [NL, 128, 8]); g2 = din("g2", [NL, D])
    w_in = din("w_in", [NL, D, 1984]); w_out = din("w_out", [NL, D, D])
    convw = din("convw", [NL, 128, 2, 31]); convp = din("convp", [NL, 128, 2, 3])
    qng = din("qng", [NL, 128, 3]); kvng = din("kvng", [NL, 128, 2])
    w_uq = din("w_uq", [NL, 384, 768]); w_ukv = din("w_ukv", [NL, 256, 1024])
    dec = din("dec", [NL, 8])
    router_w = din("router_w", [NL, D, NE]); router_b = din("router_b", [NL, NE])
    w1 = din("w1", [NL, NE, D, 2048]); w2 = din("w2", [NL, NE, D, D])
    b1c = din("b1c", [NL, 128, NE, 16]); b2 = din("b2", [NL, NE, D]); gf = din("gf", [D])
    ident_d = din("ident", [128, 128]); mc_d = din("mc", [64, TT]); ms_d = din("ms", [64, TT])
    rcs_d = din("rcs", [TT, 32]); dm_d = din("dm", [128, 5, 128]); pcol_d = din("pcol", [128, 4]); irow_d = din("irow", [64, 2, 128])

    def scratch(name, shape, dt):
        kind = "ExternalOutput" if name in dbg else "Internal"
        return nc.dram_tensor(name, list(shape), dt, kind=kind).ap()

    y_out = nc.dram_tensor("y", [L, D], F32, kind="ExternalOutput").ap()
    XR = scratch("XR", [TT, D], F32)
    MOD = scratch("MOD", [NL, 2, 6 * D], F32)
    QN = scratch("QN", [NH, 128, TT], BF16); QR = scratch("QR", [NH, 64, TT], BF16)
    KN = scratch("KN", [NH, 128, TT], BF16); KR = scratch("KR", [64, TT], BF16)
    VV = scratch("VV", [TT, 512], BF16)
    RQT = scratch("RQT", [2, 64, TT], BF16); RKT = scratch("RKT", [2, 64, TT], BF16)
    RK = scratch("RK", [TT, 128], BF16); RV = scratch("RV", [TT, 256], BF16); SG = scratch("SG", [TT, 256], F32)
    MIXT = scratch("MIXT", [D, TT], BF16)
    H2T = scratch("H2T", [D, TT], BF16)
    GATE = scratch("GATE", [TT, NE], F32)
    ZTD = scratch("ZT", [128, 2, TT], BF16)
    W1B = scratch("W1B", [NL, NE, 4, 128, 4096], BF16)
    W2B = scratch("W2B", [NL, NE, 2, 128, 4096], BF16)

    groups = []
    t = 0
    while t < C:
        n = min(512, C - t); groups.append((t, n, 1)); t += n
    while t < TT:
        n = min(512, TT - t); groups.append((t, n, 0)); t += n

    ident_f = s.sb("ident_f", [128, 128], F32)
    ident_b = s.sb("ident_b", [128, 128], BF16)
    ones_b = s.sb("ones_b", [128, 128], BF16)
    s.dma("sp", ident_f[:], ident_d, writes=[ident_f])
    s.dma("pool", ident_b[:], ident_d, writes=[ident_b])
    s.memset("dve", ones_b[:], 1.0, [ones_b])
    ones_f = s.sb("ones_f", [128, 128], F32)
    s.memset("dve", ones_f[:], 1.0, [ones_f])
    modc = s.sb("modc", [128, NL, 48, 2], F32)
    a1c = s.sb("a1c", [128, NL, 2, 8], F32)
    b1c_ = s.sb("b1c_", [128, NL, 2, 8], F32)
    dtsum = s.sb("dtsum", [128, NL, NH, 128], BF16)
    kqdec = s.sb("kqdec", [128, NL, 4, NH], F32)
    cdec = s.sb("cdec", [64, NL, 2, 2], F32)

    with s.phase():
        cc = s.sb("cc", [128, 16], F32)
        scol = s.sb("scol", [128, 16], F32)
        s.dma("sp", cc[:], ccol, writes=[cc])
        s.act(scol[:], cc[:], AF.Sigmoid, [cc], [scol])
        s.tt("dve", scol[:], scol[:], cc[:], ALU.mult, [scol, cc], [scol])
        scv = scol[:].rearrange("p (k r) -> p k r", r=2)
        bac = s.sb("bac", [128, NL, 48], F32)
        for l in range(NL):
            s.dma("sp", bac[:, l, :], b_adac[l], writes=[bac])
        wts = s.sbpool("wada", [128, 8, 512], F32, 2)
        brow = s.sbpool("brow", [2, 512], F32, 2)
        mrow = s.sbpool("mrow", [2, 512], F32, 2)
        pr = s.pspool("pr", [2, 512], F32, 2)
        pc = s.pspool("pc", [128, 4, 2], F32, 2)
        for l in range(NL):
            for n in range(12):
                wt = wts.next()
                s.dma("sp" if n % 2 == 0 else "act", wt[:], w_ada[l][:, n * 512:(n + 1) * 512].rearrange("(k p) c -> p k c", p=128), writes=[wt])
                br = brow.next()
                s.dma("sp", br[:], b_ada[l:l + 1, n * 512:(n + 1) * 512].to_broadcast([2, 512]), writes=[br])
                p = pr.next()
                for k in range(8):
                    s.mm(p[:], scv[:, k, :], wt[:, k, :], k == 0, k == 7, [scol, wt], [p])
                mr = mrow.next()
                s.tt("dve", mr[:], p[:], br[:], ALU.add, [p, br], [mr])
                s.dma("sp", MOD[l][:, n * 512:(n + 1) * 512], mr[:], reads=[mr], writes=[s.db("MOD")])
                pcc = pc.next()
                for j in range(4):
                    for k in range(8):
                        s.mm(pcc[:, j, :], wt[:, k, j * 128:(j + 1) * 128], scv[:, k, :], k == 0, k == 7, [scol, wt], [pcc])
                s.tt("dve", modc[:, l, n * 4:(n + 1) * 4, :], pcc[:], bac[:, l, n * 4:(n + 1) * 4].unsqueeze(2).to_broadcast([128, 4, 2]), ALU.add, [pcc, bac], [modc])
            g1t = s.sb("g1t", [128, 8], F32)
            s.dma("sp", g1t[:], g1c[l], writes=[g1t])
            for r in range(2):
                s.ts("dve", a1c[:, l, r, :], modc[:, l, 8:16, r], 1.0, None, ALU.add, None, [modc], [a1c])
                s.tt("dve", a1c[:, l, r, :], a1c[:, l, r, :], g1t[:], ALU.mult, [a1c, g1t], [a1c])
                s.cp("dve", b1c_[:, l, r, :], modc[:, l, 0:8, r], [modc], [b1c_])
        dmt = s.sb("dmt", [128, 5, 128], F32)
        pcl = s.sb("pcl", [128, 4], F32)
        s.dma("sp", dmt[:], dm_d, writes=[dmt])
        s.dma("sp", pcl[:], pcol_d, writes=[pcl])
        for l in range(NL):
            dcb = s.sb("dcb", [128, 8], F32)
            s.dma("sp", dcb[:], dec[l:l + 1, :].to_broadcast([128, 8]), writes=[dcb])
            lg = s.sb("lg", [128, 8], F32)
            s.act(lg[:], dcb[:], AF.Exp, [dcb], [lg], scale=-1.0)
            s.act(lg[:], lg[:], AF.Ln, [lg], [lg], bias=1.0)
            s.ts("dve", lg[:], lg[:], -1.0, None, ALU.mult, None, [lg], [lg])
            for h in range(NH):
                ef = s.sb("ef", [128, 128], F32)
                eb = s.sb("eb", [128, 128], F32)
                s.act(ef[:], dmt[:, 0, :], AF.Exp, [dmt, lg], [ef], scale=lg[:, h:h + 1])
                s.act(eb[:], dmt[:, 1, :], AF.Exp, [dmt, lg], [eb], scale=lg[:, 4 + h:5 + h])
                s.tt("dve", ef[:], ef[:], dmt[:, 2, :], ALU.mult, [ef, dmt], [ef])
                s.tt("dve", eb[:], eb[:], dmt[:, 3, :], ALU.mult, [eb, dmt], [eb])
                s.tt("dve", ef[:], ef[:], eb[:], ALU.add, [ef, eb], [ef])
                s.tt("dve", dtsum[:, l, h, :], ef[:], dmt[:, 4, :], ALU.add, [ef, dmt], [dtsum])
            s.act(kqdec[:, l, 0, :], lg[:, 0:4], AF.Exp, [lg, pcl], [kqdec], scale=pcl[:, 0:1])
            s.act(kqdec[:, l, 1, :], lg[:, 4:8], AF.Exp, [lg, pcl], [kqdec], scale=pcl[:, 1:2])
            s.act(kqdec[:, l, 2, :], lg[:, 0:4], AF.Exp, [lg, pcl], [kqdec], scale=pcl[:, 2:3])
            s.act(kqdec[:, l, 3, :], lg[:, 4:8], AF.Exp, [lg, pcl], [kqdec], scale=pcl[:, 3:4])
            cd = s.sb("cd", [64, 2, 2], F32)
            for dr in range(2):
                for pp in range(2):
                    for hl in range(2):
                        h = 2 * pp + hl
                        s.cp("dve", cd[32 * hl:32 * hl + 32, dr, pp:pp + 1], lg[32 * hl:32 * hl + 32, dr * 4 + h:dr * 4 + h + 1], [lg], [cd])
            s.act(cdec[:, l, :, :], cd[:], AF.Exp, [cd], [cdec], scale=128.0)
    SENT["s"] = s
    if stop_after == 0:
        s.finish(); s.emit(); return nc

    for l in range(NL):
        with s.phase():
            win = s.sb("win", [128, 8, 1984], BF16)
            for k in range(8):
                s.dma("pool", win[:, k, :], w_in[l][k * 128:(k + 1) * 128, :], writes=[win])
            winsw = s.sb("winsw", [128, 8, 64], BF16)
            wv = w_in[l].rearrange("(k p) c -> p k c", p=128)
            s.dma("pool", winsw[:, :, 0:32], wv[:, :, 1568:1600], writes=[winsw])
            s.dma("pool", winsw[:, :, 32:64], wv[:, :, 1536:1568], writes=[winsw])
            wuq = s.sb("wuq", [128, 3, 768], BF16)
            s.dma("pool", wuq[:], w_uq[l].rearrange("(k p) c -> p k c", p=128), writes=[wuq])
            wuqsw = s.sb("wuqsw", [128, 3, NH, 64], BF16)
            wq4 = w_uq[l].rearrange("(k p) (h c) -> p k h c", p=128, h=NH)
            for h_ in range(NH):
                s.dma("pool", wuqsw[:, :, h_, 0:32], wq4[:, :, h_, 160:192], writes=[wuqsw])
                s.dma("pool", wuqsw[:, :, h_, 32:64], wq4[:, :, h_, 128:160], writes=[wuqsw])
            wukv = s.sb("wukv", [128, 2, 1024], BF16)
            s.dma("pool", wukv[:], w_ukv[l].rearrange("(k p) c -> p k c", p=128), writes=[wukv])
            wukv4 = wukv[:].rearrange("p k (h c) -> p k h c", h=NH)
            qg_t = s.sb("qg_t", [128, 3], F32); kvg_t = s.sb("kvg_t", [128, 2], F32)
            s.dma("sp", qg_t[:], qng[l], writes=[qg_t]); s.dma("sp", kvg_t[:], kvng[l], writes=[kvg_t])

            xts = s.sbpool("xt", [128, D], F32, 2)
            junk = s.sb("junk", [128, D], F32)
            sss = s.sbpool("ss", [128, 1], F32, 2)
            xns = s.sbpool("xn", [128, D], BF16, 2)
            pTs = s.pspool("pT", [128, 8, 128], BF16, 2)
            pms = s.pspool("pm", [128, 512], F32, 6)
            hTs = s.sbpool("hT", [128, 8, 512], BF16, 2)
            sgs = s.sbpool("sg", [128, 512], F32, 2)
            cqs = s.sb("cq", [128, 3, 512], F32)
            sqs = s.sb("sq", [128, 3, 512], BF16)
            cqn = s.sb("cqn", [128, 3, 512], BF16)
            rstd = s.sbpool("rstd", [128, 512], F32, 2)
            obs = s.sbpool("ob", [128, 512], BF16, 3)
            mct = s.sb("mct", [64, 512], F32); mst = s.sb("mst", [64, 512], F32)
            r1s = s.sbpool("r1", [64, 512], F32, 2); r2s = s.sbpool("r2", [64, 512], F32, 2)
            sqg = s.sbpool("sqg", [128, 384], F32, 2); skv = s.sbpool("skv", [128, 384], F32, 2)
            rcst = s.sbpool("rcst", [128, 32], F32, 2)
            tmpr = s.sbpool("tmpr", [128, 4, 4, 16], F32, 2)
            rot = s.sbpool("rot", [128, 2, 128], BF16, 2)
            rts = s.sbpool("rts", [64, 4, 128], BF16, 2)
            rvs = s.sbpool("rvs", [128, 256], BF16, 2)
            sgo = s.sbpool("sgo", [128, 256], F32, 2)
            zq = s.sbpool("zq", [128, 2, 512], BF16, 2)

            for (t0, n, r) in groups:
                nt = n // 128
                hT = hTs.next()
                for i in range(nt):
                    tok = t0 + i * 128
                    xt = xts.next()
                    if l == 0:
                        src = ctx_in[tok:tok + 128, :] if r == 1 else x_in[tok - C:tok - C + 128, :]
                        s.dma("sp", xt[:], src, writes=[xt])
                        s.dma("act", XR[tok:tok + 128, :], xt[:], reads=[xt], writes=[s.db("XR", tok // 128)])
                    else:
                        s.dma("sp", xt[:], XR[tok:tok + 128, :], reads=[s.db("XR", tok // 128)], writes=[xt])
                    ss = sss.next()
                    s.act(junk[:], xt[:], AF.Square, [xt], [junk, ss], accum_out=ss[:])
                    s.act(ss[:], ss[:], AF.Sqrt, [ss], [ss], scale=1.0 / D, bias=EPS)
                    s.recip(ss[:], ss[:], [ss], [ss])
                    xn = xns.next()
                    s.act(xn[:], xt[:], AF.Copy, [xt, ss], [xn], scale=ss[:])
                    pT = pTs.next()
                    for k in range(8):
                        s.tr(pT[:, k, :], xn[:, k * 128:(k + 1) * 128], ident_b[:], [xn, ident_b], [pT])
                    for k in range(8):
                        if k % 2 == 0:
                            s.ts("dve", hT[:, k, i * 128:(i + 1) * 128], pT[:, k, :], a1c[:, l, r, k:k + 1], b1c_[:, l, r, k:k + 1], ALU.mult, ALU.add, [pT, a1c, b1c_], [hT])
                        else:
                            s.act(hT[:, k, i * 128:(i + 1) * 128], pT[:, k, :], AF.Identity, [pT, a1c, b1c_], [hT], scale=a1c[:, l, r, k:k + 1], bias=b1c_[:, l, r, k:k + 1])

                def proj(cols, M):
                    p = pms.next()
                    for k in range(8):
                        s.mm(p[0:M, 0:n], cols(k), hT[:, k, 0:n], k == 0, k == 7, [hT, win, winsw], [p])
                    return p

                if CUT <= 1: continue
                z = zq.next()
                for c in range(2):
                    pa = proj(lambda k: win[:, k, c * 128:(c + 1) * 128], 128)
                    pg = proj(lambda k: win[:, k, 256 + c * 128:256 + (c + 1) * 128], 128)
                    sg = sgs.next()
                    s.act(sg[:, 0:n], pg[:, 0:n], AF.Sigmoid, [pg], [sg])
                    s.tt("dve", z[:, c, 0:n], pa[:, 0:n], sg[:, 0:n], ALU.mult, [pa, sg], [z])
                s.dma("sp", ZTD[:, :, t0:t0 + n], z[:, :, 0:n], reads=[z], writes=[s.db("ZT", t0)])

                if CUT <= 2: continue
                def featnorm(c0, nk, gt, outn, raw):
                    for k in range(nk):
                        p = proj(lambda kk: win[:, kk, c0 + k * 128:c0 + (k + 1) * 128], 128)
                        s.cp("act", raw[:, k, 0:n], p[:, 0:n], [p], [raw])
                        s.tt("pool", sqs[:, k, 0:n], raw[:, k, 0:n], raw[:, k, 0:n], ALU.mult, [raw], [sqs])
                    pss = pms.next()
                    for k in range(nk):
                        s.mm(pss[:, 0:n], ones_b[:], sqs[:, k, 0:n], k == 0, k == nk - 1, [ones_b, sqs], [pss])
                    rs = rstd.next()
                    s.act(rs[:, 0:n], pss[:, 0:n], AF.Sqrt, [pss], [rs], scale=1.0 / (nk * 128), bias=EPS)
                    s.recip(rs[:, 0:n], rs[:, 0:n], [rs], [rs])
                    for k in range(nk):
                        s.stt("dve", outn[:, k, 0:n], raw[:, k, 0:n], gt[:, k:k + 1], rs[:, 0:n], ALU.mult, ALU.mult, [raw, gt, rs], [outn])

                featnorm(512, 3, qg_t, cqn, cqs)
                s.dma("sp", mct[:, 0:n], mc_d[:, t0:t0 + n], writes=[mct])
                s.dma("sp", mst[:, 0:n], ms_d[:, t0:t0 + n], writes=[mst])

                def rope_fm(p_main, p_sw, dst, dkey):
                    r1 = r1s.next(); r2 = r2s.next()
                    s.tt("dve", r1[:, 0:n], p_main[0:64, 0:n], mct[:, 0:n], ALU.mult, [p_main, mct], [r1])
                    s.tt("dve", r2[:, 0:n], p_sw[0:64, 0:n], mst[:, 0:n], ALU.mult, [p_sw, mst], [r2])
                    ob = obs.next()
                    s.tt("pool", ob[0:64, 0:n], r1[:, 0:n], r2[:, 0:n], ALU.add, [r1, r2], [ob])
                    s.dma("sp", dst, ob[0:64, 0:n], reads=[ob], writes=[dkey])

                for h in range(NH):
                    p = pms.next()
                    for k in range(3):
                        s.mm(p[:, 0:n], wuq[:, k, h * 192:h * 192 + 128], cqn[:, k, 0:n], k == 0, k == 2, [wuq, cqn], [p])
                    ob = obs.next()
                    s.cp("act", ob[:, 0:n], p[:, 0:n], [p], [ob])
                    s.dma("sp", QN[h][:, t0:t0 + n], ob[:, 0:n], reads=[ob], writes=[s.db("QN", h, t0)])
                    p1 = pms.next(); p2 = pms.next()
                    for k in range(3):
                        s.mm(p1[0:64, 0:n], wuq[:, k, h * 192 + 128:h * 192 + 192], cqn[:, k, 0:n], k == 0, k == 2, [wuq, cqn], [p1])
                    for k in range(3):
                        s.mm(p2[0:64, 0:n], wuqsw[:, k, h, :], cqn[:, k, 0:n], k == 0, k == 2, [wuqsw, cqn], [p2])
                    rope_fm(p1, p2, QR[h][:, t0:t0 + n], s.db("QR", h, t0))

                if CUT <= 3: continue
                featnorm(1280, 2, kvg_t, cqn, cqs)
                for h in range(NH):
                    p = pms.next()
                    for k in range(2):
                        s.mm(p[:, 0:n], wukv4[:, k, h, 0:128], cqn[:, k, 0:n], k == 0, k == 1, [wukv, cqn], [p])
                    ob = obs.next()
                    s.cp("act", ob[:, 0:n], p[:, 0:n], [p], [ob])
                    s.dma("sp", KN[h][:, t0:t0 + n], ob[:, 0:n], reads=[ob], writes=[s.db("KN", h, t0)])
                for i in range(nt):
                    p = pms.next()
                    for h in range(NH):
                        for k in range(2):
                            s.mm(p[:, h * 128:(h + 1) * 128], cqn[:, k, i * 128:(i + 1) * 128], wukv4[:, k, h, 128:256], k == 0, k == 1, [wukv, cqn], [p])
                    ob = obs.next()
                    s.cp("act", ob[:], p[:], [p], [ob])
                    s.dma("sp", VV[t0 + i * 128:t0 + (i + 1) * 128, :], ob[:], reads=[ob], writes=[s.db("VV", (t0 + i * 128) // 128)])
                if CUT <= 4: continue
                p1 = proj(lambda k: win[:, k, 1536:1600], 64)
                p2 = proj(lambda k: winsw[:, k, :], 64)
                rope_fm(p1, p2, KR[:, t0:t0 + n], s.db("KR", t0))
                if CUT <= 5: continue
                for i in range(nt):
                    tok = t0 + i * 128
                    pq = pms.next(); pk = pms.next()
                    for k in range(8):
                        s.mm(pq[:, 0:384], hT[:, k, i * 128:(i + 1) * 128], win[:, k, 896:1280], k == 0, k == 7, [hT, win], [pq])
                    for k in range(8):
                        s.mm(pk[:, 0:384], hT[:, k, i * 128:(i + 1) * 128], win[:, k, 1600:1984], k == 0, k == 7, [hT, win], [pk])
                    a = sqg.next(); b = skv.next()
                    s.cp("act", a[:, 0:128], pq[:, 0:128], [pq], [a])
                    s.act(b[:, 0:128], pk[:, 0:128], AF.Identity, [pk], [b], scale=K_SCALE)
                    so = sgo.next()
                    s.act(so[:], pq[:, 128:384], AF.Sigmoid, [pq], [so])
                    s.tt("dve", so[:], so[:], pq[:, 128:384], ALU.mult, [so, pq], [so])
                    s.dma("act", SG[tok:tok + 128, :], so[:], reads=[so], writes=[s.db("SG", tok // 128)])
                    rv_ = rvs.next()
                    s.cp("dve", rv_[:], pk[:, 128:384], [pk], [rv_])
                    s.dma("act", RV[tok:tok + 128, :], rv_[:], reads=[rv_], writes=[s.db("RV", tok // 128)])
                    if CUT <= 6: continue
                    cs = rcst.next()
                    s.dma("act", cs[:], rcs_d[tok:tok + 128, :], writes=[cs])
                    cosb = cs[:, 0:16].unsqueeze(1).to_broadcast([128, 4, 16])
                    sinb = cs[:, 16:32].unsqueeze(1).to_broadcast([128, 4, 16])
                    ro = rot.next()
                    tm = tmpr.next()
                    for qi, srcT in enumerate((a, b)):
                        v4 = srcT[:, 0:128].rearrange("p (h two d) -> p h two d", h=4, two=2)
                        o4 = ro[:, qi, :].rearrange("p (h two d) -> p h two d", h=4, two=2)
                        x1 = v4[:, :, 0, :]; x2 = v4[:, :, 1, :]
                        s.tt("dve", tm[:, :, 0, :], x1, cosb, ALU.mult, [srcT, cs], [tm])
                        s.tt("dve", tm[:, :, 1, :], x2, sinb, ALU.mult, [srcT, cs], [tm])
                        s.tt("dve", tm[:, :, 2, :], x2, cosb, ALU.mult, [srcT, cs], [tm])
                        s.tt("dve", tm[:, :, 3, :], x1, sinb, ALU.mult, [srcT, cs], [tm])
                        s.tt("dve", o4[:, :, 0, :], tm[:, :, 0, :], tm[:, :, 1, :], ALU.subtract, [tm], [ro])
                        s.tt("dve", o4[:, :, 1, :], tm[:, :, 2, :], tm[:, :, 3, :], ALU.add, [tm], [ro])
                    if CUT <= 7: continue
                    s.dma("sp", RK[tok:tok + 128, :], ro[:, 1, :], reads=[ro], writes=[s.db("RK", tok // 128)])
                    pT = pTs.next()
                    for qi in range(2):
                        for pp in range(2):
                            s.tr(pT[0:64, qi * 2 + pp, :], ro[:, qi, pp * 64:(pp + 1) * 64], ident_b[:], [ro, ident_b], [pT])
                    rt = rts.next()
                    s.cp("dve", rt[:], pT[0:64, 0:4, :], [pT], [rt])
                    s.dma("sp", RQT[:, :, tok:tok + 128].rearrange("a p t -> p a t"), rt[:, 0:2, :], reads=[rt], writes=[s.db("RQT", tok // 128)])
                    s.dma("sp", RKT[:, :, tok:tok + 128].rearrange("a p t -> p a t"), rt[:, 2:4, :], reads=[rt], writes=[s.db("RKT", tok // 128)])
        if stop_after == "A":
            break
        with s.phase():
            cwt = s.sb("cwt", [128, 2, 31], F32); cpt = s.sb("cpt", [128, 2, 3], F32)
            s.dma("sp", cwt[:], convw[l], writes=[cwt]); s.dma("sp", cpt[:], convp[l], writes=[cpt])
            zts = s.sbpool("zt", [128, 2, 512 + 30], BF16, 2)
            accs = s.sbpool("acc", [128, 2, 512], F32, 2)
            ybs = s.sbpool("yb", [128, 2, 512], BF16, 2)
            yqs = s.sbpool("yq", [128, 2, 512], BF16, 2)
            pS = s.pspool("pS", [128, 512], F32, 2); pQ = s.pspool("pQ", [128, 512], F32, 2)
            mean = s.sbpool("mean", [128, 512], F32, 2); var = s.sbpool("var", [128, 512], F32, 2)
            msq = s.sbpool("msq", [128, 512], F32, 2)
            dd = s.sbpool("dd", [128, 512], F32, 2)
            oc = s.sbpool("oc", [128, 512], BF16, 3)
            for (t0, n, r) in groups:
                lo, hi = (0, C) if r == 1 else (C, TT)
                zt = zts.next()
                s.memset("pool", zt[:], 0.0, [zt])
                a0 = max(lo, t0 - 15); a1 = min(hi, t0 + n + 15)
                s.dma("sp", zt[:, :, a0 - (t0 - 15):a1 - (t0 - 15)], ZTD[:, :, a0:a1], reads=[s.db("ZT", g[0]) for g in groups], writes=[zt])
                acc = accs.next(); yb = ybs.next(); yq = yqs.next()
                for c in range(2):
                    eng = "dve"
                    s.ts(eng, acc[:, c, 0:n], zt[:, c, 0:n], cwt[:, c, 0:1], cpt[:, c, 0:1], ALU.mult, ALU.add, [zt, cwt, cpt], [acc])
                    for k in range(1, 31):
                        s.stt(eng, acc[:, c, 0:n], zt[:, c, k:k + n], cwt[:, c, k:k + 1], acc[:, c, 0:n], ALU.mult, ALU.add, [zt, cwt, acc], [acc])
                    s.cp("act", yb[:, c, 0:n], acc[:, c, 0:n], [acc], [yb])
                    s.act(yq[:, c, 0:n], acc[:, c, 0:n], AF.Square, [acc], [yq])
                ps_ = pS.next(); pq_ = pQ.next()
                for c in range(2):
                    s.mm(ps_[:, 0:n], ones_b[:], yb[:, c, 0:n], c == 0, c == 1, [ones_b, yb], [ps_])
                for c in range(2):
                    s.mm(pq_[:, 0:n], ones_b[:], yq[:, c, 0:n], c == 0, c == 1, [ones_b, yq], [pq_])
                mn = mean.next(); vr = var.next(); mq = msq.next()
                s.ts("dve", mn[:, 0:n], ps_[:, 0:n], 1.0 / 256, None, ALU.mult, None, [ps_], [mn])
                s.tt("pool", mq[:, 0:n], mn[:, 0:n], mn[:, 0:n], ALU.mult, [mn], [mq])
                s.stt("dve", vr[:, 0:n], pq_[:, 0:n], 1.0 / 256, mq[:, 0:n], ALU.mult, ALU.subtract, [pq_, mq], [vr])
                s.ts("dve", vr[:, 0:n], vr[:, 0:n], 0.0, None, ALU.max, None, [vr], [vr])
                s.act(vr[:, 0:n], vr[:, 0:n], AF.Sqrt, [vr], [vr], bias=1e-5)
                s.recip(vr[:, 0:n], vr[:, 0:n], [vr], [vr])
                for c in range(2):
                    d_ = dd.next()
                    s.tt("pool", d_[:, 0:n], acc[:, c, 0:n], mn[:, 0:n], ALU.subtract, [acc, mn], [d_])
                    s.tt("pool", d_[:, 0:n], d_[:, 0:n], vr[:, 0:n], ALU.mult, [d_, vr], [d_])
                    o = oc.next()
                    s.act(d_[:, 0:n], d_[:, 0:n], AF.Identity, [d_, cpt], [d_], scale=cpt[:, c, 1:2], bias=cpt[:, c, 2:3])
                    sg2 = msq.next()
                    s.act(sg2[:, 0:n], d_[:, 0:n], AF.Sigmoid, [d_], [sg2])
                    s.tt("dve", o[:, 0:n], d_[:, 0:n], sg2[:, 0:n], ALU.mult, [d_, sg2], [o])
                    s.dma("sp", MIXT[c * 128:(c + 1) * 128, t0:t0 + n], o[:, 0:n], reads=[o], writes=[s.db("MIXT", c, t0)])
        with s.phase():
            NCH = NT
            Sst = s.sb("Sst", [64, 2, 2, NCH, 64], BF16)
            Sf = [[s.sb("Sf", [64, 128], F32) for pp in range(2)] for dr in range(2)]
            for dr in range(2):
                for pp in range(2):
                    s.memset("dve", Sf[dr][pp][:], 0.0, [Sf[dr][pp]])
            qd = s.sb("qd", [64, 2, 2, 128], F32)
            irow = s.sb("irow", [64, 2, 128], F32)
            s.dma("sp", irow[:], irow_d, writes=[irow])
            cdl = s.sb("cdl", [64, 2, 2], F32)
            s.act(cdl[:], cdec[:, l, :, :], AF.Ln, [cdec], [cdl], scale=1.0)
            s.ts("dve", cdl[:], cdl[:], 1.0 / 128, None, ALU.mult, None, [cdl], [cdl])
            for dr in range(2):
                for pp in range(2):
                    s.act(qd[:, dr, pp, :], irow[:, dr, :], AF.Exp, [irow, cdl], [qd], scale=cdl[:, dr, pp:pp + 1])
            rkt = s.sbpool("rkt", [128, 128], BF16, 3); rvt = s.sbpool("rvt", [128, 256], BF16, 3)
            kds = s.sbpool("kd", [128, 128], BF16, 3)
            pkv = s.pspool("pkv", [64, 128], F32, 2)
            nchc = C // 128
            order_f = list(range(NCH))
            order_b = list(range(nchc - 1, -1, -1)) + list(range(NCH - 1, nchc - 1, -1))
            for dr, order in ((0, order_f), (1, order_b)):
                for ci in order:
                    tok = ci * 128
                    rk_ = rkt.next(); rv_ = rvt.next()
                    s.dma("sp", rk_[:], RK[tok:tok + 128, :], reads=[s.db("RK", ci)], writes=[rk_])
                    s.dma("act", rv_[:], RV[tok:tok + 128, :], reads=[s.db("RV", ci)], writes=[rv_])
                    kd = kds.next()
                    s.tt("pool", kd[:].rearrange("p (h d) -> p h d", h=4), rk_[:].rearrange("p (h d) -> p h d", h=4),
                         kqdec[:, l, dr, :].unsqueeze(2).to_broadcast([128, 4, 32]), ALU.mult, [rk_, kqdec], [kd])
                    for pp in range(2):
                        S_ = Sf[dr][pp]
                        s.cp("act", Sst[0:32, dr, pp, ci, :], S_[0:32, 0:64], [S_], [Sst])
                        s.cp("act", Sst[32:64, dr, pp, ci, :], S_[32:64, 64:128], [S_], [Sst])
                        p = pkv.next()
                        s.mm(p[:], kd[:, pp * 64:(pp + 1) * 64], rv_[:, pp * 128:(pp + 1) * 128], True, True, [kd, rv_], [p])
                        s.stt("dve", S_[:], S_[:], cdec[:, l, dr, pp:pp + 1], p[:], ALU.mult, ALU.add, [S_, cdec, p], [S_])
            qts = s.sbpool("qt", [64, 2, 128], BF16, 2); kts = s.sbpool("kt", [64, 2, 128], BF16, 2)
            qfs = s.sbpool("qf", [64, 2, 2, 128], BF16, 2)
            sgt = s.sbpool("sgt", [128, 256], F32, 2)
            pst = s.pspool("pst", [128, 128], F32, 2)
            po = s.pspool("po", [128, 256], F32, 2)
            pps = s.sbpool("pp", [128, 128], BF16, 3)
            osb = s.sbpool("osb", [128, 256], F32, 2); osq = s.sbpool("osq", [128, 256], F32, 2)
            ssm = s.sbpool("ssm", [128, 4], F32, 2)
            yrb = s.sbpool("yrb", [128, 256], BF16, 2)
            pTr = s.pspool("pTr", [128, 2, 128], BF16, 2)
            yrt = s.sbpool("yrt", [128, 2, 128], BF16, 2)
            for ci in range(NCH):
                tok = ci * 128
                qt = qts.next(); kt = kts.next(); rv_ = rvt.next(); sg_ = sgt.next()
                s.dma("sp", qt[:], RQT[:, :, tok:tok + 128].rearrange("a p t -> p a t"), reads=[s.db("RQT", ci)], writes=[qt])
                s.dma("sp", kt[:], RKT[:, :, tok:tok + 128].rearrange("a p t -> p a t"), reads=[s.db("RKT", ci)], writes=[kt])
                s.dma("act", rv_[:], RV[tok:tok + 128, :], reads=[s.db("RV", ci)], writes=[rv_])
                s.dma("act", sg_[:], SG[tok:tok + 128, :], reads=[s.db("SG", ci)], writes=[sg_])
                qf = qfs.next()
                for dr in range(2):
                    s.tt("pool", qf[:, dr, :, :], qt[:], qd[:, dr, :, :], ALU.mult, [qt, qd], [qf])
                o = po.next()
                for h in range(NH):
                    pp = h // 2; b0 = 32 * (h % 2)
                    st = pst.next()
                    s.mm(st[:], kt[b0:b0 + 32, pp, :], qt[b0:b0 + 32, pp, :], True, True, [kt, qt], [st])
                    P = pps.next()
                    s.tt("dve", P[:], st[:], dtsum[:, l, h, :], ALU.mult, [st, dtsum], [P])
                    s.mm(o[:, h * 64:(h + 1) * 64], P[:], rv_[:, h * 64:(h + 1) * 64], True, False, [P, rv_], [o])
                    s.mm(o[:, h * 64:(h + 1) * 64], qf[b0:b0 + 32, 0, pp, :], Sst[b0:b0 + 32, 0, pp, ci, :], False, False, [qf, Sst], [o])
                    s.mm(o[:, h * 64:(h + 1) * 64], qf[b0:b0 + 32, 1, pp, :], Sst[b0:b0 + 32, 1, pp, ci, :], False, True, [qf, Sst], [o])
                ob = osb.next(); oq = osq.next(); sm = ssm.next()
                s.cp("act", ob[:], o[:], [o], [ob])
                s.tt("pool", oq[:], ob[:], ob[:], ALU.mult, [ob], [oq])
                s.op("dve", lambda e, sm=sm, oq=oq: e.reduce_sum(out=sm[:], in_=oq[:].rearrange("p (h e) -> p h e", h=4), axis=AX.X), [oq], [sm])
                s.act(sm[:], sm[:], AF.Sqrt, [sm], [sm], scale=1.0 / 64, bias=EPS)
                s.recip(sm[:], sm[:], [sm], [sm])
                s.tt("pool", ob[:].rearrange("p (h e) -> p h e", h=4), ob[:].rearrange("p (h e) -> p h e", h=4), sm[:].unsqueeze(2).to_broadcast([128, 4, 64]), ALU.mult, [ob, sm], [ob])
                yb = yrb.next()
                s.tt("pool", yb[:], ob[:], sg_[:], ALU.mult, [ob, sg_], [yb])
                pt = pTr.next()
                for c in range(2):
                    s.tr(pt[:, c, :], yb[:, c * 128:(c + 1) * 128], ident_b[:], [yb, ident_b], [pt])
                yt = yrt.next()
                s.cp("dve", yt[:], pt[:], [pt], [yt])
                s.dma("sp", MIXT[768:1024, tok:tok + 128].rearrange("(c p) t -> p c t", p=128), yt[:], reads=[yt], writes=[s.db("MIXT", "r", ci)])
        with s.phase():
            knt = s.sb("knt", [128, TT], BF16); krt = s.sb("krt", [64, TT], BF16); vt = s.sb("vt", [128, NT, 128], BF16)
            s.dma("sp", krt[:], KR, reads=[s.db("KR", g[0]) for g in groups], writes=[krt])
            qnt = s.sbpool("qnt", [128, 512], BF16, 2); qrt = s.sbpool("qrt", [64, 512], BF16, 2)
            pst = s.pspool("pst", [128, 512], F32, 3)
            pacc = s.pspool("pacc", [128, 512], F32, 2); pden = s.pspool("pden", [128, 512], F32, 2)
            Ps = s.sbpool("P", [128, 512], BF16, 4)
            paccA = s.sbpool("paccA", [128, 512], F32, 2); paccB = s.sbpool("paccB", [128, 512], F32, 2)
            rec = s.sbpool("rec", [128, 512], F32, 2)
            oat = s.sbpool("oat", [128, 512], BF16, 2)
            for h in range(NH):
                s.dma("sp", knt[:], KN[h], reads=[s.db("KN", h, g[0]) for g in groups], writes=[knt])
                s.dma("act", vt[:], VV[:, h * 128:(h + 1) * 128].rearrange("(t p) c -> p t c", p=128), reads=[s.db("VV", i) for i in range(NT)], writes=[vt])
                for (t0, n, r) in groups:
                    qn = qnt.next(); qr = qrt.next()
                    s.dma("sp", qn[:, 0:n], QN[h][:, t0:t0 + n], reads=[s.db("QN", h, t0)], writes=[qn])
                    s.dma("act", qr[:, 0:n], QR[h][:, t0:t0 + n], reads=[s.db("QR", h, t0)], writes=[qr])
                    nk = C // 128 if r == 1 else NT
                    acc = pacc.next(); den = pden.next()
                    pa = [paccA.next(), paccB.next()]
                    sts = {}; Pt = {}
                    for jt in range(nk + 1):
                        if jt < nk:
                            st = pst.next()
                            s.mm(st[:, 0:n], knt[:, jt * 128:(jt + 1) * 128], qn[:, 0:n], True, False, [knt, qn], [st])
                            s.mm(st[:, 0:n], krt[:, jt * 128:(jt + 1) * 128], qr[:, 0:n], False, True, [krt, qr], [st])
                            P = Ps.next()
                            s.act(P[:, 0:n], st[:, 0:n], AF.Exp, [st], [P], scale=ATT_SCALE)
                            Pt[jt] = P
                        if jt >= 1:
                            j = jt - 1
                            P = Pt.pop(j)
                            s.mm(acc[:, 0:n], vt[:, j, :], P[:, 0:n], j == 0, j == nk - 1, [vt, P], [acc])
                            eng = "dve" if j % 2 == 0 else "pool"
                            A_ = pa[j % 2]
                            if j < 2:
                                s.cp(eng, A_[:, 0:n], P[:, 0:n], [P], [A_])
                            else:
                                s.tt(eng, A_[:, 0:n], A_[:, 0:n], P[:, 0:n], ALU.add, [A_, P], [A_])
                    nA = 2 if nk >= 2 else 1
                    for a_ in range(nA):
                        s.mm(den[:, 0:n], ones_f[:], pa[a_][:, 0:n], a_ == 0, a_ == nA - 1, [ones_f, pa[a_]], [den])
                    rc = rec.next()
                    s.recip(rc[:, 0:n], den[:, 0:n], [den], [rc])
                    oa = oat.next()
                    s.tt("dve", oa[:, 0:n], acc[:, 0:n], rc[:, 0:n], ALU.mult, [acc, rc], [oa])
                    s.dma("sp", MIXT[256 + h * 128:256 + (h + 1) * 128, t0:t0 + n], oa[:, 0:n], reads=[oa], writes=[s.db("MIXT", "a", h, t0)])
        if stop_after == "D":
            break
        with s.phase():
            wo = s.sb("wo", [128, 8, D], BF16)
            for k in range(8):
                s.dma("pool", wo[:, k, :], w_out[l][k * 128:(k + 1) * 128, :], writes=[wo])
            rw = s.sb("rw", [128, 8, NE], F32)
            s.dma("sp", rw[:], router_w[l].rearrange("(k p) e -> p k e", p=128), writes=[rw])
            rb = s.sb("rb", [128, NE], F32)
            s.dma("sp", rb[:], router_b[l:l + 1, :].to_broadcast([128, NE]), writes=[rb])
            g2b = s.sb("g2b", [128, D], F32)
            s.dma("sp", g2b[:], g2[l:l + 1, :].to_broadcast([128, D]), writes=[g2b])
            GT1 = []; A2 = []; B2 = []
            for r in range(2):
                gt = s.sb("gt1", [128, D], F32); a2 = s.sb("a2", [128, D], F32); b2_ = s.sb("b2_", [128, D], F32)
                s.dma("sp", gt[:], MOD[l][r:r + 1, 2 * D:3 * D].to_broadcast([128, D]), reads=[s.db("MOD")], writes=[gt])
                s.dma("sp", b2_[:], MOD[l][r:r + 1, 3 * D:4 * D].to_broadcast([128, D]), reads=[s.db("MOD")], writes=[b2_])
                s.dma("sp", a2[:], MOD[l][r:r + 1, 4 * D:5 * D].to_broadcast([128, D]), reads=[s.db("MOD")], writes=[a2])
                s.stt("dve", a2[:], a2[:], 1.0, g2b[:], ALU.add, ALU.mult, [a2, g2b], [a2])
                GT1.append(gt); A2.append(a2); B2.append(b2_)
            mts = s.sbpool("mt", [128, 8, 128], BF16, 2)
            xts = s.sbpool("xt", [128, D], F32, 2)
            pmx = s.pspool("pmx", [128, D], F32, 2)
            tmps = s.sbpool("tmp", [128, D], F32, 2)
            junk = s.sb("junk", [128, D], F32)
            sss = s.sbpool("ss", [128, 1], F32, 2)
            h2s = s.sbpool("h2", [128, D], F32, 2)
            pT2 = s.pspool("pT2", [128, 8, 128], BF16, 1); pT3 = s.pspool("pT3", [128, 8, 128], BF16, 1)
            h2b = s.sbpool("h2b", [128, 16, 128], BF16, 2)
            hhis = s.sbpool("hhi", [128, D], BF16, 2); hlos = s.sbpool("hlo", [128, D], BF16, 2)
            rwh = s.sb("rwh", [128, 8, NE], BF16); rwl = s.sb("rwl", [128, 8, NE], BF16)
            s.cp("dve", rwh[:], rw[:], [rw], [rwh])
            s.tt("dve", rw[:], rw[:], rwh[:], ALU.subtract, [rw, rwh], [rw])
            s.cp("dve", rwl[:], rw[:], [rw], [rwl])
            plg = s.pspool("plg", [128, NE], F32, 2)
            lgs = s.sbpool("lgs", [128, NE], F32, 2); t8 = s.sbpool("t8", [128, 8], F32, 2)
            msk = s.sbpool("msk", [128, NE], F32, 2); ex = s.sbpool("ex", [128, NE], F32, 2)
            nm = s.sbpool("nm", [128, 1], F32, 2); sm1 = s.sbpool("sm1", [128, 1], F32, 2)
            for ti in range(NT):
                tok = ti * 128
                r = 1 if tok < C else 0
                if r == 1 and l == NL - 1:
                    continue
                mt = mts.next()
                s.dma("sp", mt[:], MIXT[:, tok:tok + 128].rearrange("(k p) t -> p k t", p=128), reads=[s.db(*k_) for k_ in list(s.dbufs) if k_[0] == "MIXT"], writes=[mt])
                xt = xts.next()
                s.dma("act", xt[:], XR[tok:tok + 128, :], reads=[s.db("XR", ti)], writes=[xt])
                pm = pmx.next()
                for hf in range(2):
                    for k in range(8):
                        s.mm(pm[:, hf * 512:(hf + 1) * 512], mt[:, k, :], wo[:, k, hf * 512:(hf + 1) * 512], k == 0, k == 7, [mt, wo], [pm])
                tp = tmps.next()
                for hf in range(2):
                    s.tt("dve", tp[:, hf * 512:(hf + 1) * 512], pm[:, hf * 512:(hf + 1) * 512], GT1[r][:, hf * 512:(hf + 1) * 512], ALU.mult, [pm, GT1[r]], [tp])
                s.tt("pool", xt[:], xt[:], tp[:], ALU.add, [xt, tp], [xt])
                s.dma("sp", XR[tok:tok + 128, :], xt[:], reads=[xt], writes=[s.db("XR", ti)])
                if CUT == 11: continue
                ss = sss.next()
                s.act(junk[:], xt[:], AF.Square, [xt], [junk, ss], accum_out=ss[:])
                s.act(ss[:], ss[:], AF.Sqrt, [ss], [ss], scale=1.0 / D, bias=EPS)
                s.recip(ss[:], ss[:], [ss], [ss])
                h2 = h2s.next()
                s.stt("dve", h2[:], xt[:], ss[:, 0:1], A2[r][:], ALU.mult, ALU.mult, [xt, ss, A2[r]], [h2])
                s.tt("pool", h2[:], h2[:], B2[r][:], ALU.add, [h2, B2[r]], [h2])
                if CUT == 12: continue
                hhi = hhis.next(); hlo = hlos.next()
                s.cp("act", hhi[:], h2[:], [h2], [hhi])
                s.tt("pool", h2[:], h2[:], hhi[:], ALU.subtract, [h2, hhi], [h2])
                s.cp("pool", hlo[:], h2[:], [h2], [hlo])
                if CUT == 13: continue
                pt = pT2.next(); ptl = pT3.next()
                for k in range(8):
                    s.tr(pt[:, k, :], hhi[:, k * 128:(k + 1) * 128], ident_b[:], [hhi, ident_b], [pt])
                for k in range(8):
                    s.tr(ptl[:, k, :], hlo[:, k * 128:(k + 1) * 128], ident_b[:], [hlo, ident_b], [ptl])
                hb_ = h2b.next()
                s.cp("dve", hb_[:, 0:8, :], pt[:], [pt], [hb_])
                s.cp("dve", hb_[:, 8:16, :], ptl[:], [ptl], [hb_])
                for k in range(8):
                    s.dma("sp" if k % 2 == 0 else "act", H2T[k * 128:(k + 1) * 128, tok:tok + 128], hb_[:, k, :], reads=[hb_], writes=[s.db("H2T", ti)])
                if CUT == 8: continue
                pl_ = plg.next()
                for k in range(8):
                    s.mm(pl_[:], hb_[:, k, :], rwh[:, k, :], k == 0, False, [hb_, rwh], [pl_])
                    s.mm(pl_[:], hb_[:, 8 + k, :], rwh[:, k, :], False, False, [hb_, rwh], [pl_])
                    s.mm(pl_[:], hb_[:, k, :], rwl[:, k, :], False, k == 7, [hb_, rwl], [pl_])
                lg_ = lgs.next()
                s.tt("dve", lg_[:], pl_[:], rb[:], ALU.add, [pl_, rb], [lg_])
                if CUT == 9: continue
                t8_ = t8.next()
                s.op("dve", lambda e, t8_=t8_, lg_=lg_: e.max(out=t8_[:], in_=lg_[:]), [lg_], [t8_])
                mk_ = msk.next()
                s.ts("dve", mk_[:], lg_[:], t8_[:, 3:4], None, ALU.is_ge, None, [lg_, t8_], [mk_])
                nm_ = nm.next()
                s.ts("dve", nm_[:], t8_[:, 0:1], -1.0, None, ALU.mult, None, [t8_], [nm_])
                ex_ = ex.next()
                s.act(ex_[:], lg_[:], AF.Exp, [lg_, nm_], [ex_], bias=nm_[:, 0:1])
                s.tt("dve", ex_[:], ex_[:], mk_[:], ALU.mult, [ex_, mk_], [ex_])
                sm_ = sm1.next()
                s.op("dve", lambda e, sm_=sm_, ex_=ex_: e.reduce_sum(out=sm_[:], in_=ex_[:], axis=AX.X), [ex_], [sm_])
                s.recip(sm_[:], sm_[:], [sm_], [sm_])
                s.ts("dve", ex_[:], ex_[:], sm_[:, 0:1], None, ALU.mult, None, [ex_, sm_], [ex_])
                s.dma("sp", GATE[tok:tok + 128, :], ex_[:], reads=[ex_], writes=[s.db("GATE", ti)])
        if stop_after == "E":
            break
        with s.phase():
            b1t = s.sb("b1t", [128, NE, 16], F32)
            s.dma("sp", b1t[:], b1c[l], writes=[b1t])
            GT2 = []
            for r in range(2):
                gt = s.sb("gt2", [128, D], F32)
                s.dma("sp", gt[:], MOD[l][r:r + 1, 5 * D:6 * D].to_broadcast([128, D]), reads=[s.db("MOD")], writes=[gt])
                GT2.append(gt)
            wps = s.sbpool("wp", [128, 8, 512], BF16, 8)
            b2s = s.sbpool("b2s", [128, D], F32, 2)
            hts = s.sb("hts", [128, 8, 1024], BF16)
            yacc = s.sb("yacc", [128, 8, D], F32)
            gts = s.sb("gts", [128, 8, NE], F32)
            actt = s.sb("actt", [128, 8, 1024], BF16)
            actt_b = [Buf("actt0"), Buf("actt1")]
            gq = s.sbpool("gq", [128, 512], F32, 3); sq_ = s.sbpool("sq_", [128, 512], F32, 3)
            lq = s.sbpool("lq", [128, 512], F32, 3); tq = s.sbpool("tq", [128, 512], F32, 3)
            tmo = s.sbpool("tmo", [128, 512], F32, 2)
            xts = s.sbpool("xt", [128, D], F32, 2)
            pg_ = s.pspool("pg", [128, 512], F32, 2); pl_ = s.pspool("pl", [128, 512], F32, 2); po_ = s.pspool("po", [128, 512], F32, 3)
            sgs_ = []
            ti = 0
            while ti < NT:
                nt_ = (C // 128) if ti < C // 128 else min(8, NT - ti)
                nt_ = min(nt_, 8)
                sgs_.append((ti, nt_)); ti += nt_
            for e_ in range(NE):
                for q in range(4):
                    wp = wps.next()
                    s.dma("pool", wp[:], w1[l][e_][:, q * 512:(q + 1) * 512].rearrange("(k p) c -> p k c", p=128), writes=[wp])
                    s.dma("sp" if q % 2 == 0 else "act", W1B[l][e_][q], wp[:].rearrange("p k c -> p (k c)"), reads=[wp], writes=[s.db("W1B", e_, q)])
                for q in range(2):
                    wp = wps.next()
                    s.dma("pool", wp[:], w2[l][e_][:, q * 512:(q + 1) * 512].rearrange("(k p) c -> p k c", p=128), writes=[wp])
                    s.dma("sp" if q % 2 == 0 else "act", W2B[l][e_][q], wp[:].rearrange("p k c -> p (k c)"), reads=[wp], writes=[s.db("W2B", e_, q)])
            for (ti0, ntl) in sgs_:
                S_ = ntl * 128; tok0 = ti0 * 128
                r = 1 if tok0 < C else 0
                if r == 1 and l == NL - 1:
                    continue
                s.dma("sp", hts[:, :, 0:S_], H2T[:, tok0:tok0 + S_].rearrange("(k p) t -> p k t", p=128), reads=[s.db("H2T", ti0 + i) for i in range(ntl)], writes=[hts])
                s.dma("act", gts[:, 0:ntl, :], GATE[tok0:tok0 + S_, :].rearrange("(i p) e -> p i e", p=128), reads=[s.db("GATE", ti0 + i) for i in range(ntl)], writes=[gts])
                s.memset("pool", yacc[:], 0.0, [yacc])
                ngs = [(a, min(512, S_ - a)) for a in range(0, S_, 512)]
                for e_ in range(NE):
                    pieces = {}
                    for q in (0, 2, 1, 3):
                        wp = wps.next()
                        s.dma("sp", wp[:].rearrange("p k c -> p (k c)"), W1B[l][e_][q], reads=[s.db("W1B", e_, q)], writes=[wp])
                        pieces[q] = wp
                    w2p = []
                    for q in range(2):
                        wp = wps.next()
                        s.dma("sp", wp[:].rearrange("p k c -> p (k c)"), W2B[l][e_][q], reads=[s.db("W2B", e_, q)], writes=[wp])
                        w2p.append(wp)
                    b2t = b2s.next()
                    s.dma("sp", b2t[:], b2[l][e_:e_ + 1, :].to_broadcast([128, D]), writes=[b2t])
                    pend = None
                    for (a, nn) in ngs:
                        for c in range(8):
                            wg = pieces[c // 4]; wl = pieces[2 + c // 4]; cc_ = c % 4
                            pg = pg_.next(); pl = pl_.next()
                            for k in range(8):
                                s.mm(pg[:, 0:nn], wg[:, k, cc_ * 128:(cc_ + 1) * 128], hts[:, k, a:a + nn], k == 0, k == 7, [wg, hts], [pg])
                            for k in range(8):
                                s.mm(pl[:, 0:nn], wl[:, k, cc_ * 128:(cc_ + 1) * 128], hts[:, k, a:a + nn], k == 0, k == 7, [wl, hts], [pl])
                            g_ = gq.next(); sg_ = sq_.next(); l_ = lq.next(); t_ = tq.next()
                            s.ts("dve", g_[:, 0:nn], pg[:, 0:nn], b1t[:, e_, c:c + 1], 7.0, ALU.add, ALU.min, [pg, b1t], [g_])
                            if pend is not None:
                                pend()
                            s.act(sg_[:, 0:nn], g_[:, 0:nn], AF.Sigmoid, [g_], [sg_], scale=1.702)
                            s.act(l_[:, 0:nn], pl[:, 0:nn], AF.Identity, [pl, b1t], [l_], bias=b1t[:, e_, 8 + c:9 + c])
                            s.ts("pool", l_[:, 0:nn], l_[:, 0:nn], 7.0, -7.0, ALU.min, ALU.max, [l_], [l_])
                            s.tt("pool", t_[:, 0:nn], g_[:, 0:nn], sg_[:, 0:nn], ALU.mult, [g_, sg_], [t_])
                            pend = (lambda c=c, a=a, nn=nn, l_=l_, t_=t_: s.stt("dve", actt[:, c, a:a + nn], l_[:, 0:nn], 1.0, t_[:, 0:nn], ALU.add, ALU.mult, [l_, t_], [actt_b[a // 512]]))
                    pend()
                    for i in range(ntl):
                        for hf in range(2):
                            po = po_.next()
                            for k in range(8):
                                s.mm(po[:], actt[:, k, i * 128:(i + 1) * 128], w2p[hf][:, k, :], k == 0, k == 7, [actt_b[i // 4], w2p[hf]], [po])
                            tm = tmo.next()
                            s.tt("dve", tm[:], po[:], b2t[:, hf * 512:(hf + 1) * 512], ALU.add, [po, b2t], [tm])
                            s.stt("dve", yacc[:, i, hf * 512:(hf + 1) * 512], tm[:], gts[:, i, e_:e_ + 1], yacc[:, i, hf * 512:(hf + 1) * 512], ALU.mult, ALU.add, [tm, gts, yacc], [yacc])
                for i in range(ntl):
                    tok = tok0 + i * 128
                    xt = xts.next()
                    s.dma("sp", xt[:], XR[tok:tok + 128, :], reads=[s.db("XR", ti0 + i)], writes=[xt])
                    s.tt("dve", yacc[:, i, :], yacc[:, i, :], GT2[r][:], ALU.mult, [yacc, GT2[r]], [yacc])
                    s.tt("pool", xt[:], xt[:], yacc[:, i, :], ALU.add, [xt, yacc], [xt])
                    s.dma("sp", XR[tok:tok + 128, :], xt[:], reads=[xt], writes=[s.db("XR", ti0 + i)])
    if stop_after is None:
        with s.phase():
            gfb = s.sb("gfb", [128, D], F32)
            s.dma("sp", gfb[:], gf.rearrange("(o d) -> o d", o=1).to_broadcast([128, D]), writes=[gfb])
            xts = s.sbpool("xt", [128, D], F32, 3)
            junk = s.sb("junk", [128, D], F32)
            sss = s.sbpool("ss", [128, 1], F32, 2)
            for ti in range(C // 128, NT):
                tok = ti * 128
                xt = xts.next()
                s.dma("sp", xt[:], XR[tok:tok + 128, :], reads=[s.db("XR", ti)], writes=[xt])
                ss = sss.next()
                s.act(junk[:], xt[:], AF.Square, [xt], [junk, ss], accum_out=ss[:])
                s.act(ss[:], ss[:], AF.Sqrt, [ss], [ss], scale=1.0 / D, bias=EPS)
                s.recip(ss[:], ss[:], [ss], [ss])
                s.stt("dve", xt[:], xt[:], ss[:, 0:1], gfb[:], ALU.mult, ALU.mult, [xt, ss, gfb], [xt])
                s.dma("act", y_out[tok - C:tok - C + 128, :], xt[:], reads=[xt])
    s.finish()
    s.emit()
    return nc


def kernel(**inputs):
    inp = {k: np.asarray(v) for k, v in inputs.items()}
    B, L, _ = inp["x"].shape
    C = inp["ctx"].shape[1]
    NL = inp["w_ada"].shape[0]
    nc = build(L, C, NL)
    cst = host_consts(L, C)
    in_maps = []
    for b in range(B):
        m = host_layout(inp, b, NL)
        m.update(cst)
        in_maps.append({k: np.ascontiguousarray(v, dtype=np.float32) for k, v in m.items()})
    res = run_bass_kernel_spmd(nc, in_maps, core_ids=list(range(B)))
    return np.stack([np.asarray(r["y"], dtype=np.float32) for r in res.results], 0)
```

```python
from contextlib import ExitStack
import numpy as np
import concourse.bass as bass
import concourse.mybir as mybir
from concourse.bass_utils import run_bass_kernel_spmd

ALU = mybir.AluOpType
AF = mybir.ActivationFunctionType
AX = mybir.AxisListType
F32 = mybir.dt.float32
BF16 = mybir.dt.bfloat16

NSLOT = 12


class Buf:
    __slots__ = ("name", "lw", "rd")

    def __init__(self, name=""):
        self.name = name
        self.lw = None
        self.rd = []


class T:
    def __init__(self, t, b):
        self.t = t
        self.b = b

    def __getitem__(self, k):
        return self.t[k]


class Pool:
    def __init__(self, tiles):
        self.tiles = tiles
        self.i = 0

    def next(self):
        t = self.tiles[self.i % len(self.tiles)]
        self.i += 1
        return t


class Sched:
    def __init__(self, nc):
        self.nc = nc
        self.engs = {"pe": nc.tensor, "act": nc.scalar, "dve": nc.vector, "pool": nc.gpsimd, "sp": nc.sync}
        self.ops = {k: [] for k in self.engs}
        self.cnt = {k: 0 for k in self.engs}
        self.sem = {k: nc.alloc_semaphore("s_" + k) for k in self.engs}
        self.dq = ("sp", "act", "pool")
        self.dsem = {q: [nc.alloc_semaphore("d_%s_%d" % (q, i)) for i in range(NSLOT)] for q in self.dq}
        self.dcnt = {q: 0 for q in self.dq}
        self.known = {k: {} for k in self.engs}
        self.stack = None
        self.uid = 0
        self.dbufs = {}

    def _nm(self, name):
        self.uid += 1
        return "%s_%d" % (name, self.uid)

    def sb(self, name, shape, dtype=F32):
        nm = self._nm(name)
        if self.stack is not None:
            t = self.stack.enter_context(self.nc.sbuf_tensor(nm, list(shape), dtype))
        else:
            t = self.nc.alloc_sbuf_tensor(nm, list(shape), dtype)
        return T(t, Buf(nm))

    def ps(self, name, shape, dtype=F32):
        nm = self._nm(name)
        if self.stack is not None:
            t = self.stack.enter_context(self.nc.psum_tensor(nm, list(shape), dtype))
        else:
            t = self.nc.alloc_psum_tensor(nm, list(shape), dtype)
        return T(t, Buf(nm))

    def sbpool(self, name, shape, dtype, n):
        return Pool([self.sb(name, shape, dtype) for _ in range(n)])

    def pspool(self, name, shape, dtype, n):
        return Pool([self.ps(name, shape, dtype) for _ in range(n)])

    def db(self, *key):
        b = self.dbufs.get(key)
        if b is None:
            b = Buf(str(key))
            self.dbufs[key] = b
        return b

    class _Phase:
        def __init__(self, s):
            self.s = s

        def __enter__(self):
            self.s.stack = ExitStack()
            self.s.stack.__enter__()
            return self

        def __exit__(self, *a):
            self.s.barrier()
            st = self.s.stack
            self.s.stack = None
            st.__exit__(None, None, None)
            return False

    def phase(self):
        return Sched._Phase(self)

    def _need(self, eng, ev, waits):
        if ev is None:
            return
        sem, val, src = ev
        if src == eng and eng == "pe":
            return
        if self.known[eng].get(sem, 0) >= val:
            return
        if waits.get(sem, (None, 0))[1] < val:
            waits[sem] = (sem, val)

    def _deps(self, eng, reads, writes):
        waits = {}
        for b in reads:
            b = b.b if isinstance(b, T) else b
            self._need(eng, b.lw, waits)
        for b in writes:
            b = b.b if isinstance(b, T) else b
            self._need(eng, b.lw, waits)
            for r in b.rd:
                self._need(eng, r, waits)
        for sem, (s_, v) in waits.items():
            self.known[eng][sem] = v
        return list(waits.values())

    def _commit(self, ev, reads, writes):
        for b in reads:
            b = b.b if isinstance(b, T) else b
            b.rd.append(ev)
        for b in writes:
            b = b.b if isinstance(b, T) else b
            b.lw = ev
            b.rd = []

    def op(self, eng, fn, reads=(), writes=()):
        waits = self._deps(eng, reads, writes)
        self.cnt[eng] += 1
        ev = (self.sem[eng], self.cnt[eng], eng)
        self.ops[eng].append((waits, fn, (self.sem[eng], 1)))
        self._commit(ev, reads, writes)

    def dma(self, q, out, in_, reads=(), writes=(), **kw):
        waits = self._deps(q, reads, writes)
        i = self.dcnt[q]
        self.dcnt[q] += 1
        slot = i % NSLOT
        sem = self.dsem[q][slot]
        rnd = i // NSLOT
        if rnd > 0:
            w = {}
            self._need(q, (sem, 16 * rnd, "dma"), w)
            for sem_, (s_, v) in w.items():
                self.known[q][sem_] = v
            waits = waits + list(w.values())
        ev = (sem, 16 * (rnd + 1), "dma")
        fn = lambda e, out=out, in_=in_, kw=kw: e.dma_start(out=out, in_=in_, **kw)
        self.ops[q].append((waits, fn, (sem, 16)))
        self._commit(ev, reads, writes)

    def _all_events(self):
        evs = []
        for k in self.engs:
            if self.cnt[k] > 0:
                evs.append((self.sem[k], self.cnt[k], k))
        for qq in self.dq:
            n = self.dcnt[qq]
            for slot in range(min(n, NSLOT)):
                cnt = (n - slot + NSLOT - 1) // NSLOT
                evs.append((self.dsem[qq][slot], 16 * cnt, "dma"))
        return evs

    def barrier(self):
        evs = self._all_events()
        for eng in self.engs:
            w = {}
            for ev in evs:
                if ev[2] == eng:
                    continue
                self._need(eng, ev, w)
            for sem_, (s_, v) in w.items():
                self.known[eng][sem_] = v
            if w:
                self.ops[eng].append((list(w.values()), None, None))

    def finish(self):
        self.barrier()

    def emit(self):
        nc = self.nc
        with nc.Block() as block:
            def run(name):
                def body(e):
                    for waits, fn, inc in self.ops[name]:
                        for sem, val in waits:
                            e.wait_ge(sem, val)
                        if fn is not None:
                            ins = fn(e)
                            ins.then_inc(inc[0], inc[1])
                return body

            block.tensor(run("pe"))
            block.scalar(run("act"))
            block.vector(run("dve"))
            block.gpsimd(run("pool"))
            block.sync(run("sp"))

    def mm(self, out, lhsT, rhs, start, stop, R, W):
        self.op("pe", lambda e: e.matmul(out, lhsT=lhsT, rhs=rhs, start=start, stop=stop), R, W)

    def tr(self, out, in_, ident, R, W):
        self.op("pe", lambda e: e.transpose(out=out, in_=in_, identity=ident), R, W)

    def act(self, out, in_, func, R, W, **kw):
        self.op("act", lambda e: e.activation(out=out, in_=in_, func=func, **kw), R, W)

    def ts(self, eng, out, in0, s1, s2, op0, op1, R, W):
        if op1 is None:
            self.op(eng, lambda e: e.tensor_scalar(out=out, in0=in0, scalar1=s1, scalar2=None, op0=op0), R, W)
        else:
            self.op(eng, lambda e: e.tensor_scalar(out=out, in0=in0, scalar1=s1, scalar2=s2, op0=op0, op1=op1), R, W)

    def tt(self, eng, out, in0, in1, op, R, W):
        self.op(eng, lambda e: e.tensor_tensor(out=out, in0=in0, in1=in1, op=op), R, W)

    def stt(self, eng, out, in0, scalar, in1, op0, op1, R, W):
        self.op(eng, lambda e: e.scalar_tensor_tensor(out=out, in0=in0, scalar=scalar, in1=in1, op0=op0, op1=op1), R, W)

    def cp(self, eng, out, in_, R, W):
        if eng == "act":
            self.op("act", lambda e: e.copy(out=out, in_=in_), R, W)
        else:
            self.op(eng, lambda e: e.tensor_copy(out=out, in_=in_), R, W)

    def recip(self, out, in_, R, W):
        self.op("dve", lambda e: e.reciprocal(out=out, in_=in_), R, W)

    def memset(self, eng, ap, val, W):
        self.op(eng, lambda e: e.memset(ap, val), (), W)


CUT = 99

D = 1024
NH = 4
EPS = 1e-6
K_SCALE = 32 ** -0.5
ATT_SCALE = 192 ** -0.5
NE = 32
SENT = {}


def host_consts(L, C):
    TT = C + L
    f32 = np.float32
    cst = {}
    cst["ident"] = np.eye(128, dtype=f32)
    rows = L // 64
    row = np.repeat(np.arange(rows), 64).astype(f32)
    col = np.tile(np.arange(64), rows).astype(f32)
    freq = (f32(10000.0) ** (-np.arange(16, dtype=f32) / f32(16))).astype(f32)
    ang = np.concatenate([row[:, None] * freq, col[:, None] * freq], axis=-1).astype(f32)
    cos = np.ones((TT, 32), f32)
    sin = np.zeros((TT, 32), f32)
    cos[C:] = np.cos(ang)
    sin[C:] = np.sin(ang)
    cst["mc"] = np.ascontiguousarray(np.concatenate([cos, cos], 1).T)
    cst["ms"] = np.ascontiguousarray(np.concatenate([-sin, sin], 1).T)
    theta = (1.0 / (f32(10000.0) ** np.linspace(0.0, 1.0, 16, dtype=f32))).astype(f32)
    a2 = (np.arange(TT, dtype=f32)[:, None] * theta).astype(f32)
    cst["rcs"] = np.ascontiguousarray(np.concatenate([np.cos(a2), np.sin(a2)], 1).astype(f32))
    j = np.arange(128)[:, None]
    i = np.arange(128)[None, :]
    dm = np.zeros((128, 5, 128), f32)
    dm[:, 0] = np.maximum(i - j, 0)
    dm[:, 1] = np.maximum(j - i, 0)
    dm[:, 2] = (i > j)
    dm[:, 3] = (j > i)
    dm[:, 4] = 2.0 * (i == j)
    cst["dm"] = dm
    jj = np.arange(128, dtype=f32)
    cst["pcol"] = np.stack([127 - jj, jj, jj + 1, 128 - jj], 1).astype(f32)
    ii = np.arange(128, dtype=f32)
    cst["irow"] = np.ascontiguousarray(np.broadcast_to(np.stack([ii + 1, 128 - ii], 0)[None], (64, 2, 128)).astype(f32))
    return cst


def col(v, p=128):
    v = np.asarray(v)
    sh = v.shape
    return np.ascontiguousarray(v.reshape(sh[:-1] + (sh[-1] // p, p)).swapaxes(-1, -2))


def host_layout(inp, b, NL):
    m = {}
    m["x"] = np.ascontiguousarray(inp["x"][b])
    m["ctx"] = np.ascontiguousarray(inp["ctx"][b])
    cv = np.stack([inp["c"][b], inp["c_ctx"]], 0)
    m["ccol"] = np.ascontiguousarray(col(cv).transpose(1, 2, 0).reshape(128, 16))
    m["w_ada"] = inp["w_ada"]
    m["b_ada"] = inp["b_ada"]
    m["b_adac"] = col(inp["b_ada"])
    m["g1c"] = col(inp["g_norm1"])
    m["g2"] = inp["g_norm2"]
    m["w_in"] = inp["w_in"]
    m["w_out"] = inp["w_out"]
    cw = inp["conv_w"]
    m["convw"] = np.ascontiguousarray(cw.reshape(NL, 31, 2, 128).transpose(0, 3, 2, 1))
    m["convp"] = np.ascontiguousarray(np.stack([col(inp["conv_b"]), col(inp["conv_ln_g"]), col(inp["conv_ln_b"])], -1))
    m["qng"] = col(inp["mla_q_norm"])
    m["kvng"] = col(inp["mla_kv_norm"])
    m["w_uq"] = inp["mla_w_uq"]
    m["w_ukv"] = inp["mla_w_ukv"]
    dec = np.concatenate([inp["ret_decay_fwd"], inp["ret_decay_bwd"]], -1)
    m["dec"] = np.ascontiguousarray(dec)
    m["router_w"] = inp["router_w"]
    m["router_b"] = inp["router_b"]
    m["w1"] = inp["moe_w1"]
    m["w2"] = inp["moe_w2"]
    m["b1c"] = np.ascontiguousarray(col(inp["moe_b1"]).transpose(0, 2, 1, 3))
    m["b2"] = inp["moe_b2"]
    m["gf"] = inp["g_final"]
    return m


def build(L, C, NL=2, dbg=(), stop_after=None):
    TT = C + L
    NT = TT // 128
    nc = bass.Bass("TRN2", target_bir_lowering=False)
    s = Sched(nc)

    def din(name, shape):
        return nc.dram_tensor(name, list(shape), F32, kind="ExternalInput").ap()

    x_in = din("x", [L, D]); ctx_in = din("ctx", [C, D]); ccol = din("ccol", [128, 16])
    w_ada = din("w_ada", [NL, D, 6 * D]); b_ada = din("b_ada", [NL, 6 * D]); b_adac = din("b_adac", [NL, 128, 48])
    g1c = din("g1c", [NL, 128, 8]); g2 = din("g2", [NL, D])
    w_in = din("w_in", [NL, D, 1984]); w_out = din("w_out", [NL, D, D])
    convw = din("convw", [NL, 128, 2, 31]); convp = din("convp", [NL, 128, 2, 3])
    qng = din("qng", [NL, 128, 3]); kvng = din("kvng", [NL, 128, 2])
    w_uq = din("w_uq", [NL, 384, 768]); w_ukv = din("w_ukv", [NL, 256, 1024])
    dec = din("dec", [NL, 8])
    router_w = din("router_w", [NL, D, NE]); router_b = din("router_b", [NL, NE])
    w1 = din("w1", [NL, NE, D, 2048]); w2 = din("w2", [NL, NE, D, D])
    b1c = din("b1c", [NL, 128, NE, 16]); b2 = din("b2", [NL, NE, D]); gf = din("gf", [D])
    ident_d = din("ident", [128, 128]); mc_d = din("mc", [64, TT]); ms_d = din("ms", [64, TT])
    rcs_d = din("rcs", [TT, 32]); dm_d = din("dm", [128, 5, 128]); pcol_d = din("pcol", [128, 4]); irow_d = din("irow", [64, 2, 128])

    def scratch(name, shape, dt):
        kind = "ExternalOutput" if name in dbg else "Internal"
        return nc.dram_tensor(name, list(shape), dt, kind=kind).ap()

    y_out = nc.dram_tensor("y", [L, D], F32, kind="ExternalOutput").ap()
    XR = scratch("XR", [TT, D], F32)
    MOD = scratch("MOD", [NL, 2, 6 * D], F32)
    QN = scratch("QN", [NH, 128, TT], BF16); QR = scratch("QR", [NH, 64, TT], BF16)
    KN = scratch("KN", [NH, 128, TT], BF16); KR = scratch("KR", [64, TT], BF16)
    VV = scratch("VV", [TT, 512], BF16)
    RQT = scratch("RQT", [2, 64, TT], BF16); RKT = scratch("RKT", [2, 64, TT], BF16)
    RK = scratch("RK", [TT, 128], BF16); RV = scratch("RV", [TT, 256], BF16); SG = scratch("SG", [TT, 256], F32)
    MIXT = scratch("MIXT", [D, TT], BF16)
    H2T = scratch("H2T", [D, TT], BF16)
    GATE = scratch("GATE", [TT, NE], F32)
    ZTD = scratch("ZT", [128, 2, TT], BF16)
    W1B = scratch("W1B", [NL, NE, 4, 128, 4096], BF16)
    W2B = scratch("W2B", [NL, NE, 2, 128, 4096], BF16)

    groups = []
    t = 0
    while t < C:
        n = min(512, C - t); groups.append((t, n, 1)); t += n
    while t < TT:
        n = min(512, TT - t); groups.append((t, n, 0)); t += n

    ident_f = s.sb("ident_f", [128, 128], F32)
    ident_b = s.sb("ident_b", [128, 128], BF16)
    ones_b = s.sb("ones_b", [128, 128], BF16)
    s.dma("sp", ident_f[:], ident_d, writes=[ident_f])
    s.dma("pool", ident_b[:], ident_d, writes=[ident_b])
    s.memset("dve", ones_b[:], 1.0, [ones_b])
    ones_f = s.sb("ones_f", [128, 128], F32)
    s.memset("dve", ones_f[:], 1.0, [ones_f])
    modc = s.sb("modc", [128, NL, 48, 2], F32)
    a1c = s.sb("a1c", [128, NL, 2, 8], F32)
    b1c_ = s.sb("b1c_", [128, NL, 2, 8], F32)
    dtsum = s.sb("dtsum", [128, NL, NH, 128], BF16)
    kqdec = s.sb("kqdec", [128, NL, 4, NH], F32)
    cdec = s.sb("cdec", [64, NL, 2, 2], F32)

    with s.phase():
        cc = s.sb("cc", [128, 16], F32)
        scol = s.sb("scol", [128, 16], F32)
        s.dma("sp", cc[:], ccol, writes=[cc])
        s.act(scol[:], cc[:], AF.Sigmoid, [cc], [scol])
        s.tt("dve", scol[:], scol[:], cc[:], ALU.mult, [scol, cc], [scol])
        scv = scol[:].rearrange("p (k r) -> p k r", r=2)
        bac = s.sb("bac", [128, NL, 48], F32)
        for l in range(NL):
            s.dma("sp", bac[:, l, :], b_adac[l], writes=[bac])
        wts = s.sbpool("wada", [128, 8, 512], F32, 2)
        brow = s.sbpool("brow", [2, 512], F32, 2)
        mrow = s.sbpool("mrow", [2, 512], F32, 2)
        pr = s.pspool("pr", [2, 512], F32, 2)
        pc = s.pspool("pc", [128, 4, 2], F32, 2)
        for l in range(NL):
            for n in range(12):
                wt = wts.next()
                s.dma("sp" if n % 2 == 0 else "act", wt[:], w_ada[l][:, n * 512:(n + 1) * 512].rearrange("(k p) c -> p k c", p=128), writes=[wt])
                br = brow.next()
                s.dma("sp", br[:], b_ada[l:l + 1, n * 512:(n + 1) * 512].to_broadcast([2, 512]), writes=[br])
                p = pr.next()
                for k in range(8):
                    s.mm(p[:], scv[:, k, :], wt[:, k, :], k == 0, k == 7, [scol, wt], [p])
                mr = mrow.next()
                s.tt("dve", mr[:], p[:], br[:], ALU.add, [p, br], [mr])
                s.dma("sp", MOD[l][:, n * 512:(n + 1) * 512], mr[:], reads=[mr], writes=[s.db("MOD")])
                pcc = pc.next()
                for j in range(4):
                    for k in range(8):
                        s.mm(pcc[:, j, :], wt[:, k, j * 128:(j + 1) * 128], scv[:, k, :], k == 0, k == 7, [scol, wt], [pcc])
                s.tt("dve", modc[:, l, n * 4:(n + 1) * 4, :], pcc[:], bac[:, l, n * 4:(n + 1) * 4].unsqueeze(2).to_broadcast([128, 4, 2]), ALU.add, [pcc, bac], [modc])
            g1t = s.sb("g1t", [128, 8], F32)
            s.dma("sp", g1t[:], g1c[l], writes=[g1t])
            for r in range(2):
                s.ts("dve", a1c[:, l, r, :], modc[:, l, 8:16, r], 1.0, None, ALU.add, None, [modc], [a1c])
                s.tt("dve", a1c[:, l, r, :], a1c[:, l, r, :], g1t[:], ALU.mult, [a1c, g1t], [a1c])
                s.cp("dve", b1c_[:, l, r, :], modc[:, l, 0:8, r], [modc], [b1c_])
        dmt = s.sb("dmt", [128, 5, 128], F32)
        pcl = s.sb("pcl", [128, 4], F32)
        s.dma("sp", dmt[:], dm_d, writes=[dmt])
        s.dma("sp", pcl[:], pcol_d, writes=[pcl])
        for l in range(NL):
            dcb = s.sb("dcb", [128, 8], F32)
            s.dma("sp", dcb[:], dec[l:l + 1, :].to_broadcast([128, 8]), writes=[dcb])
            lg = s.sb("lg", [128, 8], F32)
            s.act(lg[:], dcb[:], AF.Exp, [dcb], [lg], scale=-1.0)
            s.act(lg[:], lg[:], AF.Ln, [lg], [lg], bias=1.0)
            s.ts("dve", lg[:], lg[:], -1.0, None, ALU.mult, None, [lg], [lg])
            for h in range(NH):
                ef = s.sb("ef", [128, 128], F32)
                eb = s.sb("eb", [128, 128], F32)
                s.act(ef[:], dmt[:, 0, :], AF.Exp, [dmt, lg], [ef], scale=lg[:, h:h + 1])
                s.act(eb[:], dmt[:, 1, :], AF.Exp, [dmt, lg], [eb], scale=lg[:, 4 + h:5 + h])
                s.tt("dve", ef[:], ef[:], dmt[:, 2, :], ALU.mult, [ef, dmt], [ef])
                s.tt("dve", eb[:], eb[:], dmt[:, 3, :], ALU.mult, [eb, dmt], [eb])
                s.tt("dve", ef[:], ef[:], eb[:], ALU.add, [ef, eb], [ef])
                s.tt("dve", dtsum[:, l, h, :], ef[:], dmt[:, 4, :], ALU.add, [ef, dmt], [dtsum])
            s.act(kqdec[:, l, 0, :], lg[:, 0:4], AF.Exp, [lg, pcl], [kqdec], scale=pcl[:, 0:1])
            s.act(kqdec[:, l, 1, :], lg[:, 4:8], AF.Exp, [lg, pcl], [kqdec], scale=pcl[:, 1:2])
            s.act(kqdec[:, l, 2, :], lg[:, 0:4], AF.Exp, [lg, pcl], [kqdec], scale=pcl[:, 2:3])
            s.act(kqdec[:, l, 3, :], lg[:, 4:8], AF.Exp, [lg, pcl], [kqdec], scale=pcl[:, 3:4])
            cd = s.sb("cd", [64, 2, 2], F32)
            for dr in range(2):
                for pp in range(2):
                    for hl in range(2):
                        h = 2 * pp + hl
                        s.cp("dve", cd[32 * hl:32 * hl + 32, dr, pp:pp + 1], lg[32 * hl:32 * hl + 32, dr * 4 + h:dr * 4 + h + 1], [lg], [cd])
            s.act(cdec[:, l, :, :], cd[:], AF.Exp, [cd], [cdec], scale=128.0)
    SENT["s"] = s
    if stop_after == 0:
        s.finish(); s.emit(); return nc

    for l in range(NL):
        with s.phase():
            win = s.sb("win", [128, 8, 1984], BF16)
            for k in range(8):
                s.dma("pool", win[:, k, :], w_in[l][k * 128:(k + 1) * 128, :], writes=[win])
            winsw = s.sb("winsw", [128, 8, 64], BF16)
            wv = w_in[l].rearrange("(k p) c -> p k c", p=128)
            s.dma("pool", winsw[:, :, 0:32], wv[:, :, 1568:1600], writes=[winsw])
            s.dma("pool", winsw[:, :, 32:64], wv[:, :, 1536:1568], writes=[winsw])
            wuq = s.sb("wuq", [128, 3, 768], BF16)
            s.dma("pool", wuq[:], w_uq[l].rearrange("(k p) c -> p k c", p=128), writes=[wuq])
            wuqsw = s.sb("wuqsw", [128, 3, NH, 64], BF16)
            wq4 = w_uq[l].rearrange("(k p) (h c) -> p k h c", p=128, h=NH)
            for h_ in range(NH):
                s.dma("pool", wuqsw[:, :, h_, 0:32], wq4[:, :, h_, 160:192], writes=[wuqsw])
                s.dma("pool", wuqsw[:, :, h_, 32:64], wq4[:, :, h_, 128:160], writes=[wuqsw])
            wukv = s.sb("wukv", [128, 2, 1024], BF16)
            s.dma("pool", wukv[:], w_ukv[l].rearrange("(k p) c -> p k c", p=128), writes=[wukv])
            wukv4 = wukv[:].rearrange("p k (h c) -> p k h c", h=NH)
            qg_t = s.sb("qg_t", [128, 3], F32); kvg_t = s.sb("kvg_t", [128, 2], F32)
            s.dma("sp", qg_t[:], qng[l], writes=[qg_t]); s.dma("sp", kvg_t[:], kvng[l], writes=[kvg_t])

            xts = s.sbpool("xt", [128, D], F32, 2)
            junk = s.sb("junk", [128, D], F32)
            sss = s.sbpool("ss", [128, 1], F32, 2)
            xns = s.sbpool("xn", [128, D], BF16, 2)
            pTs = s.pspool("pT", [128, 8, 128], BF16, 2)
            pms = s.pspool("pm", [128, 512], F32, 6)
            hTs = s.sbpool("hT", [128, 8, 512], BF16, 2)
            sgs = s.sbpool("sg", [128, 512], F32, 2)
            cqs = s.sb("cq", [128, 3, 512], F32)
            sqs = s.sb("sq", [128, 3, 512], BF16)
            cqn = s.sb("cqn", [128, 3, 512], BF16)
            rstd = s.sbpool("rstd", [128, 512], F32, 2)
            obs = s.sbpool("ob", [128, 512], BF16, 3)
            mct = s.sb("mct", [64, 512], F32); mst = s.sb("mst", [64, 512], F32)
            r1s = s.sbpool("r1", [64, 512], F32, 2); r2s = s.sbpool("r2", [64, 512], F32, 2)
            sqg = s.sbpool("sqg", [128, 384], F32, 2); skv = s.sbpool("skv", [128, 384], F32, 2)
            rcst = s.sbpool("rcst", [128, 32], F32, 2)
            tmpr = s.sbpool("tmpr", [128, 4, 4, 16], F32, 2)
            rot = s.sbpool("rot", [128, 2, 128], BF16, 2)
            rts = s.sbpool("rts", [64, 4, 128], BF16, 2)
            rvs = s.sbpool("rvs", [128, 256], BF16, 2)
            sgo = s.sbpool("sgo", [128, 256], F32, 2)
            zq = s.sbpool("zq", [128, 2, 512], BF16, 2)

            for (t0, n, r) in groups:
                nt = n // 128
                hT = hTs.next()
                for i in range(nt):
                    tok = t0 + i * 128
                    xt = xts.next()
                    if l == 0:
                        src = ctx_in[tok:tok + 128, :] if r == 1 else x_in[tok - C:tok - C + 128, :]
                        s.dma("sp", xt[:], src, writes=[xt])
                        s.dma("act", XR[tok:tok + 128, :], xt[:], reads=[xt], writes=[s.db("XR", tok // 128)])
                    else:
                        s.dma("sp", xt[:], XR[tok:tok + 128, :], reads=[s.db("XR", tok // 128)], writes=[xt])
                    ss = sss.next()
                    s.act(junk[:], xt[:], AF.Square, [xt], [junk, ss], accum_out=ss[:])
                    s.act(ss[:], ss[:], AF.Sqrt, [ss], [ss], scale=1.0 / D, bias=EPS)
                    s.recip(ss[:], ss[:], [ss], [ss])
                    xn = xns.next()
                    s.act(xn[:], xt[:], AF.Copy, [xt, ss], [xn], scale=ss[:])
                    pT = pTs.next()
                    for k in range(8):
                        s.tr(pT[:, k, :], xn[:, k * 128:(k + 1) * 128], ident_b[:], [xn, ident_b], [pT])
                    for k in range(8):
                        if k % 2 == 0:
                            s.ts("dve", hT[:, k, i * 128:(i + 1) * 128], pT[:, k, :], a1c[:, l, r, k:k + 1], b1c_[:, l, r, k:k + 1], ALU.mult, ALU.add, [pT, a1c, b1c_], [hT])
                        else:
                            s.act(hT[:, k, i * 128:(i + 1) * 128], pT[:, k, :], AF.Identity, [pT, a1c, b1c_], [hT], scale=a1c[:, l, r, k:k + 1], bias=b1c_[:, l, r, k:k + 1])

                def proj(cols, M):
                    p = pms.next()
                    for k in range(8):
                        s.mm(p[0:M, 0:n], cols(k), hT[:, k, 0:n], k == 0, k == 7, [hT, win, winsw], [p])
                    return p

                if CUT <= 1: continue
                z = zq.next()
                for c in range(2):
                    pa = proj(lambda k: win[:, k, c * 128:(c + 1) * 128], 128)
                    pg = proj(lambda k: win[:, k, 256 + c * 128:256 + (c + 1) * 128], 128)
                    sg = sgs.next()
                    s.act(sg[:, 0:n], pg[:, 0:n], AF.Sigmoid, [pg], [sg])
                    s.tt("dve", z[:, c, 0:n], pa[:, 0:n], sg[:, 0:n], ALU.mult, [pa, sg], [z])
                s.dma("sp", ZTD[:, :, t0:t0 + n], z[:, :, 0:n], reads=[z], writes=[s.db("ZT", t0)])

                if CUT <= 2: continue
                def featnorm(c0, nk, gt, outn, raw):
                    for k in range(nk):
                        p = proj(lambda kk: win[:, kk, c0 + k * 128:c0 + (k + 1) * 128], 128)
                        s.cp("act", raw[:, k, 0:n], p[:, 0:n], [p], [raw])
                        s.tt("pool", sqs[:, k, 0:n], raw[:, k, 0:n], raw[:, k, 0:n], ALU.mult, [raw], [sqs])
                    pss = pms.next()
                    for k in range(nk):
                        s.mm(pss[:, 0:n], ones_b[:], sqs[:, k, 0:n], k == 0, k == nk - 1, [ones_b, sqs], [pss])
                    rs = rstd.next()
                    s.act(rs[:, 0:n], pss[:, 0:n], AF.Sqrt, [pss], [rs], scale=1.0 / (nk * 128), bias=EPS)
                    s.recip(rs[:, 0:n], rs[:, 0:n], [rs], [rs])
                    for k in range(nk):
                        s.stt("dve", outn[:, k, 0:n], raw[:, k, 0:n], gt[:, k:k + 1], rs[:, 0:n], ALU.mult, ALU.mult, [raw, gt, rs], [outn])

                featnorm(512, 3, qg_t, cqn, cqs)
                s.dma("sp", mct[:, 0:n], mc_d[:, t0:t0 + n], writes=[mct])
                s.dma("sp", mst[:, 0:n], ms_d[:, t0:t0 + n], writes=[mst])

                def rope_fm(p_main, p_sw, dst, dkey):
                    r1 = r1s.next(); r2 = r2s.next()
                    s.tt("dve", r1[:, 0:n], p_main[0:64, 0:n], mct[:, 0:n], ALU.mult, [p_main, mct], [r1])
                    s.tt("dve", r2[:, 0:n], p_sw[0:64, 0:n], mst[:, 0:n], ALU.mult, [p_sw, mst], [r2])
                    ob = obs.next()
                    s.tt("pool", ob[0:64, 0:n], r1[:, 0:n], r2[:, 0:n], ALU.add, [r1, r2], [ob])
                    s.dma("sp", dst, ob[0:64, 0:n], reads=[ob], writes=[dkey])

                for h in range(NH):
                    p = pms.next()
                    for k in range(3):
                        s.mm(p[:, 0:n], wuq[:, k, h * 192:h * 192 + 128], cqn[:, k, 0:n], k == 0, k == 2, [wuq, cqn], [p])
                    ob = obs.next()
                    s.cp("act", ob[:, 0:n], p[:, 0:n], [p], [ob])
                    s.dma("sp", QN[h][:, t0:t0 + n], ob[:, 0:n], reads=[ob], writes=[s.db("QN", h, t0)])
                    p1 = pms.next(); p2 = pms.next()
                    for k in range(3):
                        s.mm(p1[0:64, 0:n], wuq[:, k, h * 192 + 128:h * 192 + 192], cqn[:, k, 0:n], k == 0, k == 2, [wuq, cqn], [p1])
                    for k in range(3):
                        s.mm(p2[0:64, 0:n], wuqsw[:, k, h, :], cqn[:, k, 0:n], k == 0, k == 2, [wuqsw, cqn], [p2])
                    rope_fm(p1, p2, QR[h][:, t0:t0 + n], s.db("QR", h, t0))

                if CUT <= 3: continue
                featnorm(1280, 2, kvg_t, cqn, cqs)
                for h in range(NH):
                    p = pms.next()
                    for k in range(2):
                        s.mm(p[:, 0:n], wukv4[:, k, h, 0:128], cqn[:, k, 0:n], k == 0, k == 1, [wukv, cqn], [p])
                    ob = obs.next()
                    s.cp("act", ob[:, 0:n], p[:, 0:n], [p], [ob])
                    s.dma("sp", KN[h][:, t0:t0 + n], ob[:, 0:n], reads=[ob], writes=[s.db("KN", h, t0)])
                for i in range(nt):
                    p = pms.next()
                    for h in range(NH):
                        for k in range(2):
                            s.mm(p[:, h * 128:(h + 1) * 128], cqn[:, k, i * 128:(i + 1) * 128], wukv4[:, k, h, 128:256], k == 0, k == 1, [wukv, cqn], [p])
                    ob = obs.next()
                    s.cp("act", ob[:], p[:], [p], [ob])
                    s.dma("sp", VV[t0 + i * 128:t0 + (i + 1) * 128, :], ob[:], reads=[ob], writes=[s.db("VV", (t0 + i * 128) // 128)])
                if CUT <= 4: continue
                p1 = proj(lambda k: win[:, k, 1536:1600], 64)
                p2 = proj(lambda k: winsw[:, k, :], 64)
                rope_fm(p1, p2, KR[:, t0:t0 + n], s.db("KR", t0))
                if CUT <= 5: continue
                for i in range(nt):
                    tok = t0 + i * 128
                    pq = pms.next(); pk = pms.next()
                    for k in range(8):
                        s.mm(pq[:, 0:384], hT[:, k, i * 128:(i + 1) * 128], win[:, k, 896:1280], k == 0, k == 7, [hT, win], [pq])
                    for k in range(8):
                        s.mm(pk[:, 0:384], hT[:, k, i * 128:(i + 1) * 128], win[:, k, 1600:1984], k == 0, k == 7, [hT, win], [pk])
                    a = sqg.next(); b = skv.next()
                    s.cp("act", a[:, 0:128], pq[:, 0:128], [pq], [a])
                    s.act(b[:, 0:128], pk[:, 0:128], AF.Identity, [pk], [b], scale=K_SCALE)
                    so = sgo.next()
                    s.act(so[:], pq[:, 128:384], AF.Sigmoid, [pq], [so])
                    s.tt("dve", so[:], so[:], pq[:, 128:384], ALU.mult, [so, pq], [so])
                    s.dma("act", SG[tok:tok + 128, :], so[:], reads=[so], writes=[s.db("SG", tok // 128)])
                    rv_ = rvs.next()
                    s.cp("dve", rv_[:], pk[:, 128:384], [pk], [rv_])
                    s.dma("act", RV[tok:tok + 128, :], rv_[:], reads=[rv_], writes=[s.db("RV", tok // 128)])
                    if CUT <= 6: continue
                    cs = rcst.next()
                    s.dma("act", cs[:], rcs_d[tok:tok + 128, :], writes=[cs])
                    cosb = cs[:, 0:16].unsqueeze(1).to_broadcast([128, 4, 16])
                    sinb = cs[:, 16:32].unsqueeze(1).to_broadcast([128, 4, 16])
                    ro = rot.next()
                    tm = tmpr.next()
                    for qi, srcT in enumerate((a, b)):
                        v4 = srcT[:, 0:128].rearrange("p (h two d) -> p h two d", h=4, two=2)
                        o4 = ro[:, qi, :].rearrange("p (h two d) -> p h two d", h=4, two=2)
                        x1 = v4[:, :, 0, :]; x2 = v4[:, :, 1, :]
                        s.tt("dve", tm[:, :, 0, :], x1, cosb, ALU.mult, [srcT, cs], [tm])
                        s.tt("dve", tm[:, :, 1, :], x2, sinb, ALU.mult, [srcT, cs], [tm])
                        s.tt("dve", tm[:, :, 2, :], x2, cosb, ALU.mult, [srcT, cs], [tm])
                        s.tt("dve", tm[:, :, 3, :], x1, sinb, ALU.mult, [srcT, cs], [tm])
                        s.tt("dve", o4[:, :, 0, :], tm[:, :, 0, :], tm[:, :, 1, :], ALU.subtract, [tm], [ro])
                        s.tt("dve", o4[:, :, 1, :], tm[:, :, 2, :], tm[:, :, 3, :], ALU.add, [tm], [ro])
                    if CUT <= 7: continue
                    s.dma("sp", RK[tok:tok + 128, :], ro[:, 1, :], reads=[ro], writes=[s.db("RK", tok // 128)])
                    pT = pTs.next()
                    for qi in range(2):
                        for pp in range(2):
                            s.tr(pT[0:64, qi * 2 + pp, :], ro[:, qi, pp * 64:(pp + 1) * 64], ident_b[:], [ro, ident_b], [pT])
                    rt = rts.next()
                    s.cp("dve", rt[:], pT[0:64, 0:4, :], [pT], [rt])
                    s.dma("sp", RQT[:, :, tok:tok + 128].rearrange("a p t -> p a t"), rt[:, 0:2, :], reads=[rt], writes=[s.db("RQT", tok // 128)])
                    s.dma("sp", RKT[:, :, tok:tok + 128].rearrange("a p t -> p a t"), rt[:, 2:4, :], reads=[rt], writes=[s.db("RKT", tok // 128)])
        if stop_after == "A":
            break
        with s.phase():
            cwt = s.sb("cwt", [128, 2, 31], F32); cpt = s.sb("cpt", [128, 2, 3], F32)
            s.dma("sp", cwt[:], convw[l], writes=[cwt]); s.dma("sp", cpt[:], convp[l], writes=[cpt])
            zts = s.sbpool("zt", [128, 2, 512 + 30], BF16, 2)
            accs = s.sbpool("acc", [128, 2, 512], F32, 2)
            ybs = s.sbpool("yb", [128, 2, 512], BF16, 2)
            yqs = s.sbpool("yq", [128, 2, 512], BF16, 2)
            pS = s.pspool("pS", [128, 512], F32, 2); pQ = s.pspool("pQ", [128, 512], F32, 2)
            mean = s.sbpool("mean", [128, 512], F32, 2); var = s.sbpool("var", [128, 512], F32, 2)
            msq = s.sbpool("msq", [128, 512], F32, 2)
            dd = s.sbpool("dd", [128, 512], F32, 2)
            oc = s.sbpool("oc", [128, 512], BF16, 3)
            for (t0, n, r) in groups:
                lo, hi = (0, C) if r == 1 else (C, TT)
                zt = zts.next()
                s.memset("pool", zt[:], 0.0, [zt])
                a0 = max(lo, t0 - 15); a1 = min(hi, t0 + n + 15)
                s.dma("sp", zt[:, :, a0 - (t0 - 15):a1 - (t0 - 15)], ZTD[:, :, a0:a1], reads=[s.db("ZT", g[0]) for g in groups], writes=[zt])
                acc = accs.next(); yb = ybs.next(); yq = yqs.next()
                for c in range(2):
                    eng = "dve"
                    s.ts(eng, acc[:, c, 0:n], zt[:, c, 0:n], cwt[:, c, 0:1], cpt[:, c, 0:1], ALU.mult, ALU.add, [zt, cwt, cpt], [acc])
                    for k in range(1, 31):
                        s.stt(eng, acc[:, c, 0:n], zt[:, c, k:k + n], cwt[:, c, k:k + 1], acc[:, c, 0:n], ALU.mult, ALU.add, [zt, cwt, acc], [acc])
                    s.cp("act", yb[:, c, 0:n], acc[:, c, 0:n], [acc], [yb])
                    s.act(yq[:, c, 0:n], acc[:, c, 0:n], AF.Square, [acc], [yq])
                ps_ = pS.next(); pq_ = pQ.next()
                for c in range(2):
                    s.mm(ps_[:, 0:n], ones_b[:], yb[:, c, 0:n], c == 0, c == 1, [ones_b, yb], [ps_])
                for c in range(2):
                    s.mm(pq_[:, 0:n], ones_b[:], yq[:, c, 0:n], c == 0, c == 1, [ones_b, yq], [pq_])
                mn = mean.next(); vr = var.next(); mq = msq.next()
                s.ts("dve", mn[:, 0:n], ps_[:, 0:n], 1.0 / 256, None, ALU.mult, None, [ps_], [mn])
                s.tt("pool", mq[:, 0:n], mn[:, 0:n], mn[:, 0:n], ALU.mult, [mn], [mq])
                s.stt("dve", vr[:, 0:n], pq_[:, 0:n], 1.0 / 256, mq[:, 0:n], ALU.mult, ALU.subtract, [pq_, mq], [vr])
                s.ts("dve", vr[:, 0:n], vr[:, 0:n], 0.0, None, ALU.max, None, [vr], [vr])
                s.act(vr[:, 0:n], vr[:, 0:n], AF.Sqrt, [vr], [vr], bias=1e-5)
                s.recip(vr[:, 0:n], vr[:, 0:n], [vr], [vr])
                for c in range(2):
                    d_ = dd.next()
                    s.tt("pool", d_[:, 0:n], acc[:, c, 0:n], mn[:, 0:n], ALU.subtract, [acc, mn], [d_])
                    s.tt("pool", d_[:, 0:n], d_[:, 0:n], vr[:, 0:n], ALU.mult, [d_, vr], [d_])
                    o = oc.next()
                    s.act(d_[:, 0:n], d_[:, 0:n], AF.Identity, [d_, cpt], [d_], scale=cpt[:, c, 1:2], bias=cpt[:, c, 2:3])
                    sg2 = msq.next()
                    s.act(sg2[:, 0:n], d_[:, 0:n], AF.Sigmoid, [d_], [sg2])
                    s.tt("dve", o[:, 0:n], d_[:, 0:n], sg2[:, 0:n], ALU.mult, [d_, sg2], [o])
                    s.dma("sp", MIXT[c * 128:(c + 1) * 128, t0:t0 + n], o[:, 0:n], reads=[o], writes=[s.db("MIXT", c, t0)])
        with s.phase():
            NCH = NT
            Sst = s.sb("Sst", [64, 2, 2, NCH, 64], BF16)
            Sf = [[s.sb("Sf", [64, 128], F32) for pp in range(2)] for dr in range(2)]
            for dr in range(2):
                for pp in range(2):
                    s.memset("dve", Sf[dr][pp][:], 0.0, [Sf[dr][pp]])
            qd = s.sb("qd", [64, 2, 2, 128], F32)
            irow = s.sb("irow", [64, 2, 128], F32)
            s.dma("sp", irow[:], irow_d, writes=[irow])
            cdl = s.sb("cdl", [64, 2, 2], F32)
            s.act(cdl[:], cdec[:, l, :, :], AF.Ln, [cdec], [cdl], scale=1.0)
            s.ts("dve", cdl[:], cdl[:], 1.0 / 128, None, ALU.mult, None, [cdl], [cdl])
            for dr in range(2):
                for pp in range(2):
                    s.act(qd[:, dr, pp, :], irow[:, dr, :], AF.Exp, [irow, cdl], [qd], scale=cdl[:, dr, pp:pp + 1])
            rkt = s.sbpool("rkt", [128, 128], BF16, 3); rvt = s.sbpool("rvt", [128, 256], BF16, 3)
            kds = s.sbpool("kd", [128, 128], BF16, 3)
            pkv = s.pspool("pkv", [64, 128], F32, 2)
            nchc = C // 128
            order_f = list(range(NCH))
            order_b = list(range(nchc - 1, -1, -1)) + list(range(NCH - 1, nchc - 1, -1))
            for dr, order in ((0, order_f), (1, order_b)):
                for ci in order:
                    tok = ci * 128
                    rk_ = rkt.next(); rv_ = rvt.next()
                    s.dma("sp", rk_[:], RK[tok:tok + 128, :], reads=[s.db("RK", ci)], writes=[rk_])
                    s.dma("act", rv_[:], RV[tok:tok + 128, :], reads=[s.db("RV", ci)], writes=[rv_])
                    kd = kds.next()
                    s.tt("pool", kd[:].rearrange("p (h d) -> p h d", h=4), rk_[:].rearrange("p (h d) -> p h d", h=4),
                         kqdec[:, l, dr, :].unsqueeze(2).to_broadcast([128, 4, 32]), ALU.mult, [rk_, kqdec], [kd])
                    for pp in range(2):
                        S_ = Sf[dr][pp]
                        s.cp("act", Sst[0:32, dr, pp, ci, :], S_[0:32, 0:64], [S_], [Sst])
                        s.cp("act", Sst[32:64, dr, pp, ci, :], S_[32:64, 64:128], [S_], [Sst])
                        p = pkv.next()
                        s.mm(p[:], kd[:, pp * 64:(pp + 1) * 64], rv_[:, pp * 128:(pp + 1) * 128], True, True, [kd, rv_], [p])
                        s.stt("dve", S_[:], S_[:], cdec[:, l, dr, pp:pp + 1], p[:], ALU.mult, ALU.add, [S_, cdec, p], [S_])
            qts = s.sbpool("qt", [64, 2, 128], BF16, 2); kts = s.sbpool("kt", [64, 2, 128], BF16, 2)
            qfs = s.sbpool("qf", [64, 2, 2, 128], BF16, 2)
            sgt = s.sbpool("sgt", [128, 256], F32, 2)
            pst = s.pspool("pst", [128, 128], F32, 2)
            po = s.pspool("po", [128, 256], F32, 2)
            pps = s.sbpool("pp", [128, 128], BF16, 3)
            osb = s.sbpool("osb", [128, 256], F32, 2); osq = s.sbpool("osq", [128, 256], F32, 2)
            ssm = s.sbpool("ssm", [128, 4], F32, 2)
            yrb = s.sbpool("yrb", [128, 256], BF16, 2)
            pTr = s.pspool("pTr", [128, 2, 128], BF16, 2)
            yrt = s.sbpool("yrt", [128, 2, 128], BF16, 2)
            for ci in range(NCH):
                tok = ci * 128
                qt = qts.next(); kt = kts.next(); rv_ = rvt.next(); sg_ = sgt.next()
                s.dma("sp", qt[:], RQT[:, :, tok:tok + 128].rearrange("a p t -> p a t"), reads=[s.db("RQT", ci)], writes=[qt])
                s.dma("sp", kt[:], RKT[:, :, tok:tok + 128].rearrange("a p t -> p a t"), reads=[s.db("RKT", ci)], writes=[kt])
                s.dma("act", rv_[:], RV[tok:tok + 128, :], reads=[s.db("RV", ci)], writes=[rv_])
                s.dma("act", sg_[:], SG[tok:tok + 128, :], reads=[s.db("SG", ci)], writes=[sg_])
                qf = qfs.next()
                for dr in range(2):
                    s.tt("pool", qf[:, dr, :, :], qt[:], qd[:, dr, :, :], ALU.mult, [qt, qd], [qf])
                o = po.next()
                for h in range(NH):
                    pp = h // 2; b0 = 32 * (h % 2)
                    st = pst.next()
                    s.mm(st[:], kt[b0:b0 + 32, pp, :], qt[b0:b0 + 32, pp, :], True, True, [kt, qt], [st])
                    P = pps.next()
                    s.tt("dve", P[:], st[:], dtsum[:, l, h, :], ALU.mult, [st, dtsum], [P])
                    s.mm(o[:, h * 64:(h + 1) * 64], P[:], rv_[:, h * 64:(h + 1) * 64], True, False, [P, rv_], [o])
                    s.mm(o[:, h * 64:(h + 1) * 64], qf[b0:b0 + 32, 0, pp, :], Sst[b0:b0 + 32, 0, pp, ci, :], False, False, [qf, Sst], [o])
                    s.mm(o[:, h * 64:(h + 1) * 64], qf[b0:b0 + 32, 1, pp, :], Sst[b0:b0 + 32, 1, pp, ci, :], False, True, [qf, Sst], [o])
                ob = osb.next(); oq = osq.next(); sm = ssm.next()
                s.cp("act", ob[:], o[:], [o], [ob])
                s.tt("pool", oq[:], ob[:], ob[:], ALU.mult, [ob], [oq])
                s.op("dve", lambda e, sm=sm, oq=oq: e.reduce_sum(out=sm[:], in_=oq[:].rearrange("p (h e) -> p h e", h=4), axis=AX.X), [oq], [sm])
                s.act(sm[:], sm[:], AF.Sqrt, [sm], [sm], scale=1.0 / 64, bias=EPS)
                s.recip(sm[:], sm[:], [sm], [sm])
                s.tt("pool", ob[:].rearrange("p (h e) -> p h e", h=4), ob[:].rearrange("p (h e) -> p h e", h=4), sm[:].unsqueeze(2).to_broadcast([128, 4, 64]), ALU.mult, [ob, sm], [ob])
                yb = yrb.next()
                s.tt("pool", yb[:], ob[:], sg_[:], ALU.mult, [ob, sg_], [yb])
                pt = pTr.next()
                for c in range(2):
                    s.tr(pt[:, c, :], yb[:, c * 128:(c + 1) * 128], ident_b[:], [yb, ident_b], [pt])
                yt = yrt.next()
                s.cp("dve", yt[:], pt[:], [pt], [yt])
                s.dma("sp", MIXT[768:1024, tok:tok + 128].rearrange("(c p) t -> p c t", p=128), yt[:], reads=[yt], writes=[s.db("MIXT", "r", ci)])
        with s.phase():
            knt = s.sb("knt", [128, TT], BF16); krt = s.sb("krt", [128, TT], BF16); vt = s.sb("vt", [128, NT, 128], BF16)
            s.memset("pool", krt[:], 0.0, [krt])
            s.dma("sp", krt[0:64, :], KR, reads=[s.db("KR", g[0]) for g in groups], writes=[krt])
            qnt = s.sbpool("qnt", [128, 512], BF16, 2); qrt = s.sbpool("qrt", [128, 512], BF16, 2)
            for q_ in qrt.tiles:
                s.memset("pool", q_[:], 0.0, [q_])
            pst = s.pspool("pst", [128, 512], F32, 4)
            pacc = s.pspool("pacc", [128, 512], F32, 2); pden = s.pspool("pden", [128, 512], F32, 2)
            Ps = s.sbpool("P", [128, 512], BF16, 6)
            paccA = s.sbpool("paccA", [128, 512], F32, 2); paccB = s.sbpool("paccB", [128, 512], F32, 2)
            rec = s.sbpool("rec", [128, 512], F32, 2)
            oat = s.sbpool("oat", [128, 512], BF16, 2)
            for h in range(NH):
                s.dma("sp", knt[:], KN[h], reads=[s.db("KN", h, g[0]) for g in groups], writes=[knt])
                s.dma("act", vt[:], VV[:, h * 128:(h + 1) * 128].rearrange("(t p) c -> p t c", p=128), reads=[s.db("VV", i) for i in range(NT)], writes=[vt])
                for (t0, n, r) in groups:
                    qn = qnt.next(); qr = qrt.next()
                    s.dma("sp", qn[:, 0:n], QN[h][:, t0:t0 + n], reads=[s.db("QN", h, t0)], writes=[qn])
                    s.dma("act", qr[0:64, 0:n], QR[h][:, t0:t0 + n], reads=[s.db("QR", h, t0)], writes=[qr])
                    nk = C // 128 if r == 1 else NT
                    acc = pacc.next(); den = pden.next()
                    pa = [paccA.next(), paccB.next()]
                    sts = {}; Pt = {}
                    SK = 2
                    for jt in range(nk + SK):
                        if jt < nk:
                            st = pst.next()
                            s.mm(st[:, 0:n], knt[:, jt * 128:(jt + 1) * 128], qn[:, 0:n], True, False, [knt, qn], [st])
                            s.mm(st[:, 0:n], krt[:, jt * 128:(jt + 1) * 128], qr[:, 0:n], False, True, [krt, qr], [st])
                            P = Ps.next()
                            s.act(P[:, 0:n], st[:, 0:n], AF.Exp, [st], [P], scale=ATT_SCALE)
                            Pt[jt] = P
                        if jt >= SK:
                            j = jt - SK
                            P = Pt.pop(j)
                            s.mm(acc[:, 0:n], vt[:, j, :], P[:, 0:n], j == 0, j == nk - 1, [vt, P], [acc])
                            eng = "dve" if j % 2 == 0 else "pool"
                            A_ = pa[j % 2]
                            if j < 2:
                                s.cp(eng, A_[:, 0:n], P[:, 0:n], [P], [A_])
                            else:
                                s.tt(eng, A_[:, 0:n], A_[:, 0:n], P[:, 0:n], ALU.add, [A_, P], [A_])
                    nA = 2 if nk >= 2 else 1
                    for a_ in range(nA):
                        s.mm(den[:, 0:n], ones_f[:], pa[a_][:, 0:n], a_ == 0, a_ == nA - 1, [ones_f, pa[a_]], [den])
                    rc = rec.next()
                    s.recip(rc[:, 0:n], den[:, 0:n], [den], [rc])
                    oa = oat.next()
                    s.tt("dve", oa[:, 0:n], acc[:, 0:n], rc[:, 0:n], ALU.mult, [acc, rc], [oa])
                    s.dma("sp", MIXT[256 + h * 128:256 + (h + 1) * 128, t0:t0 + n], oa[:, 0:n], reads=[oa], writes=[s.db("MIXT", "a", h, t0)])
        if stop_after == "D":
            break
        with s.phase():
            wo = s.sb("wo", [128, 8, D], BF16)
            for k in range(8):
                s.dma("pool", wo[:, k, :], w_out[l][k * 128:(k + 1) * 128, :], writes=[wo])
            rw = s.sb("rw", [128, 8, NE], F32)
            s.dma("sp", rw[:], router_w[l].rearrange("(k p) e -> p k e", p=128), writes=[rw])
            rb = s.sb("rb", [128, NE], F32)
            s.dma("sp", rb[:], router_b[l:l + 1, :].to_broadcast([128, NE]), writes=[rb])
            g2b = s.sb("g2b", [128, D], F32)
            s.dma("sp", g2b[:], g2[l:l + 1, :].to_broadcast([128, D]), writes=[g2b])
            GT1 = []; A2 = []; B2 = []
            for r in range(2):
                gt = s.sb("gt1", [128, D], F32); a2 = s.sb("a2", [128, D], F32); b2_ = s.sb("b2_", [128, D], F32)
                s.dma("sp", gt[:], MOD[l][r:r + 1, 2 * D:3 * D].to_broadcast([128, D]), reads=[s.db("MOD")], writes=[gt])
                s.dma("sp", b2_[:], MOD[l][r:r + 1, 3 * D:4 * D].to_broadcast([128, D]), reads=[s.db("MOD")], writes=[b2_])
                s.dma("sp", a2[:], MOD[l][r:r + 1, 4 * D:5 * D].to_broadcast([128, D]), reads=[s.db("MOD")], writes=[a2])
                s.stt("dve", a2[:], a2[:], 1.0, g2b[:], ALU.add, ALU.mult, [a2, g2b], [a2])
                GT1.append(gt); A2.append(a2); B2.append(b2_)
            mts = s.sbpool("mt", [128, 8, 128], BF16, 2)
            xts = s.sbpool("xt", [128, D], F32, 2)
            pmx = s.pspool("pmx", [128, D], F32, 2)
            tmps = s.sbpool("tmp", [128, D], F32, 2)
            junk = s.sb("junk", [128, D], F32)
            sss = s.sbpool("ss", [128, 1], F32, 2)
            h2s = s.sbpool("h2", [128, D], F32, 2)
            pT2 = s.pspool("pT2", [128, 8, 128], BF16, 1); pT3 = s.pspool("pT3", [128, 8, 128], BF16, 1)
            h2b = s.sbpool("h2b", [128, 16, 128], BF16, 2)
            hhis = s.sbpool("hhi", [128, D], BF16, 2); hlos = s.sbpool("hlo", [128, D], BF16, 2)
            rwh = s.sb("rwh", [128, 8, NE], BF16); rwl = s.sb("rwl", [128, 8, NE], BF16)
            s.cp("dve", rwh[:], rw[:], [rw], [rwh])
            s.tt("dve", rw[:], rw[:], rwh[:], ALU.subtract, [rw, rwh], [rw])
            s.cp("dve", rwl[:], rw[:], [rw], [rwl])
            plg = s.pspool("plg", [128, NE], F32, 2)
            lgs = s.sbpool("lgs", [128, NE], F32, 2); t8 = s.sbpool("t8", [128, 8], F32, 2)
            msk = s.sbpool("msk", [128, NE], F32, 2); ex = s.sbpool("ex", [128, NE], F32, 2)
            nm = s.sbpool("nm", [128, 1], F32, 2); sm1 = s.sbpool("sm1", [128, 1], F32, 2)
            for ti in range(NT):
                tok = ti * 128
                r = 1 if tok < C else 0
                if r == 1 and l == NL - 1:
                    continue
                mt = mts.next()
                s.dma("sp", mt[:], MIXT[:, tok:tok + 128].rearrange("(k p) t -> p k t", p=128), reads=[s.db(*k_) for k_ in list(s.dbufs) if k_[0] == "MIXT"], writes=[mt])
                xt = xts.next()
                s.dma("act", xt[:], XR[tok:tok + 128, :], reads=[s.db("XR", ti)], writes=[xt])
                pm = pmx.next()
                for hf in range(2):
                    for k in range(8):
                        s.mm(pm[:, hf * 512:(hf + 1) * 512], mt[:, k, :], wo[:, k, hf * 512:(hf + 1) * 512], k == 0, k == 7, [mt, wo], [pm])
                tp = tmps.next()
                for hf in range(2):
                    s.tt("dve", tp[:, hf * 512:(hf + 1) * 512], pm[:, hf * 512:(hf + 1) * 512], GT1[r][:, hf * 512:(hf + 1) * 512], ALU.mult, [pm, GT1[r]], [tp])
                s.tt("pool", xt[:], xt[:], tp[:], ALU.add, [xt, tp], [xt])
                s.dma("sp", XR[tok:tok + 128, :], xt[:], reads=[xt], writes=[s.db("XR", ti)])
                if CUT == 11: continue
                ss = sss.next()
                s.act(junk[:], xt[:], AF.Square, [xt], [junk, ss], accum_out=ss[:])
                s.act(ss[:], ss[:], AF.Sqrt, [ss], [ss], scale=1.0 / D, bias=EPS)
                s.recip(ss[:], ss[:], [ss], [ss])
                h2 = h2s.next()
                s.stt("dve", h2[:], xt[:], ss[:, 0:1], A2[r][:], ALU.mult, ALU.mult, [xt, ss, A2[r]], [h2])
                s.tt("pool", h2[:], h2[:], B2[r][:], ALU.add, [h2, B2[r]], [h2])
                if CUT == 12: continue
                hhi = hhis.next(); hlo = hlos.next()
                s.cp("act", hhi[:], h2[:], [h2], [hhi])
                s.tt("pool", h2[:], h2[:], hhi[:], ALU.subtract, [h2, hhi], [h2])
                s.cp("pool", hlo[:], h2[:], [h2], [hlo])
                if CUT == 13: continue
                pt = pT2.next(); ptl = pT3.next()
                for k in range(8):
                    s.tr(pt[:, k, :], hhi[:, k * 128:(k + 1) * 128], ident_b[:], [hhi, ident_b], [pt])
                for k in range(8):
                    s.tr(ptl[:, k, :], hlo[:, k * 128:(k + 1) * 128], ident_b[:], [hlo, ident_b], [ptl])
                hb_ = h2b.next()
                s.cp("dve", hb_[:, 0:8, :], pt[:], [pt], [hb_])
                s.cp("dve", hb_[:, 8:16, :], ptl[:], [ptl], [hb_])
                for k in range(8):
                    s.dma("sp" if k % 2 == 0 else "act", H2T[k * 128:(k + 1) * 128, tok:tok + 128], hb_[:, k, :], reads=[hb_], writes=[s.db("H2T", ti)])
                if CUT == 8: continue
                pl_ = plg.next()
                for k in range(8):
                    s.mm(pl_[:], hb_[:, k, :], rwh[:, k, :], k == 0, False, [hb_, rwh], [pl_])
                    s.mm(pl_[:], hb_[:, 8 + k, :], rwh[:, k, :], False, False, [hb_, rwh], [pl_])
                    s.mm(pl_[:], hb_[:, k, :], rwl[:, k, :], False, k == 7, [hb_, rwl], [pl_])
                lg_ = lgs.next()
                s.tt("dve", lg_[:], pl_[:], rb[:], ALU.add, [pl_, rb], [lg_])
                if CUT == 9: continue
                t8_ = t8.next()
                s.op("dve", lambda e, t8_=t8_, lg_=lg_: e.max(out=t8_[:], in_=lg_[:]), [lg_], [t8_])
                mk_ = msk.next()
                s.ts("dve", mk_[:], lg_[:], t8_[:, 3:4], None, ALU.is_ge, None, [lg_, t8_], [mk_])
                nm_ = nm.next()
                s.ts("dve", nm_[:], t8_[:, 0:1], -1.0, None, ALU.mult, None, [t8_], [nm_])
                ex_ = ex.next()
                s.act(ex_[:], lg_[:], AF.Exp, [lg_, nm_], [ex_], bias=nm_[:, 0:1])
                s.tt("dve", ex_[:], ex_[:], mk_[:], ALU.mult, [ex_, mk_], [ex_])
                sm_ = sm1.next()
                s.op("dve", lambda e, sm_=sm_, ex_=ex_: e.reduce_sum(out=sm_[:], in_=ex_[:], axis=AX.X), [ex_], [sm_])
                s.recip(sm_[:], sm_[:], [sm_], [sm_])
                s.ts("dve", ex_[:], ex_[:], sm_[:, 0:1], None, ALU.mult, None, [ex_, sm_], [ex_])
                s.dma("sp", GATE[tok:tok + 128, :], ex_[:], reads=[ex_], writes=[s.db("GATE", ti)])
        if stop_after == "E":
            break
        with s.phase():
            b1t = s.sb("b1t", [128, NE, 16], F32)
            s.dma("sp", b1t[:], b1c[l], writes=[b1t])
            GT2 = []
            for r in range(2):
                gt = s.sb("gt2", [128, D], F32)
                s.dma("sp", gt[:], MOD[l][r:r + 1, 5 * D:6 * D].to_broadcast([128, D]), reads=[s.db("MOD")], writes=[gt])
                GT2.append(gt)
            wps = s.sbpool("wp", [128, 8, 512], BF16, 8)
            b2s = s.sbpool("b2s", [128, D], F32, 2)
            hts = s.sb("hts", [128, 8, 1024], BF16)
            yacc = s.sb("yacc", [128, 8, D], F32)
            gts = s.sb("gts", [128, 8, NE], F32)
            actt = s.sb("actt", [128, 8, 1024], BF16)
            actt_b = [Buf("actt0"), Buf("actt1")]
            gq = s.sbpool("gq", [128, 512], F32, 3); sq_ = s.sbpool("sq_", [128, 512], F32, 3)
            lq = s.sbpool("lq", [128, 512], F32, 3); tq = s.sbpool("tq", [128, 512], F32, 3)
            tmo = s.sbpool("tmo", [128, 512], F32, 2)
            xts = s.sbpool("xt", [128, D], F32, 2)
            pg_ = s.pspool("pg", [128, 512], F32, 2); pl_ = s.pspool("pl", [128, 512], F32, 2); po_ = s.pspool("po", [128, 512], F32, 3)
            sgs_ = []
            ti = 0
            while ti < NT:
                nt_ = (C // 128) if ti < C // 128 else min(8, NT - ti)
                nt_ = min(nt_, 8)
                sgs_.append((ti, nt_)); ti += nt_
            for e_ in range(NE):
                for q in range(4):
                    wp = wps.next()
                    s.dma("pool", wp[:], w1[l][e_][:, q * 512:(q + 1) * 512].rearrange("(k p) c -> p k c", p=128), writes=[wp])
                    s.dma("sp" if q % 2 == 0 else "act", W1B[l][e_][q], wp[:].rearrange("p k c -> p (k c)"), reads=[wp], writes=[s.db("W1B", e_, q)])
                for q in range(2):
                    wp = wps.next()
                    s.dma("pool", wp[:], w2[l][e_][:, q * 512:(q + 1) * 512].rearrange("(k p) c -> p k c", p=128), writes=[wp])
                    s.dma("sp" if q % 2 == 0 else "act", W2B[l][e_][q], wp[:].rearrange("p k c -> p (k c)"), reads=[wp], writes=[s.db("W2B", e_, q)])
            for (ti0, ntl) in sgs_:
                S_ = ntl * 128; tok0 = ti0 * 128
                r = 1 if tok0 < C else 0
                if r == 1 and l == NL - 1:
                    continue
                s.dma("sp", hts[:, :, 0:S_], H2T[:, tok0:tok0 + S_].rearrange("(k p) t -> p k t", p=128), reads=[s.db("H2T", ti0 + i) for i in range(ntl)], writes=[hts])
                s.dma("act", gts[:, 0:ntl, :], GATE[tok0:tok0 + S_, :].rearrange("(i p) e -> p i e", p=128), reads=[s.db("GATE", ti0 + i) for i in range(ntl)], writes=[gts])
                s.memset("pool", yacc[:], 0.0, [yacc])
                ngs = [(a, min(512, S_ - a)) for a in range(0, S_, 512)]
                for e_ in range(NE):
                    pieces = {}
                    for q in (0, 2, 1, 3):
                        wp = wps.next()
                        s.dma("sp", wp[:].rearrange("p k c -> p (k c)"), W1B[l][e_][q], reads=[s.db("W1B", e_, q)], writes=[wp])
                        pieces[q] = wp
                    w2p = []
                    for q in range(2):
                        wp = wps.next()
                        s.dma("sp", wp[:].rearrange("p k c -> p (k c)"), W2B[l][e_][q], reads=[s.db("W2B", e_, q)], writes=[wp])
                        w2p.append(wp)
                    b2t = b2s.next()
                    s.dma("sp", b2t[:], b2[l][e_:e_ + 1, :].to_broadcast([128, D]), writes=[b2t])
                    pend = None
                    for (a, nn) in ngs:
                        for c in range(8):
                            wg = pieces[c // 4]; wl = pieces[2 + c // 4]; cc_ = c % 4
                            pg = pg_.next(); pl = pl_.next()
                            for k in range(8):
                                s.mm(pg[:, 0:nn], wg[:, k, cc_ * 128:(cc_ + 1) * 128], hts[:, k, a:a + nn], k == 0, k == 7, [wg, hts], [pg])
                            for k in range(8):
                                s.mm(pl[:, 0:nn], wl[:, k, cc_ * 128:(cc_ + 1) * 128], hts[:, k, a:a + nn], k == 0, k == 7, [wl, hts], [pl])
                            g_ = gq.next(); sg_ = sq_.next(); l_ = lq.next(); t_ = tq.next()
                            s.ts("dve", g_[:, 0:nn], pg[:, 0:nn], b1t[:, e_, c:c + 1], 7.0, ALU.add, ALU.min, [pg, b1t], [g_])
                            if pend is not None:
                                pend()
                            s.act(sg_[:, 0:nn], g_[:, 0:nn], AF.Sigmoid, [g_], [sg_], scale=1.702)
                            s.act(l_[:, 0:nn], pl[:, 0:nn], AF.Identity, [pl, b1t], [l_], bias=b1t[:, e_, 8 + c:9 + c])
                            s.ts("pool", l_[:, 0:nn], l_[:, 0:nn], 7.0, -7.0, ALU.min, ALU.max, [l_], [l_])
                            s.tt("pool", t_[:, 0:nn], g_[:, 0:nn], sg_[:, 0:nn], ALU.mult, [g_, sg_], [t_])
                            pend = (lambda c=c, a=a, nn=nn, l_=l_, t_=t_: s.stt("dve", actt[:, c, a:a + nn], l_[:, 0:nn], 1.0, t_[:, 0:nn], ALU.add, ALU.mult, [l_, t_], [actt_b[a // 512]]))
                    pend()
                    for i in range(ntl):
                        for hf in range(2):
                            po = po_.next()
                            for k in range(8):
                                s.mm(po[:], actt[:, k, i * 128:(i + 1) * 128], w2p[hf][:, k, :], k == 0, k == 7, [actt_b[i // 4], w2p[hf]], [po])
                            tm = tmo.next()
                            s.tt("dve", tm[:], po[:], b2t[:, hf * 512:(hf + 1) * 512], ALU.add, [po, b2t], [tm])
                            s.stt("dve", yacc[:, i, hf * 512:(hf + 1) * 512], tm[:], gts[:, i, e_:e_ + 1], yacc[:, i, hf * 512:(hf + 1) * 512], ALU.mult, ALU.add, [tm, gts, yacc], [yacc])
                for i in range(ntl):
                    tok = tok0 + i * 128
                    xt = xts.next()
                    s.dma("sp", xt[:], XR[tok:tok + 128, :], reads=[s.db("XR", ti0 + i)], writes=[xt])
                    s.tt("dve", yacc[:, i, :], yacc[:, i, :], GT2[r][:], ALU.mult, [yacc, GT2[r]], [yacc])
                    s.tt("pool", xt[:], xt[:], yacc[:, i, :], ALU.add, [xt, yacc], [xt])
                    s.dma("sp", XR[tok:tok + 128, :], xt[:], reads=[xt], writes=[s.db("XR", ti0 + i)])
    if stop_after is None:
        with s.phase():
            gfb = s.sb("gfb", [128, D], F32)
            s.dma("sp", gfb[:], gf.rearrange("(o d) -> o d", o=1).to_broadcast([128, D]), writes=[gfb])
            xts = s.sbpool("xt", [128, D], F32, 3)
            junk = s.sb("junk", [128, D], F32)
            sss = s.sbpool("ss", [128, 1], F32, 2)
            for ti in range(C // 128, NT):
                tok = ti * 128
                xt = xts.next()
                s.dma("sp", xt[:], XR[tok:tok + 128, :], reads=[s.db("XR", ti)], writes=[xt])
                ss = sss.next()
                s.act(junk[:], xt[:], AF.Square, [xt], [junk, ss], accum_out=ss[:])
                s.act(ss[:], ss[:], AF.Sqrt, [ss], [ss], scale=1.0 / D, bias=EPS)
                s.recip(ss[:], ss[:], [ss], [ss])
                s.stt("dve", xt[:], xt[:], ss[:, 0:1], gfb[:], ALU.mult, ALU.mult, [xt, ss, gfb], [xt])
                s.dma("act", y_out[tok - C:tok - C + 128, :], xt[:], reads=[xt])
    s.finish()
    s.emit()
    return nc


def kernel(**inputs):
    inp = {k: np.asarray(v) for k, v in inputs.items()}
    B, L, _ = inp["x"].shape
    C = inp["ctx"].shape[1]
    NL = inp["w_ada"].shape[0]
    nc = build(L, C, NL)
    cst = host_consts(L, C)
    in_maps = []
    for b in range(B):
        m = host_layout(inp, b, NL)
        m.update(cst)
        in_maps.append({k: np.ascontiguousarray(v, dtype=np.float32) for k, v in m.items()})
    res = run_bass_kernel_spmd(nc, in_maps, core_ids=list(range(B)))
    return np.stack([np.asarray(r["y"], dtype=np.float32) for r in res.results], 0)
```

```python
from contextlib import ExitStack
import numpy as np
import concourse.bass as bass
import concourse.mybir as mybir
from concourse.bass_utils import run_bass_kernel_spmd

ALU = mybir.AluOpType
AF = mybir.ActivationFunctionType
AX = mybir.AxisListType
F32 = mybir.dt.float32
BF16 = mybir.dt.bfloat16

NSLOT = 12


class Buf:
    __slots__ = ("name", "lw", "rd")

    def __init__(self, name=""):
        self.name = name
        self.lw = None
        self.rd = []


class T:
    def __init__(self, t, b):
        self.t = t
        self.b = b

    def __getitem__(self, k):
        return self.t[k]


class Pool:
    def __init__(self, tiles):
        self.tiles = tiles
        self.i = 0

    def next(self):
        t = self.tiles[self.i % len(self.tiles)]
        self.i += 1
        return t


class Sched:
    def __init__(self, nc):
        self.nc = nc
        self.engs = {"pe": nc.tensor, "act": nc.scalar, "dve": nc.vector, "pool": nc.gpsimd, "sp": nc.sync}
        self.ops = {k: [] for k in self.engs}
        self.cnt = {k: 0 for k in self.engs}
        self.sem = {k: nc.alloc_semaphore("s_" + k) for k in self.engs}
        self.dq = ("sp", "act", "pool")
        self.dsem = {q: [nc.alloc_semaphore("d_%s_%d" % (q, i)) for i in range(NSLOT)] for q in self.dq}
        self.dcnt = {q: 0 for q in self.dq}
        self.known = {k: {} for k in self.engs}
        self.stack = None
        self.uid = 0
        self.dbufs = {}

    def _nm(self, name):
        self.uid += 1
        return "%s_%d" % (name, self.uid)

    def sb(self, name, shape, dtype=F32):
        nm = self._nm(name)
        if self.stack is not None:
            t = self.stack.enter_context(self.nc.sbuf_tensor(nm, list(shape), dtype))
        else:
            t = self.nc.alloc_sbuf_tensor(nm, list(shape), dtype)
        return T(t, Buf(nm))

    def ps(self, name, shape, dtype=F32):
        nm = self._nm(name)
        if self.stack is not None:
            t = self.stack.enter_context(self.nc.psum_tensor(nm, list(shape), dtype))
        else:
            t = self.nc.alloc_psum_tensor(nm, list(shape), dtype)
        return T(t, Buf(nm))

    def sbpool(self, name, shape, dtype, n):
        return Pool([self.sb(name, shape, dtype) for _ in range(n)])

    def pspool(self, name, shape, dtype, n):
        return Pool([self.ps(name, shape, dtype) for _ in range(n)])

    def db(self, *key):
        b = self.dbufs.get(key)
        if b is None:
            b = Buf(str(key))
            self.dbufs[key] = b
        return b

    class _Phase:
        def __init__(self, s):
            self.s = s

        def __enter__(self):
            self.s.stack = ExitStack()
            self.s.stack.__enter__()
            return self

        def __exit__(self, *a):
            self.s.barrier()
            st = self.s.stack
            self.s.stack = None
            st.__exit__(None, None, None)
            return False

    def phase(self):
        return Sched._Phase(self)

    def _need(self, eng, ev, waits):
        if ev is None:
            return
        sem, val, src = ev
        if src == eng and eng == "pe":
            return
        if self.known[eng].get(sem, 0) >= val:
            return
        if waits.get(sem, (None, 0))[1] < val:
            waits[sem] = (sem, val)

    def _deps(self, eng, reads, writes):
        waits = {}
        for b in reads:
            b = b.b if isinstance(b, T) else b
            self._need(eng, b.lw, waits)
        for b in writes:
            b = b.b if isinstance(b, T) else b
            self._need(eng, b.lw, waits)
            for r in b.rd:
                self._need(eng, r, waits)
        for sem, (s_, v) in waits.items():
            self.known[eng][sem] = v
        return list(waits.values())

    def _commit(self, ev, reads, writes):
        for b in reads:
            b = b.b if isinstance(b, T) else b
            b.rd.append(ev)
        for b in writes:
            b = b.b if isinstance(b, T) else b
            b.lw = ev
            b.rd = []

    def op(self, eng, fn, reads=(), writes=()):
        waits = self._deps(eng, reads, writes)
        self.cnt[eng] += 1
        ev = (self.sem[eng], self.cnt[eng], eng)
        self.ops[eng].append((waits, fn, (self.sem[eng], 1)))
        self._commit(ev, reads, writes)

    def dma(self, q, out, in_, reads=(), writes=(), **kw):
        waits = self._deps(q, reads, writes)
        i = self.dcnt[q]
        self.dcnt[q] += 1
        slot = i % NSLOT
        sem = self.dsem[q][slot]
        rnd = i // NSLOT
        if rnd > 0:
            w = {}
            self._need(q, (sem, 16 * rnd, "dma"), w)
            for sem_, (s_, v) in w.items():
                self.known[q][sem_] = v
            waits = waits + list(w.values())
        ev = (sem, 16 * (rnd + 1), "dma")
        fn = lambda e, out=out, in_=in_, kw=kw: e.dma_start(out=out, in_=in_, **kw)
        self.ops[q].append((waits, fn, (sem, 16)))
        self._commit(ev, reads, writes)

    def _all_events(self):
        evs = []
        for k in self.engs:
            if self.cnt[k] > 0:
                evs.append((self.sem[k], self.cnt[k], k))
        for qq in self.dq:
            n = self.dcnt[qq]
            for slot in range(min(n, NSLOT)):
                cnt = (n - slot + NSLOT - 1) // NSLOT
                evs.append((self.dsem[qq][slot], 16 * cnt, "dma"))
        return evs

    def barrier(self):
        evs = self._all_events()
        for eng in self.engs:
            w = {}
            for ev in evs:
                if ev[2] == eng:
                    continue
                self._need(eng, ev, w)
            for sem_, (s_, v) in w.items():
                self.known[eng][sem_] = v
            if w:
                self.ops[eng].append((list(w.values()), None, None))

    def finish(self):
        self.barrier()

    def emit(self):
        nc = self.nc
        with nc.Block() as block:
            def run(name):
                def body(e):
                    for waits, fn, inc in self.ops[name]:
                        for sem, val in waits:
                            e.wait_ge(sem, val)
                        if fn is not None:
                            ins = fn(e)
                            ins.then_inc(inc[0], inc[1])
                return body

            block.tensor(run("pe"))
            block.scalar(run("act"))
            block.vector(run("dve"))
            block.gpsimd(run("pool"))
            block.sync(run("sp"))

    def mm(self, out, lhsT, rhs, start, stop, R, W):
        self.op("pe", lambda e: e.matmul(out, lhsT=lhsT, rhs=rhs, start=start, stop=stop), R, W)

    def tr(self, out, in_, ident, R, W):
        self.op("pe", lambda e: e.transpose(out=out, in_=in_, identity=ident), R, W)

    def act(self, out, in_, func, R, W, **kw):
        self.op("act", lambda e: e.activation(out=out, in_=in_, func=func, **kw), R, W)

    def ts(self, eng, out, in0, s1, s2, op0, op1, R, W):
        if op1 is None:
            self.op(eng, lambda e: e.tensor_scalar(out=out, in0=in0, scalar1=s1, scalar2=None, op0=op0), R, W)
        else:
            self.op(eng, lambda e: e.tensor_scalar(out=out, in0=in0, scalar1=s1, scalar2=s2, op0=op0, op1=op1), R, W)

    def tt(self, eng, out, in0, in1, op, R, W):
        self.op(eng, lambda e: e.tensor_tensor(out=out, in0=in0, in1=in1, op=op), R, W)

    def stt(self, eng, out, in0, scalar, in1, op0, op1, R, W):
        self.op(eng, lambda e: e.scalar_tensor_tensor(out=out, in0=in0, scalar=scalar, in1=in1, op0=op0, op1=op1), R, W)

    def cp(self, eng, out, in_, R, W):
        if eng == "act":
            self.op("act", lambda e: e.copy(out=out, in_=in_), R, W)
        else:
            self.op(eng, lambda e: e.tensor_copy(out=out, in_=in_), R, W)

    def recip(self, out, in_, R, W):
        self.op("dve", lambda e: e.reciprocal(out=out, in_=in_), R, W)

    def memset(self, eng, ap, val, W):
        self.op(eng, lambda e: e.memset(ap, val), (), W)


CUT = 99

D = 1024
NH = 4
EPS = 1e-6
K_SCALE = 32 ** -0.5
ATT_SCALE = 192 ** -0.5
NE = 32
SENT = {}


def host_consts(L, C):
    TT = C + L
    f32 = np.float32
    cst = {}
    cst["ident"] = np.eye(128, dtype=f32)
    rows = L // 64
    row = np.repeat(np.arange(rows), 64).astype(f32)
    col = np.tile(np.arange(64), rows).astype(f32)
    freq = (f32(10000.0) ** (-np.arange(16, dtype=f32) / f32(16))).astype(f32)
    ang = np.concatenate([row[:, None] * freq, col[:, None] * freq], axis=-1).astype(f32)
    cos = np.ones((TT, 32), f32)
    sin = np.zeros((TT, 32), f32)
    cos[C:] = np.cos(ang)
    sin[C:] = np.sin(ang)
    cst["mc"] = np.ascontiguousarray(np.concatenate([cos, cos], 1).T)
    cst["ms"] = np.ascontiguousarray(np.concatenate([-sin, sin], 1).T)
    theta = (1.0 / (f32(10000.0) ** np.linspace(0.0, 1.0, 16, dtype=f32))).astype(f32)
    a2 = (np.arange(TT, dtype=f32)[:, None] * theta).astype(f32)
    cst["rcs"] = np.ascontiguousarray(np.concatenate([np.cos(a2), np.sin(a2)], 1).astype(f32))
    j = np.arange(128)[:, None]
    i = np.arange(128)[None, :]
    dm = np.zeros((128, 5, 128), f32)
    dm[:, 0] = np.maximum(i - j, 0)
    dm[:, 1] = np.maximum(j - i, 0)
    dm[:, 2] = (i > j)
    dm[:, 3] = (j > i)
    dm[:, 4] = 2.0 * (i == j)
    cst["dm"] = dm
    jj = np.arange(128, dtype=f32)
    cst["pcol"] = np.stack([127 - jj, jj, jj + 1, 128 - jj], 1).astype(f32)
    ii = np.arange(128, dtype=f32)
    cst["irow"] = np.ascontiguousarray(np.broadcast_to(np.stack([ii + 1, 128 - ii], 0)[None], (64, 2, 128)).astype(f32))
    return cst


def col(v, p=128):
    v = np.asarray(v)
    sh = v.shape
    return np.ascontiguousarray(v.reshape(sh[:-1] + (sh[-1] // p, p)).swapaxes(-1, -2))


def host_layout(inp, b, NL):
    m = {}
    m["x"] = np.ascontiguousarray(inp["x"][b])
    m["ctx"] = np.ascontiguousarray(inp["ctx"][b])
    cv = np.stack([inp["c"][b], inp["c_ctx"]], 0)
    m["ccol"] = np.ascontiguousarray(col(cv).transpose(1, 2, 0).reshape(128, 16))
    m["w_ada"] = inp["w_ada"]
    m["b_ada"] = inp["b_ada"]
    m["b_adac"] = col(inp["b_ada"])
    m["g1c"] = col(inp["g_norm1"])
    m["g2"] = inp["g_norm2"]
    m["w_in"] = inp["w_in"]
    m["w_out"] = inp["w_out"]
    cw = inp["conv_w"]
    m["convw"] = np.ascontiguousarray(cw.reshape(NL, 31, 2, 128).transpose(0, 3, 2, 1))
    m["convp"] = np.ascontiguousarray(np.stack([col(inp["conv_b"]), col(inp["conv_ln_g"]), col(inp["conv_ln_b"])], -1))
    m["qng"] = col(inp["mla_q_norm"])
    m["kvng"] = col(inp["mla_kv_norm"])
    m["w_uq"] = inp["mla_w_uq"]
    m["w_ukv"] = inp["mla_w_ukv"]
    dec = np.concatenate([inp["ret_decay_fwd"], inp["ret_decay_bwd"]], -1)
    m["dec"] = np.ascontiguousarray(dec)
    m["router_w"] = inp["router_w"]
    m["router_b"] = inp["router_b"]
    m["w1"] = inp["moe_w1"]
    m["w2"] = inp["moe_w2"]
    m["b1c"] = np.ascontiguousarray(col(inp["moe_b1"]).transpose(0, 2, 1, 3))
    m["b2"] = inp["moe_b2"]
    m["gf"] = inp["g_final"]
    return m


def build(L, C, NL=2, dbg=(), stop_after=None):
    TT = C + L
    NT = TT // 128
    nc = bass.Bass("TRN2", target_bir_lowering=False)
    s = Sched(nc)

    def din(name, shape):
        return nc.dram_tensor(name, list(shape), F32, kind="ExternalInput").ap()

    x_in = din("x", [L, D]); ctx_in = din("ctx", [C, D]); ccol = din("ccol", [128, 16])
    w_ada = din("w_ada", [NL, D, 6 * D]); b_ada = din("b_ada", [NL, 6 * D]); b_adac = din("b_adac", [NL, 128, 48])
    g1c = din("g1c", [NL, 128, 8]); g2 = din("g2", [NL, D])
    w_in = din("w_in", [NL, D, 1984]); w_out = din("w_out", [NL, D, D])
    convw = din("convw", [NL, 128, 2, 31]); convp = din("convp", [NL, 128, 2, 3])
    qng = din("qng", [NL, 128, 3]); kvng = din("kvng", [NL, 128, 2])
    w_uq = din("w_uq", [NL, 384, 768]); w_ukv = din("w_ukv", [NL, 256, 1024])
    dec = din("dec", [NL, 8])
    router_w = din("router_w", [NL, D, NE]); router_b = din("router_b", [NL, NE])
    w1 = din("w1", [NL, NE, D, 2048]); w2 = din("w2", [NL, NE, D, D])
    b1c = din("b1c", [NL, 128, NE, 16]); b2 = din("b2", [NL, NE, D]); gf = din("gf", [D])
    ident_d = din("ident", [128, 128]); mc_d = din("mc", [64, TT]); ms_d = din("ms", [64, TT])
    rcs_d = din("rcs", [TT, 32]); dm_d = din("dm", [128, 5, 128]); pcol_d = din("pcol", [128, 4]); irow_d = din("irow", [64, 2, 128])

    def scratch(name, shape, dt):
        kind = "ExternalOutput" if name in dbg else "Internal"
        return nc.dram_tensor(name, list(shape), dt, kind=kind).ap()

    y_out = nc.dram_tensor("y", [L, D], F32, kind="ExternalOutput").ap()
    XR = scratch("XR", [TT, D], F32)
    MOD = scratch("MOD", [NL, 2, 6 * D], F32)
    QN = scratch("QN", [NH, 128, TT], BF16); QR = scratch("QR", [NH, 64, TT], BF16)
    KN = scratch("KN", [NH, 128, TT], BF16); KR = scratch("KR", [64, TT], BF16)
    VV = scratch("VV", [TT, 512], BF16)
    RQT = scratch("RQT", [2, 64, TT], BF16); RKT = scratch("RKT", [2, 64, TT], BF16)
    RK = scratch("RK", [TT, 128], BF16); RV = scratch("RV", [TT, 256], BF16); SG = scratch("SG", [TT, 256], F32)
    MIXT = scratch("MIXT", [D, TT], BF16)
    H2T = scratch("H2T", [D, TT], BF16)
    GATE = scratch("GATE", [TT, NE], F32)
    ZTD = scratch("ZT", [128, 2, TT], BF16)
    W1B = scratch("W1B", [NL, NE, 4, 128, 4096], BF16)
    W2B = scratch("W2B", [NL, NE, 2, 128, 4096], BF16)

    groups = []
    t = 0
    while t < C:
        n = min(512, C - t); groups.append((t, n, 1)); t += n
    while t < TT:
        n = min(512, TT - t); groups.append((t, n, 0)); t += n

    ident_f = s.sb("ident_f", [128, 128], F32)
    ident_b = s.sb("ident_b", [128, 128], BF16)
    ones_b = s.sb("ones_b", [128, 128], BF16)
    s.dma("sp", ident_f[:], ident_d, writes=[ident_f])
    s.dma("pool", ident_b[:], ident_d, writes=[ident_b])
    s.memset("dve", ones_b[:], 1.0, [ones_b])
    ones_f = s.sb("ones_f", [128, 128], F32)
    s.memset("dve", ones_f[:], 1.0, [ones_f])
    modc = s.sb("modc", [128, NL, 48, 2], F32)
    a1c = s.sb("a1c", [128, NL, 2, 8], F32)
    b1c_ = s.sb("b1c_", [128, NL, 2, 8], F32)
    dtsum = s.sb("dtsum", [128, NL, NH, 128], BF16)
    kqdec = s.sb("kqdec", [128, NL, 4, NH], F32)
    cdec = s.sb("cdec", [64, NL, 2, 2], F32)

    with s.phase():
        cc = s.sb("cc", [128, 16], F32)
        scol = s.sb("scol", [128, 16], F32)
        s.dma("sp", cc[:], ccol, writes=[cc])
        s.act(scol[:], cc[:], AF.Sigmoid, [cc], [scol])
        s.tt("dve", scol[:], scol[:], cc[:], ALU.mult, [scol, cc], [scol])
        scv = scol[:].rearrange("p (k r) -> p k r", r=2)
        bac = s.sb("bac", [128, NL, 48], F32)
        for l in range(NL):
            s.dma("sp", bac[:, l, :], b_adac[l], writes=[bac])
        wts = s.sbpool("wada", [128, 8, 512], F32, 2)
        brow = s.sbpool("brow", [2, 512], F32, 2)
        mrow = s.sbpool("mrow", [2, 512], F32, 2)
        pr = s.pspool("pr", [2, 512], F32, 2)
        pc = s.pspool("pc", [128, 4, 2], F32, 2)
        for l in range(NL):
            for n in range(12):
                wt = wts.next()
                s.dma("sp" if n % 2 == 0 else "act", wt[:], w_ada[l][:, n * 512:(n + 1) * 512].rearrange("(k p) c -> p k c", p=128), writes=[wt])
                br = brow.next()
                s.dma("sp", br[:], b_ada[l:l + 1, n * 512:(n + 1) * 512].to_broadcast([2, 512]), writes=[br])
                p = pr.next()
                for k in range(8):
                    s.mm(p[:], scv[:, k, :], wt[:, k, :], k == 0, k == 7, [scol, wt], [p])
                mr = mrow.next()
                s.tt("dve", mr[:], p[:], br[:], ALU.add, [p, br], [mr])
                s.dma("sp", MOD[l][:, n * 512:(n + 1) * 512], mr[:], reads=[mr], writes=[s.db("MOD")])
                pcc = pc.next()
                for j in range(4):
                    for k in range(8):
                        s.mm(pcc[:, j, :], wt[:, k, j * 128:(j + 1) * 128], scv[:, k, :], k == 0, k == 7, [scol, wt], [pcc])
                s.tt("dve", modc[:, l, n * 4:(n + 1) * 4, :], pcc[:], bac[:, l, n * 4:(n + 1) * 4].unsqueeze(2).to_broadcast([128, 4, 2]), ALU.add, [pcc, bac], [modc])
            g1t = s.sb("g1t", [128, 8], F32)
            s.dma("sp", g1t[:], g1c[l], writes=[g1t])
            for r in range(2):
                s.ts("dve", a1c[:, l, r, :], modc[:, l, 8:16, r], 1.0, None, ALU.add, None, [modc], [a1c])
                s.tt("dve", a1c[:, l, r, :], a1c[:, l, r, :], g1t[:], ALU.mult, [a1c, g1t], [a1c])
                s.cp("dve", b1c_[:, l, r, :], modc[:, l, 0:8, r], [modc], [b1c_])
        dmt = s.sb("dmt", [128, 5, 128], F32)
        pcl = s.sb("pcl", [128, 4], F32)
        s.dma("sp", dmt[:], dm_d, writes=[dmt])
        s.dma("sp", pcl[:], pcol_d, writes=[pcl])
        for l in range(NL):
            dcb = s.sb("dcb", [128, 8], F32)
            s.dma("sp", dcb[:], dec[l:l + 1, :].to_broadcast([128, 8]), writes=[dcb])
            lg = s.sb("lg", [128, 8], F32)
            s.act(lg[:], dcb[:], AF.Exp, [dcb], [lg], scale=-1.0)
            s.act(lg[:], lg[:], AF.Ln, [lg], [lg], bias=1.0)
            s.ts("dve", lg[:], lg[:], -1.0, None, ALU.mult, None, [lg], [lg])
            for h in range(NH):
                ef = s.sb("ef", [128, 128], F32)
                eb = s.sb("eb", [128, 128], F32)
                s.act(ef[:], dmt[:, 0, :], AF.Exp, [dmt, lg], [ef], scale=lg[:, h:h + 1])
                s.act(eb[:], dmt[:, 1, :], AF.Exp, [dmt, lg], [eb], scale=lg[:, 4 + h:5 + h])
                s.tt("dve", ef[:], ef[:], dmt[:, 2, :], ALU.mult, [ef, dmt], [ef])
                s.tt("dve", eb[:], eb[:], dmt[:, 3, :], ALU.mult, [eb, dmt], [eb])
                s.tt("dve", ef[:], ef[:], eb[:], ALU.add, [ef, eb], [ef])
                s.tt("dve", dtsum[:, l, h, :], ef[:], dmt[:, 4, :], ALU.add, [ef, dmt], [dtsum])
            s.act(kqdec[:, l, 0, :], lg[:, 0:4], AF.Exp, [lg, pcl], [kqdec], scale=pcl[:, 0:1])
            s.act(kqdec[:, l, 1, :], lg[:, 4:8], AF.Exp, [lg, pcl], [kqdec], scale=pcl[:, 1:2])
            s.act(kqdec[:, l, 2, :], lg[:, 0:4], AF.Exp, [lg, pcl], [kqdec], scale=pcl[:, 2:3])
            s.act(kqdec[:, l, 3, :], lg[:, 4:8], AF.Exp, [lg, pcl], [kqdec], scale=pcl[:, 3:4])
            cd = s.sb("cd", [64, 2, 2], F32)
            for dr in range(2):
                for pp in range(2):
                    for hl in range(2):
                        h = 2 * pp + hl
                        s.cp("dve", cd[32 * hl:32 * hl + 32, dr, pp:pp + 1], lg[32 * hl:32 * hl + 32, dr * 4 + h:dr * 4 + h + 1], [lg], [cd])
            s.act(cdec[:, l, :, :], cd[:], AF.Exp, [cd], [cdec], scale=128.0)
    SENT["s"] = s
    if stop_after == 0:
        s.finish(); s.emit(); return nc

    for l in range(NL):
        with s.phase():
            win = s.sb("win", [128, 8, 1984], BF16)
            for k in range(8):
                s.dma("pool", win[:, k, :], w_in[l][k * 128:(k + 1) * 128, :], writes=[win])
            winsw = s.sb("winsw", [128, 8, 64], BF16)
            wv = w_in[l].rearrange("(k p) c -> p k c", p=128)
            s.dma("pool", winsw[:, :, 0:32], wv[:, :, 1568:1600], writes=[winsw])
            s.dma("pool", winsw[:, :, 32:64], wv[:, :, 1536:1568], writes=[winsw])
            wuq = s.sb("wuq", [128, 3, 768], BF16)
            s.dma("pool", wuq[:], w_uq[l].rearrange("(k p) c -> p k c", p=128), writes=[wuq])
            wuqsw = s.sb("wuqsw", [128, 3, NH, 64], BF16)
            wq4 = w_uq[l].rearrange("(k p) (h c) -> p k h c", p=128, h=NH)
            for h_ in range(NH):
                s.dma("pool", wuqsw[:, :, h_, 0:32], wq4[:, :, h_, 160:192], writes=[wuqsw])
                s.dma("pool", wuqsw[:, :, h_, 32:64], wq4[:, :, h_, 128:160], writes=[wuqsw])
            wukv = s.sb("wukv", [128, 2, 1024], BF16)
            s.dma("pool", wukv[:], w_ukv[l].rearrange("(k p) c -> p k c", p=128), writes=[wukv])
            wukv4 = wukv[:].rearrange("p k (h c) -> p k h c", h=NH)
            qg_t = s.sb("qg_t", [128, 3], F32); kvg_t = s.sb("kvg_t", [128, 2], F32)
            s.dma("sp", qg_t[:], qng[l], writes=[qg_t]); s.dma("sp", kvg_t[:], kvng[l], writes=[kvg_t])

            xts = s.sbpool("xt", [128, D], F32, 2)
            junk = s.sb("junk", [128, D], F32)
            sss = s.sbpool("ss", [128, 1], F32, 2)
            xns = s.sbpool("xn", [128, D], BF16, 2)
            pTs = s.pspool("pT", [128, 8, 128], BF16, 2)
            pms = s.pspool("pm", [128, 512], F32, 6)
            hTs = s.sbpool("hT", [128, 8, 512], BF16, 2)
            sgs = s.sbpool("sg", [128, 512], F32, 2)
            cqs = s.sb("cq", [128, 3, 512], F32)
            sqs = s.sb("sq", [128, 3, 512], BF16)
            cqn = s.sb("cqn", [128, 3, 512], BF16)
            rstd = s.sbpool("rstd", [128, 512], F32, 2)
            obs = s.sbpool("ob", [128, 512], BF16, 3)
            mct = s.sb("mct", [64, 512], F32); mst = s.sb("mst", [64, 512], F32)
            r1s = s.sbpool("r1", [64, 512], F32, 2); r2s = s.sbpool("r2", [64, 512], F32, 2)
            sqg = s.sbpool("sqg", [128, 384], F32, 2); skv = s.sbpool("skv", [128, 384], F32, 2)
            rcst = s.sbpool("rcst", [128, 32], F32, 2)
            tmpr = s.sbpool("tmpr", [128, 4, 4, 16], F32, 2)
            rot = s.sbpool("rot", [128, 2, 128], BF16, 2)
            rts = s.sbpool("rts", [64, 4, 128], BF16, 2)
            rvs = s.sbpool("rvs", [128, 256], BF16, 2)
            sgo = s.sbpool("sgo", [128, 256], F32, 2)
            zq = s.sbpool("zq", [128, 2, 512], BF16, 2)

            for (t0, n, r) in groups:
                nt = n // 128
                hT = hTs.next()
                for i in range(nt):
                    tok = t0 + i * 128
                    xt = xts.next()
                    if l == 0:
                        src = ctx_in[tok:tok + 128, :] if r == 1 else x_in[tok - C:tok - C + 128, :]
                        s.dma("sp", xt[:], src, writes=[xt])
                        s.dma("act", XR[tok:tok + 128, :], xt[:], reads=[xt], writes=[s.db("XR", tok // 128)])
                    else:
                        s.dma("sp", xt[:], XR[tok:tok + 128, :], reads=[s.db("XR", tok // 128)], writes=[xt])
                    ss = sss.next()
                    s.act(junk[:], xt[:], AF.Square, [xt], [junk, ss], accum_out=ss[:])
                    s.act(ss[:], ss[:], AF.Sqrt, [ss], [ss], scale=1.0 / D, bias=EPS)
                    s.recip(ss[:], ss[:], [ss], [ss])
                    xn = xns.next()
                    s.act(xn[:], xt[:], AF.Copy, [xt, ss], [xn], scale=ss[:])
                    pT = pTs.next()
                    for k in range(8):
                        s.tr(pT[:, k, :], xn[:, k * 128:(k + 1) * 128], ident_b[:], [xn, ident_b], [pT])
                    for k in range(8):
                        if k % 2 == 0:
                            s.ts("dve", hT[:, k, i * 128:(i + 1) * 128], pT[:, k, :], a1c[:, l, r, k:k + 1], b1c_[:, l, r, k:k + 1], ALU.mult, ALU.add, [pT, a1c, b1c_], [hT])
                        else:
                            s.act(hT[:, k, i * 128:(i + 1) * 128], pT[:, k, :], AF.Identity, [pT, a1c, b1c_], [hT], scale=a1c[:, l, r, k:k + 1], bias=b1c_[:, l, r, k:k + 1])

                def proj(cols, M):
                    p = pms.next()
                    for k in range(8):
                        s.mm(p[0:M, 0:n], cols(k), hT[:, k, 0:n], k == 0, k == 7, [hT, win, winsw], [p])
                    return p

                if CUT <= 1: continue
                z = zq.next()
                for c in range(2):
                    pa = proj(lambda k: win[:, k, c * 128:(c + 1) * 128], 128)
                    pg = proj(lambda k: win[:, k, 256 + c * 128:256 + (c + 1) * 128], 128)
                    sg = sgs.next()
                    s.act(sg[:, 0:n], pg[:, 0:n], AF.Sigmoid, [pg], [sg])
                    s.tt("dve", z[:, c, 0:n], pa[:, 0:n], sg[:, 0:n], ALU.mult, [pa, sg], [z])
                s.dma("sp", ZTD[:, :, t0:t0 + n], z[:, :, 0:n], reads=[z], writes=[s.db("ZT", t0)])

                if CUT <= 2: continue
                def featnorm(c0, nk, gt, outn, raw):
                    for k in range(nk):
                        p = proj(lambda kk: win[:, kk, c0 + k * 128:c0 + (k + 1) * 128], 128)
                        s.cp("act", raw[:, k, 0:n], p[:, 0:n], [p], [raw])
                        s.tt("pool", sqs[:, k, 0:n], raw[:, k, 0:n], raw[:, k, 0:n], ALU.mult, [raw], [sqs])
                    pss = pms.next()
                    for k in range(nk):
                        s.mm(pss[:, 0:n], ones_b[:], sqs[:, k, 0:n], k == 0, k == nk - 1, [ones_b, sqs], [pss])
                    rs = rstd.next()
                    s.act(rs[:, 0:n], pss[:, 0:n], AF.Sqrt, [pss], [rs], scale=1.0 / (nk * 128), bias=EPS)
                    s.recip(rs[:, 0:n], rs[:, 0:n], [rs], [rs])
                    for k in range(nk):
                        s.stt("dve", outn[:, k, 0:n], raw[:, k, 0:n], gt[:, k:k + 1], rs[:, 0:n], ALU.mult, ALU.mult, [raw, gt, rs], [outn])

                featnorm(512, 3, qg_t, cqn, cqs)
                s.dma("sp", mct[:, 0:n], mc_d[:, t0:t0 + n], writes=[mct])
                s.dma("sp", mst[:, 0:n], ms_d[:, t0:t0 + n], writes=[mst])

                def rope_fm(p_main, p_sw, dst, dkey):
                    r1 = r1s.next(); r2 = r2s.next()
                    s.tt("dve", r1[:, 0:n], p_main[0:64, 0:n], mct[:, 0:n], ALU.mult, [p_main, mct], [r1])
                    s.tt("dve", r2[:, 0:n], p_sw[0:64, 0:n], mst[:, 0:n], ALU.mult, [p_sw, mst], [r2])
                    ob = obs.next()
                    s.tt("pool", ob[0:64, 0:n], r1[:, 0:n], r2[:, 0:n], ALU.add, [r1, r2], [ob])
                    s.dma("sp", dst, ob[0:64, 0:n], reads=[ob], writes=[dkey])

                for h in range(NH):
                    p = pms.next()
                    for k in range(3):
                        s.mm(p[:, 0:n], wuq[:, k, h * 192:h * 192 + 128], cqn[:, k, 0:n], k == 0, k == 2, [wuq, cqn], [p])
                    ob = obs.next()
                    s.cp("act", ob[:, 0:n], p[:, 0:n], [p], [ob])
                    s.dma("sp", QN[h][:, t0:t0 + n], ob[:, 0:n], reads=[ob], writes=[s.db("QN", h, t0)])
                    p1 = pms.next(); p2 = pms.next()
                    for k in range(3):
                        s.mm(p1[0:64, 0:n], wuq[:, k, h * 192 + 128:h * 192 + 192], cqn[:, k, 0:n], k == 0, k == 2, [wuq, cqn], [p1])
                    for k in range(3):
                        s.mm(p2[0:64, 0:n], wuqsw[:, k, h, :], cqn[:, k, 0:n], k == 0, k == 2, [wuqsw, cqn], [p2])
                    rope_fm(p1, p2, QR[h][:, t0:t0 + n], s.db("QR", h, t0))

                if CUT <= 3: continue
                featnorm(1280, 2, kvg_t, cqn, cqs)
                for h in range(NH):
                    p = pms.next()
                    for k in range(2):
                        s.mm(p[:, 0:n], wukv4[:, k, h, 0:128], cqn[:, k, 0:n], k == 0, k == 1, [wukv, cqn], [p])
                    ob = obs.next()
                    s.cp("act", ob[:, 0:n], p[:, 0:n], [p], [ob])
                    s.dma("sp", KN[h][:, t0:t0 + n], ob[:, 0:n], reads=[ob], writes=[s.db("KN", h, t0)])
                for i in range(nt):
                    p = pms.next()
                    for h in range(NH):
                        for k in range(2):
                            s.mm(p[:, h * 128:(h + 1) * 128], cqn[:, k, i * 128:(i + 1) * 128], wukv4[:, k, h, 128:256], k == 0, k == 1, [wukv, cqn], [p])
                    ob = obs.next()
                    s.cp("act", ob[:], p[:], [p], [ob])
                    s.dma("sp", VV[t0 + i * 128:t0 + (i + 1) * 128, :], ob[:], reads=[ob], writes=[s.db("VV", (t0 + i * 128) // 128)])
                if CUT <= 4: continue
                p1 = proj(lambda k: win[:, k, 1536:1600], 64)
                p2 = proj(lambda k: winsw[:, k, :], 64)
                rope_fm(p1, p2, KR[:, t0:t0 + n], s.db("KR", t0))
                if CUT <= 5: continue
                for i in range(nt):
                    tok = t0 + i * 128
                    pq = pms.next(); pk = pms.next()
                    for k in range(8):
                        s.mm(pq[:, 0:384], hT[:, k, i * 128:(i + 1) * 128], win[:, k, 896:1280], k == 0, k == 7, [hT, win], [pq])
                    for k in range(8):
                        s.mm(pk[:, 0:384], hT[:, k, i * 128:(i + 1) * 128], win[:, k, 1600:1984], k == 0, k == 7, [hT, win], [pk])
                    a = sqg.next(); b = skv.next()
                    s.cp("act", a[:, 0:128], pq[:, 0:128], [pq], [a])
                    s.act(b[:, 0:128], pk[:, 0:128], AF.Identity, [pk], [b], scale=K_SCALE)
                    so = sgo.next()
                    s.act(so[:], pq[:, 128:384], AF.Sigmoid, [pq], [so])
                    s.tt("dve", so[:], so[:], pq[:, 128:384], ALU.mult, [so, pq], [so])
                    s.dma("act", SG[tok:tok + 128, :], so[:], reads=[so], writes=[s.db("SG", tok // 128)])
                    rv_ = rvs.next()
                    s.cp("dve", rv_[:], pk[:, 128:384], [pk], [rv_])
                    s.dma("act", RV[tok:tok + 128, :], rv_[:], reads=[rv_], writes=[s.db("RV", tok // 128)])
                    if CUT <= 6: continue
                    cs = rcst.next()
                    s.dma("act", cs[:], rcs_d[tok:tok + 128, :], writes=[cs])
                    cosb = cs[:, 0:16].unsqueeze(1).to_broadcast([128, 4, 16])
                    sinb = cs[:, 16:32].unsqueeze(1).to_broadcast([128, 4, 16])
                    ro = rot.next()
                    tm = tmpr.next()
                    for qi, srcT in enumerate((a, b)):
                        v4 = srcT[:, 0:128].rearrange("p (h two d) -> p h two d", h=4, two=2)
                        o4 = ro[:, qi, :].rearrange("p (h two d) -> p h two d", h=4, two=2)
                        x1 = v4[:, :, 0, :]; x2 = v4[:, :, 1, :]
                        s.tt("dve", tm[:, :, 0, :], x1, cosb, ALU.mult, [srcT, cs], [tm])
                        s.tt("dve", tm[:, :, 1, :], x2, sinb, ALU.mult, [srcT, cs], [tm])
                        s.tt("dve", tm[:, :, 2, :], x2, cosb, ALU.mult, [srcT, cs], [tm])
                        s.tt("dve", tm[:, :, 3, :], x1, sinb, ALU.mult, [srcT, cs], [tm])
                        s.tt("dve", o4[:, :, 0, :], tm[:, :, 0, :], tm[:, :, 1, :], ALU.subtract, [tm], [ro])
                        s.tt("dve", o4[:, :, 1, :], tm[:, :, 2, :], tm[:, :, 3, :], ALU.add, [tm], [ro])
                    if CUT <= 7: continue
                    s.dma("sp", RK[tok:tok + 128, :], ro[:, 1, :], reads=[ro], writes=[s.db("RK", tok // 128)])
                    pT = pTs.next()
                    for qi in range(2):
                        for pp in range(2):
                            s.tr(pT[0:64, qi * 2 + pp, :], ro[:, qi, pp * 64:(pp + 1) * 64], ident_b[:], [ro, ident_b], [pT])
                    rt = rts.next()
                    s.cp("dve", rt[:], pT[0:64, 0:4, :], [pT], [rt])
                    s.dma("sp", RQT[:, :, tok:tok + 128].rearrange("a p t -> p a t"), rt[:, 0:2, :], reads=[rt], writes=[s.db("RQT", tok // 128)])
                    s.dma("sp", RKT[:, :, tok:tok + 128].rearrange("a p t -> p a t"), rt[:, 2:4, :], reads=[rt], writes=[s.db("RKT", tok // 128)])
        if stop_after == "A":
            break
        with s.phase():
            cwt = s.sb("cwt", [128, 2, 31], F32); cpt = s.sb("cpt", [128, 2, 3], F32)
            s.dma("sp", cwt[:], convw[l], writes=[cwt]); s.dma("sp", cpt[:], convp[l], writes=[cpt])
            zts = s.sbpool("zt", [128, 2, 512 + 30], BF16, 2)
            accs = s.sbpool("acc", [128, 2, 512], F32, 2)
            ybs = s.sbpool("yb", [128, 2, 512], BF16, 2)
            yqs = s.sbpool("yq", [128, 2, 512], BF16, 2)
            pS = s.pspool("pS", [128, 512], F32, 2); pQ = s.pspool("pQ", [128, 512], F32, 2)
            mean = s.sbpool("mean", [128, 512], F32, 2); var = s.sbpool("var", [128, 512], F32, 2)
            msq = s.sbpool("msq", [128, 512], F32, 2)
            dd = s.sbpool("dd", [128, 512], F32, 2)
            oc = s.sbpool("oc", [128, 512], BF16, 3)
            for (t0, n, r) in groups:
                lo, hi = (0, C) if r == 1 else (C, TT)
                zt = zts.next()
                s.memset("pool", zt[:], 0.0, [zt])
                a0 = max(lo, t0 - 15); a1 = min(hi, t0 + n + 15)
                s.dma("sp", zt[:, :, a0 - (t0 - 15):a1 - (t0 - 15)], ZTD[:, :, a0:a1], reads=[s.db("ZT", g[0]) for g in groups], writes=[zt])
                acc = accs.next(); yb = ybs.next(); yq = yqs.next()
                for c in range(2):
                    eng = "dve"
                    s.ts(eng, acc[:, c, 0:n], zt[:, c, 0:n], cwt[:, c, 0:1], cpt[:, c, 0:1], ALU.mult, ALU.add, [zt, cwt, cpt], [acc])
                    for k in range(1, 31):
                        s.stt(eng, acc[:, c, 0:n], zt[:, c, k:k + n], cwt[:, c, k:k + 1], acc[:, c, 0:n], ALU.mult, ALU.add, [zt, cwt, acc], [acc])
                    s.cp("act", yb[:, c, 0:n], acc[:, c, 0:n], [acc], [yb])
                    s.act(yq[:, c, 0:n], acc[:, c, 0:n], AF.Square, [acc], [yq])
                ps_ = pS.next(); pq_ = pQ.next()
                for c in range(2):
                    s.mm(ps_[:, 0:n], ones_b[:], yb[:, c, 0:n], c == 0, c == 1, [ones_b, yb], [ps_])
                for c in range(2):
                    s.mm(pq_[:, 0:n], ones_b[:], yq[:, c, 0:n], c == 0, c == 1, [ones_b, yq], [pq_])
                mn = mean.next(); vr = var.next(); mq = msq.next()
                s.ts("dve", mn[:, 0:n], ps_[:, 0:n], 1.0 / 256, None, ALU.mult, None, [ps_], [mn])
                s.tt("pool", mq[:, 0:n], mn[:, 0:n], mn[:, 0:n], ALU.mult, [mn], [mq])
                s.stt("dve", vr[:, 0:n], pq_[:, 0:n], 1.0 / 256, mq[:, 0:n], ALU.mult, ALU.subtract, [pq_, mq], [vr])
                s.ts("dve", vr[:, 0:n], vr[:, 0:n], 0.0, None, ALU.max, None, [vr], [vr])
                s.act(vr[:, 0:n], vr[:, 0:n], AF.Sqrt, [vr], [vr], bias=1e-5)
                s.recip(vr[:, 0:n], vr[:, 0:n], [vr], [vr])
                for c in range(2):
                    d_ = dd.next()
                    s.tt("pool", d_[:, 0:n], acc[:, c, 0:n], mn[:, 0:n], ALU.subtract, [acc, mn], [d_])
                    s.tt("pool", d_[:, 0:n], d_[:, 0:n], vr[:, 0:n], ALU.mult, [d_, vr], [d_])
                    o = oc.next()
                    s.act(d_[:, 0:n], d_[:, 0:n], AF.Identity, [d_, cpt], [d_], scale=cpt[:, c, 1:2], bias=cpt[:, c, 2:3])
                    sg2 = msq.next()
                    s.act(sg2[:, 0:n], d_[:, 0:n], AF.Sigmoid, [d_], [sg2])
                    s.tt("dve", o[:, 0:n], d_[:, 0:n], sg2[:, 0:n], ALU.mult, [d_, sg2], [o])
                    s.dma("sp", MIXT[c * 128:(c + 1) * 128, t0:t0 + n], o[:, 0:n], reads=[o], writes=[s.db("MIXT", c, t0)])
        with s.phase():
            NCH = NT
            Sst = s.sb("Sst", [64, 2, 2, NCH, 64], BF16)
            Sf = [[s.sb("Sf", [64, 128], F32) for pp in range(2)] for dr in range(2)]
            for dr in range(2):
                for pp in range(2):
                    s.memset("dve", Sf[dr][pp][:], 0.0, [Sf[dr][pp]])
            qd = s.sb("qd", [64, 2, 2, 128], F32)
            irow = s.sb("irow", [64, 2, 128], F32)
            s.dma("sp", irow[:], irow_d, writes=[irow])
            cdl = s.sb("cdl", [64, 2, 2], F32)
            s.act(cdl[:], cdec[:, l, :, :], AF.Ln, [cdec], [cdl], scale=1.0)
            s.ts("dve", cdl[:], cdl[:], 1.0 / 128, None, ALU.mult, None, [cdl], [cdl])
            for dr in range(2):
                for pp in range(2):
                    s.act(qd[:, dr, pp, :], irow[:, dr, :], AF.Exp, [irow, cdl], [qd], scale=cdl[:, dr, pp:pp + 1])
            rkt = s.sbpool("rkt", [128, 128], BF16, 3); rvt = s.sbpool("rvt", [128, 256], BF16, 3)
            kds = s.sbpool("kd", [128, 128], BF16, 3)
            pkv = s.pspool("pkv", [64, 128], F32, 2)
            nchc = C // 128
            order_f = list(range(NCH))
            order_b = list(range(nchc - 1, -1, -1)) + list(range(NCH - 1, nchc - 1, -1))
            for dr, order in ((0, order_f), (1, order_b)):
                for ci in order:
                    tok = ci * 128
                    rk_ = rkt.next(); rv_ = rvt.next()
                    s.dma("sp", rk_[:], RK[tok:tok + 128, :], reads=[s.db("RK", ci)], writes=[rk_])
                    s.dma("act", rv_[:], RV[tok:tok + 128, :], reads=[s.db("RV", ci)], writes=[rv_])
                    kd = kds.next()
                    s.tt("pool", kd[:].rearrange("p (h d) -> p h d", h=4), rk_[:].rearrange("p (h d) -> p h d", h=4),
                         kqdec[:, l, dr, :].unsqueeze(2).to_broadcast([128, 4, 32]), ALU.mult, [rk_, kqdec], [kd])
                    for pp in range(2):
                        S_ = Sf[dr][pp]
                        s.cp("act", Sst[0:32, dr, pp, ci, :], S_[0:32, 0:64], [S_], [Sst])
                        s.cp("act", Sst[32:64, dr, pp, ci, :], S_[32:64, 64:128], [S_], [Sst])
                        p = pkv.next()
                        s.mm(p[:], kd[:, pp * 64:(pp + 1) * 64], rv_[:, pp * 128:(pp + 1) * 128], True, True, [kd, rv_], [p])
                        s.stt("dve", S_[:], S_[:], cdec[:, l, dr, pp:pp + 1], p[:], ALU.mult, ALU.add, [S_, cdec, p], [S_])
            qts = s.sbpool("qt", [64, 2, 128], BF16, 2); kts = s.sbpool("kt", [64, 2, 128], BF16, 2)
            qfs = s.sbpool("qf", [64, 2, 2, 128], BF16, 2)
            sgt = s.sbpool("sgt", [128, 256], F32, 2)
            pst = s.pspool("pst", [128, 128], F32, 2)
            po = s.pspool("po", [128, 256], F32, 2)
            pps = s.sbpool("pp", [128, 128], BF16, 3)
            osb = s.sbpool("osb", [128, 256], F32, 2); osq = s.sbpool("osq", [128, 256], F32, 2)
            ssm = s.sbpool("ssm", [128, 4], F32, 2)
            yrb = s.sbpool("yrb", [128, 256], BF16, 2)
            pTr = s.pspool("pTr", [128, 2, 128], BF16, 2)
            yrt = s.sbpool("yrt", [128, 2, 128], BF16, 2)
            for ci in range(NCH):
                tok = ci * 128
                qt = qts.next(); kt = kts.next(); rv_ = rvt.next(); sg_ = sgt.next()
                s.dma("sp", qt[:], RQT[:, :, tok:tok + 128].rearrange("a p t -> p a t"), reads=[s.db("RQT", ci)], writes=[qt])
                s.dma("sp", kt[:], RKT[:, :, tok:tok + 128].rearrange("a p t -> p a t"), reads=[s.db("RKT", ci)], writes=[kt])
                s.dma("act", rv_[:], RV[tok:tok + 128, :], reads=[s.db("RV", ci)], writes=[rv_])
                s.dma("act", sg_[:], SG[tok:tok + 128, :], reads=[s.db("SG", ci)], writes=[sg_])
                qf = qfs.next()
                for dr in range(2):
                    s.tt("pool", qf[:, dr, :, :], qt[:], qd[:, dr, :, :], ALU.mult, [qt, qd], [qf])
                o = po.next()
                for h in range(NH):
                    pp = h // 2; b0 = 32 * (h % 2)
                    st = pst.next()
                    s.mm(st[:], kt[b0:b0 + 32, pp, :], qt[b0:b0 + 32, pp, :], True, True, [kt, qt], [st])
                    P = pps.next()
                    s.tt("dve", P[:], st[:], dtsum[:, l, h, :], ALU.mult, [st, dtsum], [P])
                    s.mm(o[:, h * 64:(h + 1) * 64], P[:], rv_[:, h * 64:(h + 1) * 64], True, False, [P, rv_], [o])
                    s.mm(o[:, h * 64:(h + 1) * 64], qf[b0:b0 + 32, 0, pp, :], Sst[b0:b0 + 32, 0, pp, ci, :], False, False, [qf, Sst], [o])
                    s.mm(o[:, h * 64:(h + 1) * 64], qf[b0:b0 + 32, 1, pp, :], Sst[b0:b0 + 32, 1, pp, ci, :], False, True, [qf, Sst], [o])
                ob = osb.next(); oq = osq.next(); sm = ssm.next()
                s.cp("act", ob[:], o[:], [o], [ob])
                s.tt("pool", oq[:], ob[:], ob[:], ALU.mult, [ob], [oq])
                s.op("dve", lambda e, sm=sm, oq=oq: e.reduce_sum(out=sm[:], in_=oq[:].rearrange("p (h e) -> p h e", h=4), axis=AX.X), [oq], [sm])
                s.act(sm[:], sm[:], AF.Sqrt, [sm], [sm], scale=1.0 / 64, bias=EPS)
                s.recip(sm[:], sm[:], [sm], [sm])
                s.tt("pool", ob[:].rearrange("p (h e) -> p h e", h=4), ob[:].rearrange("p (h e) -> p h e", h=4), sm[:].unsqueeze(2).to_broadcast([128, 4, 64]), ALU.mult, [ob, sm], [ob])
                yb = yrb.next()
                s.tt("pool", yb[:], ob[:], sg_[:], ALU.mult, [ob, sg_], [yb])
                pt = pTr.next()
                for c in range(2):
                    s.tr(pt[:, c, :], yb[:, c * 128:(c + 1) * 128], ident_b[:], [yb, ident_b], [pt])
                yt = yrt.next()
                s.cp("dve", yt[:], pt[:], [pt], [yt])
                s.dma("sp", MIXT[768:1024, tok:tok + 128].rearrange("(c p) t -> p c t", p=128), yt[:], reads=[yt], writes=[s.db("MIXT", "r", ci)])
        with s.phase():
            knt = s.sb("knt", [128, TT], BF16); krt = s.sb("krt", [128, TT], BF16); vt = s.sb("vt", [128, NT, 128], BF16)
            s.memset("pool", krt[:], 0.0, [krt])
            s.dma("sp", krt[0:64, :], KR, reads=[s.db("KR", g[0]) for g in groups], writes=[krt])
            qnt = s.sbpool("qnt", [128, 512], BF16, 2); qrt = s.sbpool("qrt", [128, 512], BF16, 2)
            for q_ in qrt.tiles:
                s.memset("pool", q_[:], 0.0, [q_])
            pst = s.pspool("pst", [128, 512], F32, 4)
            pacc = s.pspool("pacc", [128, 512], F32, 2); pden = s.pspool("pden", [128, 512], F32, 2)
            Ps = s.sbpool("P", [128, 512], BF16, 6)
            paccA = s.sbpool("paccA", [128, 512], F32, 2); paccB = s.sbpool("paccB", [128, 512], F32, 2)
            rec = s.sbpool("rec", [128, 512], F32, 2)
            oat = s.sbpool("oat", [128, 512], BF16, 2)
            for h in range(NH):
                s.dma("sp", knt[:], KN[h], reads=[s.db("KN", h, g[0]) for g in groups], writes=[knt])
                s.dma("act", vt[:], VV[:, h * 128:(h + 1) * 128].rearrange("(t p) c -> p t c", p=128), reads=[s.db("VV", i) for i in range(NT)], writes=[vt])
                for (t0, n, r) in groups:
                    qn = qnt.next(); qr = qrt.next()
                    s.dma("sp", qn[:, 0:n], QN[h][:, t0:t0 + n], reads=[s.db("QN", h, t0)], writes=[qn])
                    s.dma("act", qr[0:64, 0:n], QR[h][:, t0:t0 + n], reads=[s.db("QR", h, t0)], writes=[qr])
                    nk = C // 128 if r == 1 else NT
                    acc = pacc.next(); den = pden.next()
                    pa = [paccA.next(), paccB.next()]
                    sts = {}; Pt = {}
                    SK = 2
                    for jt in range(nk + SK):
                        if jt < nk:
                            st = pst.next()
                            s.mm(st[:, 0:n], knt[:, jt * 128:(jt + 1) * 128], qn[:, 0:n], True, False, [knt, qn], [st])
                            s.mm(st[:, 0:n], krt[:, jt * 128:(jt + 1) * 128], qr[:, 0:n], False, True, [krt, qr], [st])
                            P = Ps.next()
                            s.act(P[:, 0:n], st[:, 0:n], AF.Exp, [st], [P], scale=ATT_SCALE)
                            Pt[jt] = P
                        if jt >= SK:
                            j = jt - SK
                            P = Pt.pop(j)
                            s.mm(acc[:, 0:n], vt[:, j, :], P[:, 0:n], j == 0, j == nk - 1, [vt, P], [acc])
                            eng = "dve" if j % 2 == 0 else "pool"
                            A_ = pa[j % 2]
                            if j < 2:
                                s.cp(eng, A_[:, 0:n], P[:, 0:n], [P], [A_])
                            else:
                                s.tt(eng, A_[:, 0:n], A_[:, 0:n], P[:, 0:n], ALU.add, [A_, P], [A_])
                    nA = 2 if nk >= 2 else 1
                    for a_ in range(nA):
                        s.mm(den[:, 0:n], ones_f[:], pa[a_][:, 0:n], a_ == 0, a_ == nA - 1, [ones_f, pa[a_]], [den])
                    rc = rec.next()
                    s.recip(rc[:, 0:n], den[:, 0:n], [den], [rc])
                    oa = oat.next()
                    s.tt("dve", oa[:, 0:n], acc[:, 0:n], rc[:, 0:n], ALU.mult, [acc, rc], [oa])
                    s.dma("sp", MIXT[256 + h * 128:256 + (h + 1) * 128, t0:t0 + n], oa[:, 0:n], reads=[oa], writes=[s.db("MIXT", "a", h, t0)])
        if stop_after == "D":
            break
        with s.phase():
            wo = s.sb("wo", [128, 8, D], BF16)
            for k in range(8):
                s.dma("pool", wo[:, k, :], w_out[l][k * 128:(k + 1) * 128, :], writes=[wo])
            rw = s.sb("rw", [128, 8, NE], F32)
            s.dma("sp", rw[:], router_w[l].rearrange("(k p) e -> p k e", p=128), writes=[rw])
            rb = s.sb("rb", [128, NE], F32)
            s.dma("sp", rb[:], router_b[l:l + 1, :].to_broadcast([128, NE]), writes=[rb])
            g2b = s.sb("g2b", [128, D], F32)
            s.dma("sp", g2b[:], g2[l:l + 1, :].to_broadcast([128, D]), writes=[g2b])
            GT1 = []; A2 = []; B2 = []
            for r in range(2):
                gt = s.sb("gt1", [128, D], F32); a2 = s.sb("a2", [128, D], F32); b2_ = s.sb("b2_", [128, D], F32)
                s.dma("sp", gt[:], MOD[l][r:r + 1, 2 * D:3 * D].to_broadcast([128, D]), reads=[s.db("MOD")], writes=[gt])
                s.dma("sp", b2_[:], MOD[l][r:r + 1, 3 * D:4 * D].to_broadcast([128, D]), reads=[s.db("MOD")], writes=[b2_])
                s.dma("sp", a2[:], MOD[l][r:r + 1, 4 * D:5 * D].to_broadcast([128, D]), reads=[s.db("MOD")], writes=[a2])
                s.stt("dve", a2[:], a2[:], 1.0, g2b[:], ALU.add, ALU.mult, [a2, g2b], [a2])
                GT1.append(gt); A2.append(a2); B2.append(b2_)
            mts = s.sbpool("mt", [128, 8, 128], BF16, 2)
            xts = s.sbpool("xt", [128, D], F32, 2)
            pmx = s.pspool("pmx", [128, D], F32, 2)
            tmps = s.sbpool("tmp", [128, D], F32, 2)
            junk = s.sb("junk", [128, D], F32)
            sss = s.sbpool("ss", [128, 1], F32, 2)
            h2s = s.sbpool("h2", [128, D], F32, 3)
            pT2 = s.pspool("pT2", [128, 8, 128], BF16, 1); pT3 = s.pspool("pT3", [128, 8, 128], BF16, 1)
            h2b = s.sbpool("h2b", [128, 16, 128], BF16, 2)
            hhis = s.sbpool("hhi", [128, D], BF16, 2); hlos = s.sbpool("hlo", [128, D], BF16, 2)
            rwh = s.sb("rwh", [128, 8, NE], BF16); rwl = s.sb("rwl", [128, 8, NE], BF16)
            s.cp("dve", rwh[:], rw[:], [rw], [rwh])
            s.tt("dve", rw[:], rw[:], rwh[:], ALU.subtract, [rw, rwh], [rw])
            s.cp("dve", rwl[:], rw[:], [rw], [rwl])
            plg = s.pspool("plg", [128, NE], F32, 2)
            lgs = s.sbpool("lgs", [128, NE], F32, 2); t8 = s.sbpool("t8", [128, 8], F32, 2)
            msk = s.sbpool("msk", [128, NE], F32, 2); ex = s.sbpool("ex", [128, NE], F32, 2)
            nm = s.sbpool("nm", [128, 1], F32, 2); sm1 = s.sbpool("sm1", [128, 1], F32, 2)
            pend2 = None
            for ti in range(NT):
                tok = ti * 128
                r = 1 if tok < C else 0
                if r == 1 and l == NL - 1:
                    continue
                mt = mts.next()
                s.dma("sp", mt[:], MIXT[:, tok:tok + 128].rearrange("(k p) t -> p k t", p=128), reads=[s.db(*k_) for k_ in list(s.dbufs) if k_[0] == "MIXT"], writes=[mt])
                xt = xts.next()
                s.dma("act", xt[:], XR[tok:tok + 128, :], reads=[s.db("XR", ti)], writes=[xt])
                pm = pmx.next()
                for hf in range(2):
                    for k in range(8):
                        s.mm(pm[:, hf * 512:(hf + 1) * 512], mt[:, k, :], wo[:, k, hf * 512:(hf + 1) * 512], k == 0, k == 7, [mt, wo], [pm])
                tp = tmps.next()
                for hf in range(2):
                    s.tt("dve", tp[:, hf * 512:(hf + 1) * 512], pm[:, hf * 512:(hf + 1) * 512], GT1[r][:, hf * 512:(hf + 1) * 512], ALU.mult, [pm, GT1[r]], [tp])
                s.tt("pool", xt[:], xt[:], tp[:], ALU.add, [xt, tp], [xt])
                s.dma("sp", XR[tok:tok + 128, :], xt[:], reads=[xt], writes=[s.db("XR", ti)])
                if CUT == 11: continue
                ss = sss.next()
                s.act(junk[:], xt[:], AF.Square, [xt], [junk, ss], accum_out=ss[:])
                s.act(ss[:], ss[:], AF.Sqrt, [ss], [ss], scale=1.0 / D, bias=EPS)
                s.recip(ss[:], ss[:], [ss], [ss])
                h2 = h2s.next()
                s.stt("dve", h2[:], xt[:], ss[:, 0:1], A2[r][:], ALU.mult, ALU.mult, [xt, ss, A2[r]], [h2])
                s.tt("pool", h2[:], h2[:], B2[r][:], ALU.add, [h2, B2[r]], [h2])
                def stage2(h2=h2, tok=tok, ti=ti):
                    hhi = hhis.next(); hlo = hlos.next()
                    s.cp("act", hhi[:], h2[:], [h2], [hhi])
                    s.tt("pool", h2[:], h2[:], hhi[:], ALU.subtract, [h2, hhi], [h2])
                    s.cp("pool", hlo[:], h2[:], [h2], [hlo])
                    pt = pT2.next(); ptl = pT3.next()
                    for k in range(8):
                        s.tr(pt[:, k, :], hhi[:, k * 128:(k + 1) * 128], ident_b[:], [hhi, ident_b], [pt])
                    for k in range(8):
                        s.tr(ptl[:, k, :], hlo[:, k * 128:(k + 1) * 128], ident_b[:], [hlo, ident_b], [ptl])
                    hb_ = h2b.next()
                    s.cp("dve", hb_[:, 0:8, :], pt[:], [pt], [hb_])
                    s.cp("dve", hb_[:, 8:16, :], ptl[:], [ptl], [hb_])
                    for k in range(8):
                        s.dma("sp" if k % 2 == 0 else "act", H2T[k * 128:(k + 1) * 128, tok:tok + 128], hb_[:, k, :], reads=[hb_], writes=[s.db("H2T", ti)])
                    pl_ = plg.next()
                    for k in range(8):
                        s.mm(pl_[:], hb_[:, k, :], rwh[:, k, :], k == 0, False, [hb_, rwh], [pl_])
                        s.mm(pl_[:], hb_[:, 8 + k, :], rwh[:, k, :], False, False, [hb_, rwh], [pl_])
                        s.mm(pl_[:], hb_[:, k, :], rwl[:, k, :], False, k == 7, [hb_, rwl], [pl_])
                    lg_ = lgs.next()
                    s.tt("dve", lg_[:], pl_[:], rb[:], ALU.add, [pl_, rb], [lg_])
                    t8_ = t8.next()
                    s.op("dve", lambda e, t8_=t8_, lg_=lg_: e.max(out=t8_[:], in_=lg_[:]), [lg_], [t8_])
                    mk_ = msk.next()
                    s.ts("dve", mk_[:], lg_[:], t8_[:, 3:4], None, ALU.is_ge, None, [lg_, t8_], [mk_])
                    nm_ = nm.next()
                    s.ts("dve", nm_[:], t8_[:, 0:1], -1.0, None, ALU.mult, None, [t8_], [nm_])
                    ex_ = ex.next()
                    s.act(ex_[:], lg_[:], AF.Exp, [lg_, nm_], [ex_], bias=nm_[:, 0:1])
                    s.tt("dve", ex_[:], ex_[:], mk_[:], ALU.mult, [ex_, mk_], [ex_])
                    sm_ = sm1.next()
                    s.op("dve", lambda e, sm_=sm_, ex_=ex_: e.reduce_sum(out=sm_[:], in_=ex_[:], axis=AX.X), [ex_], [sm_])
                    s.recip(sm_[:], sm_[:], [sm_], [sm_])
                    s.ts("dve", ex_[:], ex_[:], sm_[:, 0:1], None, ALU.mult, None, [ex_, sm_], [ex_])
                    s.dma("sp", GATE[tok:tok + 128, :], ex_[:], reads=[ex_], writes=[s.db("GATE", ti)])
                if pend2 is not None:
                    pend2()
                pend2 = stage2
                if ti == NT - 1:
                    pend2(); pend2 = None
        if stop_after == "E":
            break
        with s.phase():
            b1t = s.sb("b1t", [128, NE, 16], F32)
            s.dma("sp", b1t[:], b1c[l], writes=[b1t])
            GT2 = []
            for r in range(2):
                gt = s.sb("gt2", [128, D], F32)
                s.dma("sp", gt[:], MOD[l][r:r + 1, 5 * D:6 * D].to_broadcast([128, D]), reads=[s.db("MOD")], writes=[gt])
                GT2.append(gt)
            wps = s.sbpool("wp", [128, 8, 512], BF16, 8)
            b2s = s.sbpool("b2s", [128, D], F32, 2)
            hts = s.sb("hts", [128, 8, 1024], BF16)
            yacc = s.sb("yacc", [128, 8, D], F32)
            gts = s.sb("gts", [128, 8, NE], F32)
            actt = s.sb("actt", [128, 8, 1024], BF16)
            actt_b = [Buf("actt0"), Buf("actt1")]
            gq = s.sbpool("gq", [128, 512], F32, 3); sq_ = s.sbpool("sq_", [128, 512], F32, 3)
            lq = s.sbpool("lq", [128, 512], F32, 3); tq = s.sbpool("tq", [128, 512], F32, 3)
            tmo = s.sbpool("tmo", [128, 512], F32, 2)
            xts = s.sbpool("xt", [128, D], F32, 2)
            pg_ = s.pspool("pg", [128, 512], F32, 2); pl_ = s.pspool("pl", [128, 512], F32, 2); po_ = s.pspool("po", [128, 512], F32, 3)
            sgs_ = []
            ti = 0
            while ti < NT:
                nt_ = (C // 128) if ti < C // 128 else min(8, NT - ti)
                nt_ = min(nt_, 8)
                sgs_.append((ti, nt_)); ti += nt_
            for e_ in range(NE):
                for q in range(4):
                    wp = wps.next()
                    s.dma("pool", wp[:], w1[l][e_][:, q * 512:(q + 1) * 512].rearrange("(k p) c -> p k c", p=128), writes=[wp])
                    s.dma("sp" if q % 2 == 0 else "act", W1B[l][e_][q], wp[:].rearrange("p k c -> p (k c)"), reads=[wp], writes=[s.db("W1B", e_, q)])
                for q in range(2):
                    wp = wps.next()
                    s.dma("pool", wp[:], w2[l][e_][:, q * 512:(q + 1) * 512].rearrange("(k p) c -> p k c", p=128), writes=[wp])
                    s.dma("sp" if q % 2 == 0 else "act", W2B[l][e_][q], wp[:].rearrange("p k c -> p (k c)"), reads=[wp], writes=[s.db("W2B", e_, q)])
            for (ti0, ntl) in sgs_:
                S_ = ntl * 128; tok0 = ti0 * 128
                r = 1 if tok0 < C else 0
                if r == 1 and l == NL - 1:
                    continue
                s.dma("sp", hts[:, :, 0:S_], H2T[:, tok0:tok0 + S_].rearrange("(k p) t -> p k t", p=128), reads=[s.db("H2T", ti0 + i) for i in range(ntl)], writes=[hts])
                s.dma("act", gts[:, 0:ntl, :], GATE[tok0:tok0 + S_, :].rearrange("(i p) e -> p i e", p=128), reads=[s.db("GATE", ti0 + i) for i in range(ntl)], writes=[gts])
                s.memset("pool", yacc[:], 0.0, [yacc])
                ngs = [(a, min(512, S_ - a)) for a in range(0, S_, 512)]
                for e_ in range(NE):
                    pieces = {}
                    for q in (0, 2, 1, 3):
                        wp = wps.next()
                        s.dma("sp", wp[:].rearrange("p k c -> p (k c)"), W1B[l][e_][q], reads=[s.db("W1B", e_, q)], writes=[wp])
                        pieces[q] = wp
                    w2p = []
                    for q in range(2):
                        wp = wps.next()
                        s.dma("sp", wp[:].rearrange("p k c -> p (k c)"), W2B[l][e_][q], reads=[s.db("W2B", e_, q)], writes=[wp])
                        w2p.append(wp)
                    b2t = b2s.next()
                    s.dma("sp", b2t[:], b2[l][e_:e_ + 1, :].to_broadcast([128, D]), writes=[b2t])
                    pend = None
                    for (a, nn) in ngs:
                        for c in range(8):
                            wg = pieces[c // 4]; wl = pieces[2 + c // 4]; cc_ = c % 4
                            pg = pg_.next(); pl = pl_.next()
                            for k in range(8):
                                s.mm(pg[:, 0:nn], wg[:, k, cc_ * 128:(cc_ + 1) * 128], hts[:, k, a:a + nn], k == 0, k == 7, [wg, hts], [pg])
                            for k in range(8):
                                s.mm(pl[:, 0:nn], wl[:, k, cc_ * 128:(cc_ + 1) * 128], hts[:, k, a:a + nn], k == 0, k == 7, [wl, hts], [pl])
                            g_ = gq.next(); sg_ = sq_.next(); l_ = lq.next(); t_ = tq.next()
                            s.ts("dve", g_[:, 0:nn], pg[:, 0:nn], b1t[:, e_, c:c + 1], 7.0, ALU.add, ALU.min, [pg, b1t], [g_])
                            if pend is not None:
                                pend()
                            s.act(sg_[:, 0:nn], g_[:, 0:nn], AF.Sigmoid, [g_], [sg_], scale=1.702)
                            s.act(l_[:, 0:nn], pl[:, 0:nn], AF.Identity, [pl, b1t], [l_], bias=b1t[:, e_, 8 + c:9 + c])
                            s.ts("pool", l_[:, 0:nn], l_[:, 0:nn], 7.0, -7.0, ALU.min, ALU.max, [l_], [l_])
                            s.tt("pool", t_[:, 0:nn], g_[:, 0:nn], sg_[:, 0:nn], ALU.mult, [g_, sg_], [t_])
                            pend = (lambda c=c, a=a, nn=nn, l_=l_, t_=t_: s.stt("dve", actt[:, c, a:a + nn], l_[:, 0:nn], 1.0, t_[:, 0:nn], ALU.add, ALU.mult, [l_, t_], [actt_b[a // 512]]))
                    pend()
                    for i in range(ntl):
                        for hf in range(2):
                            po = po_.next()
                            for k in range(8):
                                s.mm(po[:], actt[:, k, i * 128:(i + 1) * 128], w2p[hf][:, k, :], k == 0, k == 7, [actt_b[i // 4], w2p[hf]], [po])
                            tm = tmo.next()
                            s.tt("dve", tm[:], po[:], b2t[:, hf * 512:(hf + 1) * 512], ALU.add, [po, b2t], [tm])
                            s.stt("dve", yacc[:, i, hf * 512:(hf + 1) * 512], tm[:], gts[:, i, e_:e_ + 1], yacc[:, i, hf * 512:(hf + 1) * 512], ALU.mult, ALU.add, [tm, gts, yacc], [yacc])
                for i in range(ntl):
                    tok = tok0 + i * 128
                    xt = xts.next()
                    s.dma("sp", xt[:], XR[tok:tok + 128, :], reads=[s.db("XR", ti0 + i)], writes=[xt])
                    s.tt("dve", yacc[:, i, :], yacc[:, i, :], GT2[r][:], ALU.mult, [yacc, GT2[r]], [yacc])
                    s.tt("pool", xt[:], xt[:], yacc[:, i, :], ALU.add, [xt, yacc], [xt])
                    s.dma("sp", XR[tok:tok + 128, :], xt[:], reads=[xt], writes=[s.db("XR", ti0 + i)])
    if stop_after is None:
        with s.phase():
            gfb = s.sb("gfb", [128, D], F32)
            s.dma("sp", gfb[:], gf.rearrange("(o d) -> o d", o=1).to_broadcast([128, D]), writes=[gfb])
            xts = s.sbpool("xt", [128, D], F32, 3)
            junk = s.sb("junk", [128, D], F32)
            sss = s.sbpool("ss", [128, 1], F32, 2)
            for ti in range(C // 128, NT):
                tok = ti * 128
                xt = xts.next()
                s.dma("sp", xt[:], XR[tok:tok + 128, :], reads=[s.db("XR", ti)], writes=[xt])
                ss = sss.next()
                s.act(junk[:], xt[:], AF.Square, [xt], [junk, ss], accum_out=ss[:])
                s.act(ss[:], ss[:], AF.Sqrt, [ss], [ss], scale=1.0 / D, bias=EPS)
                s.recip(ss[:], ss[:], [ss], [ss])
                s.stt("dve", xt[:], xt[:], ss[:, 0:1], gfb[:], ALU.mult, ALU.mult, [xt, ss, gfb], [xt])
                s.dma("act", y_out[tok - C:tok - C + 128, :], xt[:], reads=[xt])
    s.finish()
    s.emit()
    return nc


def kernel(**inputs):
    inp = {k: np.asarray(v) for k, v in inputs.items()}
    B, L, _ = inp["x"].shape
    C = inp["ctx"].shape[1]
    NL = inp["w_ada"].shape[0]
    nc = build(L, C, NL)
    cst = host_consts(L, C)
    in_maps = []
    for b in range(B):
        m = host_layout(inp, b, NL)
        m.update(cst)
        in_maps.append({k: np.ascontiguousarray(v, dtype=np.float32) for k, v in m.items()})
    res = run_bass_kernel_spmd(nc, in_maps, core_ids=list(range(B)))
    return np.stack([np.asarray(r["y"], dtype=np.float32) for r in res.results], 0)
```
